# Optimizing a Trainium2 kernel written in Bass

```python
import jax, jax.numpy as jnp
from jax import lax
import numpy as np

D_MODEL = 2048
BATCH = 8
SEQ = 4096
DEPTH = 2

CHUNK = 64
Q_BLOCK = 128
HEAD_DIM = 128
N_HEADS = D_MODEL // HEAD_DIM
N_FOX = N_HEADS // 2
N_SB = N_HEADS - N_FOX
N_DSA = N_HEADS
IDX_HEADS = 16
IDX_DIM = 64
TOPK_MAX = 256
D_FF = 4 * D_MODEL
ROPE_THETA = 10000.0
EPS = 1e-6
MIX_WIDTH = N_HEADS * HEAD_DIM
N_EVEN = (DEPTH + 1) // 2
N_ODD = DEPTH // 2
EVEN_SPLITS = [N_FOX * HEAD_DIM, 2 * N_FOX * HEAD_DIM, 3 * N_FOX * HEAD_DIM,
               3 * N_FOX * HEAD_DIM + N_FOX,
               3 * N_FOX * HEAD_DIM + N_FOX + N_SB * HEAD_DIM,
               3 * N_FOX * HEAD_DIM + N_FOX + 2 * N_SB * HEAD_DIM]
EVEN_WIDTH = 3 * N_FOX * HEAD_DIM + N_FOX + 3 * N_SB * HEAD_DIM
ODD_SPLITS = [N_DSA * HEAD_DIM, N_DSA * HEAD_DIM + HEAD_DIM, N_DSA * HEAD_DIM + 2 * HEAD_DIM,
              N_DSA * HEAD_DIM + 2 * HEAD_DIM + IDX_HEADS * IDX_DIM,
              N_DSA * HEAD_DIM + 2 * HEAD_DIM + IDX_HEADS * IDX_DIM + IDX_DIM]
ODD_WIDTH = N_DSA * HEAD_DIM + 2 * HEAD_DIM + IDX_HEADS * IDX_DIM + IDX_DIM + IDX_HEADS

kernel_name = "hybrid_fox_stickbreak_dsa_trunk"


def rms_norm(x, g):
    xf = x.astype(jnp.float32)
    y = xf * lax.rsqrt(jnp.mean(xf * xf, axis=-1, keepdims=True) + EPS)
    return (y * g.astype(jnp.float32)).astype(x.dtype)


def rope(x, pos):
    half = x.shape[-1] // 2
    inv = ROPE_THETA ** (-jnp.arange(half, dtype=jnp.float32) / half)
    ang = pos.astype(jnp.float32)[..., None] * inv
    cos = jnp.cos(ang)[:, :, None, :]
    sin = jnp.sin(ang)[:, :, None, :]
    xf = x.astype(jnp.float32)
    x1, x2 = xf[..., :half], xf[..., half:]
    return jnp.concatenate([x1 * cos - x2 * sin, x2 * cos + x1 * sin], axis=-1).astype(x.dtype)


def fox_sb_mixer(h, w_in, b_forget):
    B, S, _ = h.shape
    proj = h @ w_in
    fq, fk, fv, fg, sq, sk, sv = jnp.split(proj, EVEN_SPLITS, axis=-1)
    fq, fk, fv = (a.reshape(B, S, N_FOX, HEAD_DIM) for a in (fq, fk, fv))
    sq, sk, sv = (a.reshape(B, S, N_SB, HEAD_DIM) for a in (sq, sk, sv))
    log_f = jax.nn.log_sigmoid(fg.astype(jnp.float32) + b_forget.astype(jnp.float32))
    F = jnp.cumsum(log_f, axis=1).transpose(0, 2, 1)
    scale = HEAD_DIM ** -0.5
    outs_f, outs_s = [], []
    for q0 in range(0, S, Q_BLOCK):
        q1 = q0 + Q_BLOCK
        t = jnp.arange(q0, q1)[:, None]
        s = jnp.arange(q1)[None, :]
        logit = jnp.einsum('bthd,bshd->bhts', fq[:, q0:q1], fk[:, :q1]).astype(jnp.float32) * scale
        decay = F[:, :, q0:q1, None] - F[:, :, None, :q1]
        logit = jnp.where(s <= t, logit + decay, -jnp.inf)
        p = jax.nn.softmax(logit, axis=-1).astype(h.dtype)
        outs_f.append(jnp.einsum('bhts,bshd->bthd', p, fv[:, :q1]))
        z = jnp.einsum('bthd,bshd->bhts', sq[:, q0:q1], sk[:, :q1]).astype(jnp.float32) * scale
        strict = s < t
        log_1mb = jnp.where(strict, jax.nn.log_sigmoid(-z), 0.0)
        suffix = lax.cumsum(log_1mb, axis=3, reverse=True) - log_1mb
        A = jnp.where(strict, jnp.exp(jax.nn.log_sigmoid(z) + suffix), 0.0).astype(h.dtype)
        outs_s.append(jnp.einsum('bhts,bshd->bthd', A, sv[:, :q1]))
    o_f = jnp.concatenate(outs_f, axis=1).reshape(B, S, N_FOX * HEAD_DIM)
    o_s = jnp.concatenate(outs_s, axis=1).reshape(B, S, N_SB * HEAD_DIM)
    return jnp.concatenate([o_f, o_s], axis=-1)


def dsa_mixer(h, w_in, pos):
    B, S, _ = h.shape
    topk = min(TOPK_MAX, S // 4)
    proj = h @ w_in
    q, k, v, qi, ki, wi = jnp.split(proj, ODD_SPLITS, axis=-1)
    q = rope(q.reshape(B, S, N_DSA, HEAD_DIM), pos)
    k = rope(k.reshape(B, S, 1, HEAD_DIM), pos)[:, :, 0]
    qi = rope(qi.reshape(B, S, IDX_HEADS, IDX_DIM), pos)
    ki = rope(ki.reshape(B, S, 1, IDX_DIM), pos)[:, :, 0]
    wi = wi.astype(jnp.float32) * (IDX_HEADS ** -0.5)
    gather = jax.vmap(lambda tab, idx: tab[idx])
    scale = HEAD_DIM ** -0.5
    outs = []
    for q0 in range(0, S, Q_BLOCK):
        q1 = q0 + Q_BLOCK
        Lk = min(S, max(q1, topk))
        t = jnp.arange(q0, q1)
        s = jnp.arange(Lk)
        adm = (s // CHUNK)[None, :] <= (t // CHUNK)[:, None]
        isc = jnp.einsum('bthd,bsd->bths', qi[:, q0:q1], ki[:, :Lk]).astype(jnp.float32) * (IDX_DIM ** -0.5)
        I = jnp.einsum('bths,bth->bts', jax.nn.relu(isc), wi[:, q0:q1])
        I = jnp.where(adm[None], I, -jnp.inf)
        vals, idx = lax.top_k(I, topk)
        valid = jnp.isfinite(vals)
        ksel = gather(k, idx)
        vsel = gather(v, idx)
        sc = jnp.einsum('bthd,btkd->bthk', q[:, q0:q1], ksel).astype(jnp.float32) * scale
        sc = jnp.where(valid[:, :, None, :], sc, -jnp.inf)
        p = jax.nn.softmax(sc, axis=-1).astype(h.dtype)
        outs.append(jnp.einsum('bthk,btkd->bthd', p, vsel))
    return jnp.concatenate(outs, axis=1).reshape(B, S, N_DSA * HEAD_DIM)


def setup_inputs(seed: int = 0) -> dict:
    key = jax.random.key(seed)
    ks = jax.random.split(key, 16)
    f32 = jnp.float32
    x = jax.random.normal(ks[0], (BATCH, SEQ, D_MODEL), f32)
    c = jax.random.normal(ks[1], (BATCH, D_MODEL), f32)
    offset = jax.random.randint(ks[2], (BATCH, 1), 0, 4096, dtype=jnp.int32)
    positions = (offset + jnp.arange(SEQ, dtype=jnp.int32)[None, :]).astype(jnp.int32)
    ada_w = jax.random.normal(ks[3], (DEPTH, D_MODEL, 6 * D_MODEL), f32) * (0.5 * D_MODEL ** -0.5)
    ada_b = jax.random.normal(ks[4], (DEPTH, 6 * D_MODEL), f32) * 0.02
    norm_g = 1.0 + 0.1 * jax.random.normal(ks[5], (DEPTH, 4, D_MODEL), f32)
    mix_w_out = jax.random.normal(ks[6], (DEPTH, MIX_WIDTH, D_MODEL), f32) * (MIX_WIDTH ** -0.5)
    even_w_in = jax.random.normal(ks[7], (N_EVEN, D_MODEL, EVEN_WIDTH), f32) * (D_MODEL ** -0.5)
    even_b_forget = 3.0 + 0.5 * jax.random.normal(ks[8], (N_EVEN, N_FOX), f32)
    odd_w_in = jax.random.normal(ks[9], (N_ODD, D_MODEL, ODD_WIDTH), f32) * (D_MODEL ** -0.5)
    ff_w1 = jax.random.normal(ks[10], (DEPTH, D_MODEL, D_FF), f32) * (D_MODEL ** -0.5)
    ff_w2 = jax.random.normal(ks[11], (DEPTH, D_FF, D_MODEL), f32) * (D_FF ** -0.5)
    return {"x": x, "c": c, "positions": positions, "ada_w": ada_w, "ada_b": ada_b,
            "norm_g": norm_g, "mix_w_out": mix_w_out, "even_w_in": even_w_in,
            "even_b_forget": even_b_forget, "odd_w_in": odd_w_in,
            "ff_w1": ff_w1, "ff_w2": ff_w2}


def reference(x, c, positions, ada_w, ada_b, norm_g, mix_w_out, even_w_in,
              even_b_forget, odd_w_in, ff_w1, ff_w2):
    cs = jax.nn.silu(c)
    for l in range(DEPTH):
        mod = (cs @ ada_w[l] + ada_b[l])[:, None, :]
        sh_a, sc_a, g_a, sh_m, sc_m, g_m = jnp.split(mod, 6, axis=-1)
        h = rms_norm(x, norm_g[l, 0]) * (1.0 + sc_a) + sh_a
        if l % 2 == 0:
            o = fox_sb_mixer(h, even_w_in[l // 2], even_b_forget[l // 2])
        else:
            o = dsa_mixer(h, odd_w_in[l // 2], positions)
        x = x + g_a * rms_norm(o @ mix_w_out[l], norm_g[l, 1])
        h = rms_norm(x, norm_g[l, 2]) * (1.0 + sc_m) + sh_m
        y = jnp.square(jax.nn.relu(h @ ff_w1[l])) @ ff_w2[l]
        x = x + g_m * rms_norm(y, norm_g[l, 3])
    return x
```

```python
import contextlib
import numpy as np
import concourse.bass as bass
import concourse.mybir as mybir
from concourse.bass_utils import run_bass_kernel_spmd

ACT = mybir.ActivationFunctionType
ALU = mybir.AluOpType
AX = mybir.AxisListType
F32, BF16, I32 = mybir.dt.float32, mybir.dt.bfloat16, mybir.dt.int32

S, D, DFF, HD = 4096, 2048, 8192, 128
NT = S // 128
KC = D // 128
EVEN_W, ODD_W = 6152, 3408
EPS = 1e-6
NEG = -1.0e30
SB_WIN = 3
NSLOT = 8

C_ID, C_TRI, C_TRS, C_LOW, C_ONE, C_INV = 0, 128, 256, 384, 512, 640
C_W = 704


def make_consts():
    c = np.zeros((128, C_W), np.float32)
    a = np.arange(128)
    c[:, C_ID:C_ID + 128] = np.eye(128)
    c[:, C_TRI:C_TRI + 128] = (a[:, None] <= a[None, :])
    c[:, C_TRS:C_TRS + 128] = (a[:, None] < a[None, :])
    c[:, C_LOW:C_LOW + 128] = (a[:, None] >= a[None, :])
    c[:, C_ONE:C_ONE + 128] = 1.0
    inv = (10000.0 ** (-np.arange(64, dtype=np.float32) / 64)).astype(np.float32)
    c[:, C_INV:C_INV + 64] = inv[None, :]
    return c


_uid = [0]


def _sbt(nc, name, shape, dty):
    _uid[0] += 1
    return nc.sbuf_tensor(f"{name}_u{_uid[0]}", shape, dty)


class Res:
    __slots__ = ("w", "r", "x")

    def __init__(self, x=False):
        self.w = None
        self.r = {}
        self.x = x


class Eng:
    def __init__(self, name, obj, sem, is_pe=False):
        self.name, self.obj, self.sem, self.is_pe = name, obj, sem, is_pe
        self.n = 0
        self.waited = {}
        self.dma_sems, self.dma_cnt, self.dma_i = [], [], 0


class KB:
    def __init__(self, nc):
        self.nc = nc
        self.stack = contextlib.ExitStack()
        mk = lambda nm: self.stack.enter_context(nc.semaphore(nm))
        self.pe = Eng("pe", nc.tensor, mk("s_pe"), True)
        self.act = Eng("act", nc.scalar, mk("s_act"))
        self.dve = Eng("dve", nc.vector, mk("s_dve"))
        self.pool = Eng("pool", nc.gpsimd, mk("s_pool"))
        self.sp = Eng("sp", nc.sync, mk("s_sp"))
        self.engs = [self.pe, self.act, self.dve, self.pool, self.sp]
        self.queues = [self.sp, self.pool, self.act]
        for q in self.queues:
            q.dma_sems = [mk(f"d_{q.name}{i}") for i in range(NSLOT)]
            q.dma_cnt = [0] * NSLOT
        self.n_ins = 0

    def _wait(self, eng, tk):
        sem, val, src = tk
        if src is eng and eng.is_pe:
            return
        key = sem.name
        if eng.waited.get(key, 0) >= val:
            return
        eng.obj.wait_ge(sem, val)
        eng.waited[key] = val

    def _deps(self, eng, reads, writes):
        for r in reads:
            if r.w is not None:
                self._wait(eng, r.w)
            if r.x:
                for k, tk in r.r.items():
                    if k != eng.name:
                        self._wait(eng, tk)
        for w in writes:
            if w.w is not None:
                self._wait(eng, w.w)
            for tk in w.r.values():
                self._wait(eng, tk)

    def _mark(self, key, tk, reads, writes):
        for r in reads:
            r.r[key] = tk
        for w in writes:
            w.w = tk
            w.r = {}

    def op(self, eng, fn, reads=(), writes=()):
        self._deps(eng, reads, writes)
        ins = fn()
        eng.n += 1
        ins.then_inc(eng.sem, 1)
        tk = (eng.sem, eng.n, eng)
        self._mark(eng.name, tk, reads, writes)
        self.n_ins += 1
        return tk

    def dma(self, q, out, in_, reads=(), writes=()):
        slot = q.dma_i % NSLOT
        q.dma_i += 1
        sem = q.dma_sems[slot]
        if q.dma_cnt[slot] > 0:
            self._wait(q, (sem, 16 * q.dma_cnt[slot], None))
        self._deps(q, reads, writes)
        q.obj.dma_start(out=out, in_=in_).then_inc(sem, 16)
        q.dma_cnt[slot] += 1
        tk = (sem, 16 * q.dma_cnt[slot], None)
        self._mark(sem.name, tk, reads, writes)
        self.n_ins += 1
        return tk

    def barrier(self):
        for e in self.engs:
            for f in self.engs:
                if f is not e and f.n > 0:
                    self._wait(e, (f.sem, f.n, f))
            for q in self.queues:
                for i in range(NSLOT):
                    if q.dma_cnt[i] > 0:
                        self._wait(e, (q.dma_sems[i], 16 * q.dma_cnt[i], None))


class Ring:
    def __init__(self, ctx, nc, name, n, shape, dtype):
        self.t = [ctx.enter_context(_sbt(nc, f"{name}{i}", shape, dtype)) for i in range(n)]
        self.r = [Res() for _ in range(n)]
        self.i = 0

    def next(self):
        k = self.i % len(self.t)
        self.i += 1
        return self.t[k], self.r[k]


class Prog:
    def __init__(self, stages=None, xsrc_is_out=False):
        self.stages = stages
        nc = self.nc = bass.Bass("TRN2", target_bir_lowering=False)
        kb = self.kb = KB(nc)
        import os
        dbg = set(os.environ.get("DEBUG_OUT", "").split(","))
        dt = lambda name, shape, dty, kind: nc.dram_tensor(name, shape, dty, kind=("ExternalOutput" if name in dbg else kind)).ap()
        I, O, N = "ExternalInput", "ExternalOutput", "Internal"
        self.x = dt("x", [S, D], F32, I)
        self.c = dt("c", [16, 128], F32, I)
        self.pos = dt("pos", [32, 128], I32, I)
        self.ada_w = dt("ada_w", [2, D, 6 * D], F32, I)
        self.ada_b = dt("ada_b", [2, 6 * D], F32, I)
        self.norm_g = dt("norm_g", [2, 4, D], F32, I)
        self.mix_w_out = dt("mix_w_out", [2, D, D], F32, I)
        self.even_w_in = dt("even_w_in", [D, EVEN_W], F32, I)
        self.even_b = dt("even_b", [1, 8], F32, I)
        self.odd_w_in = dt("odd_w_in", [D, ODD_W], F32, I)
        self.ff_w1 = dt("ff_w1", [2, D, DFF], F32, I)
        self.ff_w2 = dt("ff_w2", [2, DFF, D], F32, I)
        self.cst = dt("cst", [128, C_W], F32, I)
        self.out = dt("out", [S, D], F32, O)
        self.wb_in0 = dt("wb_in0", [D, EVEN_W], BF16, N)
        self.wb_in1 = dt("wb_in1", [D, ODD_W], BF16, N)
        self.wb_out = [dt(f"wb_out{l}", [D, D], BF16, N) for l in range(2)]
        self.wb_f1 = [dt(f"wb_f1{l}", [D, DFF], BF16, N) for l in range(2)]
        self.wb_f2 = [dt(f"wb_f2{l}", [DFF, D], BF16, N) for l in range(2)]
        self.modd = dt("modd", [2, 6 * D], F32, N)
        self.hT = dt("hT", [KC, 128, S], BF16, N)
        self.oT = dt("oT", [KC, 128, S], BF16, N)
        self.qkT = dt("qkT", [32, 128, S], BF16, N)
        self.vt = dt("vt", [16, 128, NT, 128], BF16, N)
        self.qT1 = dt("qT1", [16, 128, S], BF16, N)
        self.qiT1 = dt("qiT1", [8, 128, S], BF16, N)
        self.dbg = dt("dbg", [128, 512], F32, N)
        self.kT1d = dt("kT1d", [128, S], BF16, N)
        self.kiT2d = dt("kiT2d", [128, S], BF16, N)
        self.v1d = dt("v1d", [128, NT, 128], BF16, N)
        self.wd = dt("wd", [128, NT, 16], F32, N)
        self.ps = [nc.alloc_psum_tensor(f"ps{b}", [128, 512], F32) for b in range(8)]
        self.pr = [Res(True) for _ in range(8)]
        A = nc.alloc_sbuf_tensor
        self.cst_sb = A("cst_sb", [128, C_W], F32)
        self.cb = A("cb", [128, 640], BF16)
        self.eps_t = A("eps_t", [128, 1], F32)
        self.one_t = A("one_t", [128, 1], F32)
        self.zero_t = A("zero_t", [128, 1], F32)
        self.acol = A("acol", [128, 16], F32)
        self.bcol = A("bcol", [128, 16], F32)
        self.G = A("G", [128, D], F32)
        self.Pcol = A("Pcol", [128, 256], F32)
        self.Pb = A("Pb", [128, 256], F32)
        self.r_cst, self.r_cols, self.r_G, self.r_P = Res(), Res(), Res(), Res()
        self.build()

    def ident_f(self, n=128):
        return self.cst_sb[0:n, C_ID:C_ID + n]

    def ident_b(self):
        return self.cb[:, C_ID:C_ID + 128]

    def on(self, name):
        return self.stages is None or name in self.stages

    def build(self):
        kb, nc = self.kb, self.nc
        kb.dma(kb.sp, self.cst_sb[:], self.cst[:, :], writes=[self.r_cst])
        kb.op(kb.dve, lambda: nc.vector.tensor_copy(self.cb[:], self.cst_sb[:, 0:640]), reads=[self.r_cst], writes=[self.r_cst])
        kb.op(kb.dve, lambda: nc.vector.memset(self.eps_t[:], EPS), writes=[self.r_cst])
        kb.op(kb.dve, lambda: nc.vector.memset(self.one_t[:], 1.0), writes=[self.r_cst])
        kb.op(kb.dve, lambda: nc.vector.memset(self.zero_t[:], 0.0), writes=[self.r_cst])
        kb.barrier()
        if self.on("cast"):
            self.stage_cast()
            kb.barrier()
        if self.on("ada"):
            self.stage_ada()
            kb.barrier()
        xsrc = self.x
        for l in range(2):
            if self.on(f"mix{l}"):
                self.stage_normT(l, "a", xsrc)
                kb.barrier()
                if l == 0:
                    self.stage_inproj0()
                    kb.barrier()
                    self.stage_attn0()
                    kb.barrier()
                else:
                    self.stage_inproj1()
                    kb.barrier()
                    self.stage_dsa()
                    kb.barrier()
                self.stage_outproj(l, xsrc)
                kb.barrier()
                xsrc = self.out
            if self.on(f"ffn{l}"):
                self.stage_normT(l, "m", xsrc)
                kb.barrier()
                if self.stages is None or "skipffn" not in self.stages:
                    self.stage_ffn(l, xsrc)
                    kb.barrier()
                    xsrc = self.out
        kb.barrier()

    def stage_cast(self):
        kb, nc = self.kb, self.nc
        jobs = [(self.even_w_in, self.wb_in0)]
        for l in range(2):
            pass
        jobs += [(self.mix_w_out[0], self.wb_out[0]), (self.ff_w1[0], self.wb_f1[0]), (self.ff_w2[0], self.wb_f2[0]),
                 (self.odd_w_in, self.wb_in1), (self.mix_w_out[1], self.wb_out[1]), (self.ff_w1[1], self.wb_f1[1]),
                 (self.ff_w2[1], self.wb_f2[1])]
        CW = 2048
        with contextlib.ExitStack() as ctx:
            rf = Ring(ctx, nc, "cf", 4, [128, CW], F32)
            rb = Ring(ctx, nc, "cbf", 4, [128, CW], BF16)
            k = 0
            for src, dst in jobs:
                R, C = src.shape
                for rbk in range(R // 128):
                    for c0 in range(0, C, CW):
                        cw = min(CW, C - c0)
                        tf, rfr = rf.next()
                        tb, rbr = rb.next()
                        kb.dma(kb.sp, tf[:, 0:cw], src[rbk * 128:(rbk + 1) * 128, c0:c0 + cw], writes=[rfr])
                        if k % 2 == 0:
                            kb.op(kb.act, lambda: nc.scalar.copy(tb[:, 0:cw], tf[:, 0:cw]), reads=[rfr], writes=[rbr])
                        else:
                            kb.op(kb.dve, lambda: nc.vector.tensor_copy(tb[:, 0:cw], tf[:, 0:cw]), reads=[rfr], writes=[rbr])
                        kb.dma(kb.pool, dst[rbk * 128:(rbk + 1) * 128, c0:c0 + cw], tb[:, 0:cw], reads=[rbr])
                        k += 1

    def stage_ada(self):
        kb, nc = self.kb, self.nc
        with contextlib.ExitStack() as ctx:
            T = lambda name, shape, dty: ctx.enter_context(_sbt(nc, name, shape, dty))
            c16, sg16, csT = T("c16", [16, 128], F32), T("sg16", [16, 128], F32), T("csT", [128, 16], F32)
            brow, mrow = T("brow", [1, 6 * D], F32), T("mrow", [1, 6 * D], F32)
            r_c, r_b, r_m = Res(), Res(), Res()
            wr = Ring(ctx, nc, "adaw", 3, [128, 3072], F32)
            kb.dma(kb.sp, c16[:], self.c[:, :], writes=[r_c])
            kb.op(kb.act, lambda: nc.scalar.activation(out=sg16[:], in_=c16[:], func=ACT.Sigmoid), reads=[r_c], writes=[r_c])
            kb.op(kb.dve, lambda: nc.vector.tensor_mul(c16[:], c16[:], sg16[:]), reads=[r_c], writes=[r_c])
            kb.op(kb.pe, lambda: nc.tensor.transpose(self.ps[0][:, 0:16], c16[:], self.ident_f(16)), reads=[r_c, self.r_cst], writes=[self.pr[0]])
            kb.op(kb.dve, lambda: nc.vector.tensor_copy(csT[:], self.ps[0][:, 0:16]), reads=[self.pr[0]], writes=[r_c])
            for l in range(2):
                kb.dma(kb.sp, brow[:], self.ada_b[l:l + 1, :], writes=[r_b])
                for g in range(4):
                    for kc in range(KC):
                        wt, wres = wr.next()
                        kb.dma(kb.sp, wt[:], self.ada_w[l, kc * 128:(kc + 1) * 128, g * 3072:(g + 1) * 3072], writes=[wres])
                        for b in range(6):
                            kb.op(kb.pe, lambda b=b: nc.tensor.matmul(self.ps[b][0:1, :], lhsT=csT[:, kc:kc + 1], rhs=wt[:, b * 512:(b + 1) * 512],
                                                                   start=(kc == 0), stop=(kc == KC - 1)),
                                  reads=[r_c, wres], writes=[self.pr[b]])
                    for b in range(6):
                        c0 = g * 3072 + b * 512
                        kb.op(kb.dve, lambda b=b, c0=c0: nc.vector.tensor_tensor(out=mrow[0:1, c0:c0 + 512], in0=self.ps[b][0:1, :], in1=brow[0:1, c0:c0 + 512], op=ALU.add),
                              reads=[self.pr[b], r_b], writes=[r_m])
                kb.dma(kb.pool, self.modd[l:l + 1, :], mrow[:], reads=[r_m])

    def prep_cols(self, ctx, l, which):
        kb, nc = self.kb, self.nc
        off = 0 if which == "a" else 3
        gi = 0 if which == "a" else 2
        T = lambda name, shape, dty: ctx.enter_context(_sbt(nc, name, shape, dty))
        sc16, sh16, gm16 = T("sc16", [16, 128], F32), T("sh16", [16, 128], F32), T("gm16", [16, 128], F32)
        r = Res()
        v16 = lambda ap: ap.rearrange("(c p) -> c p", p=128)
        kb.dma(kb.sp, sh16[:], v16(self.modd[l, off * D:(off + 1) * D]), writes=[r])
        kb.dma(kb.sp, sc16[:], v16(self.modd[l, (off + 1) * D:(off + 2) * D]), writes=[r])
        kb.dma(kb.sp, gm16[:], v16(self.norm_g[l, gi, :]), writes=[r])
        kb.op(kb.dve, lambda: nc.vector.scalar_tensor_tensor(out=sc16[:], in0=sc16[:], scalar=1.0, in1=gm16[:], op0=ALU.add, op1=ALU.mult), reads=[r], writes=[r])
        kb.op(kb.pe, lambda: nc.tensor.transpose(self.ps[0][:, 0:16], sc16[:], self.ident_f(16)), reads=[r, self.r_cst], writes=[self.pr[0]])
        kb.op(kb.pe, lambda: nc.tensor.transpose(self.ps[1][:, 0:16], sh16[:], self.ident_f(16)), reads=[r, self.r_cst], writes=[self.pr[1]])
        kb.op(kb.dve, lambda: nc.vector.tensor_copy(self.acol[:], self.ps[0][:, 0:16]), reads=[self.pr[0]], writes=[self.r_cols])
        kb.op(kb.dve, lambda: nc.vector.tensor_copy(self.bcol[:], self.ps[1][:, 0:16]), reads=[self.pr[1]], writes=[self.r_cols])

    def prep_G(self, ctx, l, which):
        kb, nc = self.kb, self.nc
        off = 2 if which == "a" else 5
        gi = 1 if which == "a" else 3
        T = lambda name, shape, dty: ctx.enter_context(_sbt(nc, name, shape, dty))
        grow, gmrow = T("grow", [1, D], F32), T("gmrow", [1, D], F32)
        r = Res()
        kb.dma(kb.sp, grow[:], self.modd[l:l + 1, off * D:(off + 1) * D], writes=[r])
        kb.dma(kb.sp, gmrow[:], self.norm_g[l, gi:gi + 1, :], writes=[r])
        kb.op(kb.dve, lambda: nc.vector.tensor_mul(grow[:], grow[:], gmrow[:]), reads=[r], writes=[r])
        for n in range(4):
            kb.op(kb.pe, lambda n=n: nc.tensor.matmul(self.ps[n][:, :], lhsT=self.cst_sb[0:1, C_ONE:C_ONE + 128], rhs=grow[0:1, n * 512:(n + 1) * 512], start=True, stop=True),
                  reads=[r, self.r_cst], writes=[self.pr[n]])
            kb.op(kb.dve, lambda n=n: nc.vector.tensor_copy(self.G[:, n * 512:(n + 1) * 512], self.ps[n][:, :]), reads=[self.pr[n]], writes=[self.r_G])

    def stage_normT(self, l, which, xsrc):
        kb, nc = self.kb, self.nc
        with contextlib.ExitStack() as ctx:
            T = lambda name, shape, dty: ctx.enter_context(_sbt(nc, name, shape, dty))
            with contextlib.ExitStack() as c2:
                self.prep_cols(c2, l, which)
            kb.barrier()
            rx = Ring(ctx, nc, "nx", 3, [128, D], F32)
            rxn = Ring(ctx, nc, "nxn", 2, [128, D], BF16)
            rst = Ring(ctx, nc, "nst", 4, [128, 4], F32)
            rh = Ring(ctx, nc, "nh", 2, [128, KC, 512], BF16)
            junk = T("njunk", [128, D], BF16)
            rj = Res()
            pbank = 0
            for t4 in range(NT // 4):
                hts, hres = rh.next()
                for tt in range(4):
                    t = t4 * 4 + tt
                    xt, xr = rx.next()
                    xn, xnr = rxn.next()
                    st, sr = rst.next()
                    kb.dma(kb.sp, xt[:], xsrc[t * 128:(t + 1) * 128, :], writes=[xr])
                    kb.op(kb.act, lambda: nc.scalar.activation(out=junk[:], in_=xt[:], func=ACT.Square, accum_out=st[:, 0:1]), reads=[xr], writes=[rj, sr])
                    kb.op(kb.act, lambda: nc.scalar.activation(out=st[:, 1:2], in_=st[:, 0:1], func=ACT.Sqrt, bias=self.eps_t[:], scale=1.0 / D), reads=[sr], writes=[sr])
                    kb.op(kb.dve, lambda: nc.vector.reciprocal(st[:, 2:3], st[:, 1:2]), reads=[sr], writes=[sr])
                    kb.op(kb.dve, lambda: nc.vector.tensor_scalar(out=xn[:], in0=xt[:], scalar1=st[:, 2:3], scalar2=None, op0=ALU.mult), reads=[xr, sr], writes=[xnr])
                    for half in range(2):
                        b = pbank % 4
                        pbank += 1
                        pv = self.ps[b][:].bitcast(BF16)
                        for k8 in range(8):
                            kc = half * 8 + k8
                            kb.op(kb.pe, lambda kc=kc, k8=k8, pv=pv: nc.tensor.transpose(pv[:, k8 * 128:(k8 + 1) * 128], xn[:, kc * 128:(kc + 1) * 128], self.ident_b()),
                                  reads=[xnr, self.r_cst], writes=[self.pr[b]])
                        for k8 in range(8):
                            kc = half * 8 + k8
                            dst = hts[:, kc, tt * 128:(tt + 1) * 128]
                            src = pv[:, k8 * 128:(k8 + 1) * 128]
                            if k8 % 2 == 0:
                                kb.op(kb.act, lambda dst=dst, src=src, kc=kc: nc.scalar.activation(out=dst, in_=src, func=ACT.Identity, bias=self.bcol[:, kc:kc + 1], scale=self.acol[:, kc:kc + 1]),
                                      reads=[self.pr[b], self.r_cols], writes=[hres])
                            else:
                                kb.op(kb.dve, lambda dst=dst, src=src, kc=kc: nc.vector.tensor_scalar(out=dst, in0=src, scalar1=self.acol[:, kc:kc + 1], scalar2=self.bcol[:, kc:kc + 1], op0=ALU.mult, op1=ALU.add),
                                      reads=[self.pr[b], self.r_cols], writes=[hres])
                kb.dma(kb.pool, self.hT.rearrange("k p t -> p k t")[:, :, t4 * 512:(t4 + 1) * 512], hts[:], reads=[hres])

    def rstd_from_ss(self, st, sr, ncols):
        kb, nc = self.kb, self.nc
        kb.op(kb.dve, lambda: nc.vector.tensor_reduce(out=st[:, 4:5], in_=st[:, 0:ncols], axis=AX.X, op=ALU.add), reads=[sr], writes=[sr])
        kb.op(kb.act, lambda: nc.scalar.activation(out=st[:, 5:6], in_=st[:, 4:5], func=ACT.Sqrt, bias=self.eps_t[:], scale=1.0 / D), reads=[sr], writes=[sr])
        kb.op(kb.dve, lambda: nc.vector.reciprocal(st[:, 6:7], st[:, 5:6]), reads=[sr], writes=[sr])

    def stage_outproj(self, l, xsrc):
        kb, nc = self.kb, self.nc
        with contextlib.ExitStack() as ctx:
            T = lambda name, shape, dty: ctx.enter_context(_sbt(nc, name, shape, dty))
            with contextlib.ExitStack() as c2:
                self.prep_G(c2, l, "a")
            kb.barrier()
            wo = T("wo", [128, KC, D], BF16)
            r_wo = Res()
            wv = self.wb_out[l].rearrange("(k p) c -> p k c", p=128)
            for n in range(4):
                kb.dma(kb.sp, wo[:, :, n * 512:(n + 1) * 512], wv[:, :, n * 512:(n + 1) * 512], writes=[r_wo])
            ro = Ring(ctx, nc, "oo", 2, [128, KC, 512], BF16)
            rx = Ring(ctx, nc, "ox", 2, [128, D], F32)
            rt1 = Ring(ctx, nc, "ot1", 2, [128, D], F32)
            rst = Ring(ctx, nc, "ost", 4, [128, 8], F32)
            junk = T("ojunk", [128, 512], BF16)
            rj = Res()
            for t4 in range(NT // 4):
                ot, ores = ro.next()
                kb.dma(kb.sp, ot[:], self.oT.rearrange("k p t -> p k t")[:, :, t4 * 512:(t4 + 1) * 512], writes=[ores])
                for tt in range(4):
                    t = t4 * 4 + tt
                    xt, xr = rx.next()
                    t1, t1r = rt1.next()
                    st, sr = rst.next()
                    kb.dma(kb.sp, xt[:], xsrc[t * 128:(t + 1) * 128, :], writes=[xr])
                    base = (t % 2) * 4
                    for n in range(4):
                        b = base + n
                        for kc in range(KC):
                            kb.op(kb.pe, lambda kc=kc, b=b, n=n: nc.tensor.matmul(self.ps[b][:, :], lhsT=ot[:, kc, tt * 128:(tt + 1) * 128], rhs=wo[:, kc, n * 512:(n + 1) * 512],
                                                                             start=(kc == 0), stop=(kc == KC - 1)),
                                  reads=[ores, r_wo], writes=[self.pr[b]])
                        kb.op(kb.act, lambda b=b, n=n: nc.scalar.activation(out=junk[:], in_=self.ps[b][:, :], func=ACT.Square, accum_out=st[:, n:n + 1]), reads=[self.pr[b]], writes=[rj, sr])
                        kb.op(kb.dve, lambda b=b, n=n: nc.vector.tensor_tensor(out=t1[:, n * 512:(n + 1) * 512], in0=self.ps[b][:, :], in1=self.G[:, n * 512:(n + 1) * 512], op=ALU.mult),
                              reads=[self.pr[b], self.r_G], writes=[t1r])
                    self.rstd_from_ss(st, sr, 4)
                    kb.op(kb.dve, lambda: nc.vector.scalar_tensor_tensor(out=t1[:], in0=t1[:], scalar=st[:, 6:7], in1=xt[:], op0=ALU.mult, op1=ALU.add), reads=[t1r, sr, xr], writes=[t1r])
                    kb.dma(kb.pool, self.out[t * 128:(t + 1) * 128, :], t1[:], reads=[t1r])

    def stage_ffn(self, l, xsrc):
        kb, nc = self.kb, self.nc
        with contextlib.ExitStack() as ctx:
            T = lambda name, shape, dty: ctx.enter_context(_sbt(nc, name, shape, dty))
            with contextlib.ExitStack() as c2:
                self.prep_G(c2, l, "m")
            kb.barrier()
            rh = Ring(ctx, nc, "fh", 1, [128, KC, 512], BF16)
            uT = T("uT", [128, 64, 512], BF16)
            r_u = [Res() for _ in range(64)]
            rw1 = Ring(ctx, nc, "fw1", 3, [128, KC, 256], BF16)
            rw2 = Ring(ctx, nc, "fw2", 3, [128, 8, 512], BF16)
            rtmp = Ring(ctx, nc, "ftmp", 3, [128, 512], F32)
            ysb = T("ysb", [128, 4, D], F32)
            r_y = [Res() for _ in range(4)]
            rx = Ring(ctx, nc, "fx", 2, [128, D], F32)
            rst = Ring(ctx, nc, "fst", 8, [128, 8], F32)
            junk = T("fjunk", [128, 512], BF16)
            rj = Res()
            w1v = self.wb_f1[l].rearrange("(k p) c -> p k c", p=128)
            w2v = self.wb_f2[l].rearrange("(f p) c -> p f c", p=128)
            pb1 = 0
            import os
            for t4 in range(int(os.environ.get("FFN_BLOCKS", NT // 4))):
                ht, hres = rh.next()
                kb.dma(kb.sp, ht[:], self.hT.rearrange("k p t -> p k t")[:, :, t4 * 512:(t4 + 1) * 512], writes=[hres])
                for fb in range(32):
                    w1, w1r = rw1.next()
                    kb.dma(kb.sp, w1[:], w1v[:, :, fb * 256:(fb + 1) * 256], writes=[w1r])
                    for fi in range(2):
                        f = fb * 2 + fi
                        b = pb1 % 4
                        pb1 += 1
                        for kc in range(KC):
                            kb.op(kb.pe, lambda kc=kc, b=b, fi=fi: nc.tensor.matmul(self.ps[b][:, :], lhsT=w1[:, kc, fi * 128:(fi + 1) * 128], rhs=ht[:, kc, :],
                                                                               start=(kc == 0), stop=(kc == KC - 1)),
                                  reads=[w1r, hres], writes=[self.pr[b]])
                        tmp, tr = rtmp.next()
                        kb.op(kb.act, lambda b=b: nc.scalar.activation(out=tmp[:], in_=self.ps[b][:, :], func=ACT.Relu), reads=[self.pr[b]], writes=[tr])
                        kb.op(kb.dve, lambda f=f: nc.vector.tensor_tensor(out=uT[:, f, :], in0=tmp[:], in1=tmp[:], op=ALU.mult), reads=[tr], writes=[r_u[f]])
                if os.environ.get("FFN_PHASE") == "1":
                    continue
                sts = [rst.next() for _ in range(4)]
                for n in range(4):
                    for fg in range(8):
                        w2, w2r = rw2.next()
                        kb.dma(kb.sp, w2[:], w2v[:, fg * 8:(fg + 1) * 8, n * 512:(n + 1) * 512], writes=[w2r])
                        for j in range(8):
                            f = fg * 8 + j
                            for tt in range(4):
                                b = 4 + tt
                                kb.op(kb.pe, lambda f=f, j=j, tt=tt, b=b: nc.tensor.matmul(self.ps[b][:, :], lhsT=uT[:, f, tt * 128:(tt + 1) * 128], rhs=w2[:, j, :],
                                                                                       start=(f == 0), stop=(f == 63)),
                                      reads=[r_u[f], w2r], writes=[self.pr[b]])
                    for tt in range(4):
                        b = 4 + tt
                        st, sr = sts[tt]
                        if os.environ.get("FFN_NOEV") == "1":
                            continue
                        if os.environ.get("FFN_NOEV") != "2":
                            kb.op(kb.act, lambda b=b, st=st: nc.scalar.activation(out=junk[:], in_=self.ps[b][:, :], func=ACT.Square, accum_out=st[:, n:n + 1]), reads=[self.pr[b]], writes=[rj, sr])
                        kb.op(kb.dve, lambda b=b, tt=tt: nc.vector.tensor_tensor(out=ysb[:, tt, n * 512:(n + 1) * 512], in0=self.ps[b][:, :], in1=self.G[:, n * 512:(n + 1) * 512], op=ALU.mult),
                              reads=[self.pr[b], self.r_G], writes=[r_y[tt]])
                if os.environ.get("FFN_PHASE") == "2":
                    continue
                for tt in range(4):
                    t = t4 * 4 + tt
                    st, sr = sts[tt]
                    xt, xr = rx.next()
                    kb.dma(kb.sp, xt[:], xsrc[t * 128:(t + 1) * 128, :], writes=[xr])
                    self.rstd_from_ss(st, sr, 4)
                    kb.op(kb.dve, lambda tt=tt, st=st: nc.vector.scalar_tensor_tensor(out=xt[:], in0=ysb[:, tt, :], scalar=st[:, 6:7], in1=xt[:], op0=ALU.mult, op1=ALU.add),
                          reads=[r_y[tt], sr, xr], writes=[xr])
                    kb.dma(kb.pool, self.out[t * 128:(t + 1) * 128, :], xt[:], reads=[xr])

    def stage_inproj0(self):
        kb, nc = self.kb, self.nc
        hTv = self.hT.rearrange("k p t -> p k t")
        wv = self.wb_in0.rearrange("(k p) c -> p k c", p=128)
        qk_blocks = [(0, 0, 0), (512, 4, 0), (1024, 0, 1), (1536, 4, 1), (3080, 8, 0), (3592, 12, 0), (4104, 8, 1), (4616, 12, 1)]
        v_blocks = [(2048, 0), (2560, 4), (5128, 8), (5640, 12)]
        with contextlib.ExitStack() as ctx:
            T = lambda name, shape, dty: ctx.enter_context(_sbt(nc, name, shape, dty))
            rh = Ring(ctx, nc, "ih", 2, [128, KC, 512], BF16)
            rw = Ring(ctx, nc, "iw", 3, [128, KC, 512], BF16)
            rs = Ring(ctx, nc, "is", 4, [128, 512], BF16)
            wg = T("iwg", [128, KC, 128], BF16)
            nlf = T("nlf", [8, S], F32)
            brow, nb = T("ibrow", [1, 8], F32), T("inb", [8, 1], F32)
            etmp = T("ietmp", [8, 512], F32)
            r_wg, r_nlf, r_b, r_e = Res(), Res(), Res(), Res()
            kb.dma(kb.sp, wg[:], wv[:, :, 3008:3136], writes=[r_wg])
            kb.dma(kb.sp, brow[:], self.even_b[:, :], writes=[r_b])
            kb.op(kb.pe, lambda: nc.tensor.matmul(self.ps[7][0:8, 0:1], lhsT=brow[0:1, 0:8], rhs=self.cst_sb[0:1, C_ONE:C_ONE + 1], start=True, stop=True),
                  reads=[r_b, self.r_cst], writes=[self.pr[7]])
            kb.op(kb.dve, lambda: nc.vector.tensor_scalar(out=nb[:], in0=self.ps[7][0:8, 0:1], scalar1=-1.0, scalar2=None, op0=ALU.mult), reads=[self.pr[7]], writes=[r_b])
            pb = 0
            ev = 0
            for t4 in range(NT // 4):
                ht, hres = rh.next()
                kb.dma(kb.sp, ht[:], hTv[:, :, t4 * 512:(t4 + 1) * 512], writes=[hres])
                b = pb % 7
                pb += 1
                for kc in range(KC):
                    kb.op(kb.pe, lambda: nc.tensor.matmul(self.ps[b][0:8, :], lhsT=wg[:, kc, 64:72], rhs=ht[:, kc, :], start=(kc == 0), stop=(kc == KC - 1)),
                          reads=[r_wg, hres], writes=[self.pr[b]])
                kb.op(kb.act, lambda: nc.scalar.activation(out=etmp[:], in_=self.ps[b][0:8, :], func=ACT.Exp, bias=nb[:], scale=-1.0), reads=[self.pr[b], r_b], writes=[r_e])
                kb.op(kb.act, lambda: nc.scalar.activation(out=nlf[:, t4 * 512:(t4 + 1) * 512], in_=etmp[:], func=ACT.Ln, bias=self.one_t[0:8, :], scale=1.0), reads=[r_e], writes=[r_nlf])
                for (c0, h0, isk) in qk_blocks:
                    w, wr = rw.next()
                    kb.dma(kb.sp, w[:], wv[:, :, c0:c0 + 512], writes=[wr])
                    for hi in range(4):
                        b = pb % 7
                        pb += 1
                        for kc in range(KC):
                            kb.op(kb.pe, lambda: nc.tensor.matmul(self.ps[b][:, :], lhsT=w[:, kc, hi * 128:(hi + 1) * 128], rhs=ht[:, kc, :], start=(kc == 0), stop=(kc == KC - 1)),
                                  reads=[wr, hres], writes=[self.pr[b]])
                        stg, sr = rs.next()
                        if ev % 2 == 0:
                            kb.op(kb.act, lambda: nc.scalar.copy(stg[:], self.ps[b][:, :]), reads=[self.pr[b]], writes=[sr])
                        else:
                            kb.op(kb.dve, lambda: nc.vector.tensor_copy(stg[:], self.ps[b][:, :]), reads=[self.pr[b]], writes=[sr])
                        ev += 1
                        kb.dma(kb.pool, self.qkT[2 * (h0 + hi) + isk, :, t4 * 512:(t4 + 1) * 512], stg[:], reads=[sr])
                for (c0, h0) in v_blocks:
                    w, wr = rw.next()
                    kb.dma(kb.sp, w[:], wv[:, :, c0:c0 + 512], writes=[wr])
                    for tt in range(4):
                        j = t4 * 4 + tt
                        b = pb % 7
                        pb += 1
                        for kc in range(KC):
                            kb.op(kb.pe, lambda: nc.tensor.matmul(self.ps[b][:, :], lhsT=ht[:, kc, tt * 128:(tt + 1) * 128], rhs=w[:, kc, :], start=(kc == 0), stop=(kc == KC - 1)),
                                  reads=[wr, hres], writes=[self.pr[b]])
                        stg, sr = rs.next()
                        if ev % 2 == 0:
                            kb.op(kb.act, lambda: nc.scalar.copy(stg[:], self.ps[b][:, :]), reads=[self.pr[b]], writes=[sr])
                        else:
                            kb.op(kb.dve, lambda: nc.vector.tensor_copy(stg[:], self.ps[b][:, :]), reads=[self.pr[b]], writes=[sr])
                        ev += 1
                        kb.dma(kb.pool, self.vt[h0:h0 + 4, :, j, :].rearrange("h p d -> p h d"), stg[:].rearrange("p (h d) -> p h d", h=4), reads=[sr])
            Pt = T("iP", [8, S], F32)
            R = T("iR", [8, 8, 32], F32)
            r_P, r_R = Res(), Res()
            kb.op(kb.dve, lambda: nc.vector.tensor_tensor_scan(out=Pt[:], data0=self.one_t[0:8, 0:1].to_broadcast([8, S]), data1=nlf[:], initial=0.0, op0=ALU.mult, op1=ALU.add),
                  reads=[r_nlf], writes=[r_P])
            for j in range(NT):
                kb.op(kb.pe, lambda: nc.tensor.transpose(self.ps[0][:, j * 8:(j + 1) * 8], Pt[0:8, j * 128:(j + 1) * 128], self.ident_f(8)), reads=[r_P, self.r_cst], writes=[self.pr[0]])
            kb.op(kb.dve, lambda: nc.vector.tensor_copy(self.Pcol[:], self.ps[0][:, 0:256]), reads=[self.pr[0]], writes=[self.r_P])
            for h in range(8):
                kb.op(kb.dve, lambda: nc.vector.tensor_scalar(out=R[:, h, :], in0=Pt[0:8, 0:S:128], scalar1=self.cst_sb[0:8, C_ID + h:C_ID + h + 1], scalar2=None, op0=ALU.mult),
                      reads=[r_P, self.r_cst], writes=[r_R])
            kb.op(kb.pe, lambda: nc.tensor.matmul(self.ps[1][:, 0:256], lhsT=self.cst_sb[0:8, C_ONE:C_ONE + 128], rhs=R[:].rearrange("k h i -> k (h i)"), start=True, stop=True),
                  reads=[r_R, self.r_cst], writes=[self.pr[1]])
            kb.op(kb.dve, lambda: nc.vector.tensor_copy(self.Pb[:], self.ps[1][:, 0:256]), reads=[self.pr[1]], writes=[self.r_P])
            kb.dma(kb.pool, self.dbg[:, 0:256], self.Pcol[:], reads=[self.r_P])
            kb.dma(kb.pool, self.dbg[:, 256:512], self.Pb[:], reads=[self.r_P])

    def stage_attn0(self):
        kb, nc = self.kb, self.nc
        scale = HD ** -0.5
        import os
        heads = [int(v) for v in os.environ["ATTN_HEADS"].split(",")] if "ATTN_HEADS" in os.environ else range(16)
        with contextlib.ExitStack() as ctx:
            T = lambda name, shape, dty: ctx.enter_context(_sbt(nc, name, shape, dty))
            rq = Ring(ctx, nc, "aq", 2, [128, S], BF16)
            rk = Ring(ctx, nc, "ak", 2, [128, S], BF16)
            rnk = Ring(ctx, nc, "ank", 1, [128, S], BF16)
            rv = Ring(ctx, nc, "av", 2, [128, NT, 129], BF16)
            roT = Ring(ctx, nc, "aoT", 2, [128, S], BF16)
            rp = Ring(ctx, nc, "ap", 4, [128, 128], BF16)
            rsp = Ring(ctx, nc, "asp", 3, [128, 128], BF16)
            re_ = Ring(ctx, nc, "ae", 3, [128, 128], F32)
            rT = Ring(ctx, nc, "aT", 2, [128, 128], F32)
            rR = Ring(ctx, nc, "aR", 2, [128, 128], F32)
            rbias = Ring(ctx, nc, "ab", 2, [128, 32], F32)
            rosb = Ring(ctx, nc, "aosb", 2, [128, 128], BF16)
            rrd = Ring(ctx, nc, "ard", 2, [128, 1], F32)
            for vt_, vr_ in zip(rv.t, rv.r):
                kb.op(kb.pool, lambda: nc.gpsimd.memset(vt_[:, :, 128:129], 1.0), writes=[vr_])
            tri, trs = self.cb[:, C_TRI:C_TRI + 128], self.cb[:, C_TRS:C_TRS + 128]
            low, ones_b = self.cb[:, C_LOW:C_LOW + 128], self.cb[:, C_ONE:C_ONE + 128]
            Pcol = self.Pcol[:].rearrange("p (j h) -> p j h", h=8)
            Pb = self.Pb[:].rearrange("p (h i) -> p h i", i=32)
            sbank = 0
            for h in heads:
                fox = h < 8
                qT, qr = rq.next()
                kT, kr = rk.next()
                V, vr = rv.next()
                oTs, oTr = roT.next()
                kb.dma(kb.sp, qT[:], self.qkT[2 * h, :, :], writes=[qr])
                kb.dma(kb.sp, kT[:], self.qkT[2 * h + 1, :, :], writes=[kr])
                kb.dma(kb.sp, V[:, :, 0:128], self.vt[h, :, :, :], writes=[vr])
                if not fox:
                    nkT, nkr = rnk.next()
                    kb.op(kb.dve, lambda: nc.vector.tensor_scalar(out=nkT[:], in0=kT[:], scalar1=-scale, scalar2=None, op0=ALU.mult), reads=[kr], writes=[nkr])
                for i in range(NT):
                    bO = 3 + (i % 2)
                    qi_ = qT[:, i * 128:(i + 1) * 128]
                    if fox:
                        bias, br = rbias.next()
                        kb.op(kb.dve, lambda: nc.vector.tensor_scalar(out=bias[:], in0=Pcol[:, :, h], scalar1=Pb[:, h, i:i + 1], scalar2=None, op0=ALU.subtract),
                              reads=[self.r_P], writes=[br])
                        for j in range(i + 1):
                            bS = sbank % 3
                            sbank += 1
                            kb.op(kb.pe, lambda: nc.tensor.matmul(self.ps[bS][:, 0:128], lhsT=kT[:, j * 128:(j + 1) * 128], rhs=qi_, start=True, stop=True),
                                  reads=[kr, qr], writes=[self.pr[bS]])
                            PT, pr_ = rp.next()
                            kb.op(kb.act, lambda: nc.scalar.activation(out=PT[:], in_=self.ps[bS][:, 0:128], func=ACT.Exp, bias=bias[:, j:j + 1], scale=scale),
                                  reads=[self.pr[bS], br], writes=[pr_])
                            if j == i:
                                kb.op(kb.pool, lambda: nc.gpsimd.tensor_tensor(out=PT[:], in0=PT[:], in1=tri, op=ALU.mult), reads=[pr_, self.r_cst], writes=[pr_])
                            kb.op(kb.pe, lambda: nc.tensor.matmul(self.ps[bO][:, 0:129], lhsT=PT[:], rhs=V[:, j, :], start=(j == 0), stop=(j == i)),
                                  reads=[pr_, vr], writes=[self.pr[bO]])
                        rd, rdr = rrd.next()
                        osb, osr = rosb.next()
                        kb.op(kb.dve, lambda: nc.vector.reciprocal(rd[:], self.ps[bO][:, 128:129]), reads=[self.pr[bO]], writes=[rdr])
                        kb.op(kb.act, lambda: nc.scalar.activation(out=osb[:], in_=self.ps[bO][:, 0:128], func=ACT.Copy, scale=rd[:, 0:1]), reads=[self.pr[bO], rdr], writes=[osr])
                    else:
                        js = [j for j in range(i, i - SB_WIN, -1) if j >= 0]
                        Racc, rr = rR.next()
                        for idx, j in enumerate(js):
                            last = idx == len(js) - 1
                            bS = sbank % 3
                            sbank += 1
                            kb.op(kb.pe, lambda: nc.tensor.matmul(self.ps[bS][:, 0:128], lhsT=kT[:, j * 128:(j + 1) * 128], rhs=qi_, start=True, stop=True),
                                  reads=[kr, qr], writes=[self.pr[bS]])
                            e, er = re_.next()
                            SP, spr = rsp.next()
                            kb.op(kb.act, lambda: nc.scalar.activation(out=e[:], in_=self.ps[bS][:, 0:128], func=ACT.Exp, scale=scale), reads=[self.pr[bS]], writes=[er])
                            kb.op(kb.act, lambda: nc.scalar.activation(out=SP[:], in_=e[:], func=ACT.Ln, bias=self.one_t[:], scale=1.0), reads=[er], writes=[spr])
                            if j == i:
                                kb.op(kb.pool, lambda: nc.gpsimd.tensor_tensor(out=SP[:], in0=SP[:], in1=trs, op=ALU.mult), reads=[spr, self.r_cst], writes=[spr])
                            kb.op(kb.pe, lambda: nc.tensor.matmul(self.ps[6][:, 0:128], lhsT=low, rhs=SP[:], start=True, stop=False), reads=[spr, self.r_cst], writes=[self.pr[6]])
                            kb.op(kb.pe, lambda: nc.tensor.matmul(self.ps[6][:, 0:128], lhsT=nkT[:, j * 128:(j + 1) * 128], rhs=qi_, start=False, stop=True),
                                  reads=[nkr, qr], writes=[self.pr[6]])
                            A, ar = rp.next()
                            if idx == 0:
                                kb.op(kb.act, lambda: nc.scalar.activation(out=A[:], in_=self.ps[6][:, 0:128], func=ACT.Exp, scale=-1.0), reads=[self.pr[6]], writes=[ar])
                            else:
                                Tt, tr_ = rT.next()
                                kb.op(kb.dve, lambda: nc.vector.tensor_tensor(out=Tt[:], in0=self.ps[6][:, 0:128], in1=Racc[:], op=ALU.add), reads=[self.pr[6], rr], writes=[tr_])
                                kb.op(kb.act, lambda: nc.scalar.activation(out=A[:], in_=Tt[:], func=ACT.Exp, scale=-1.0), reads=[tr_], writes=[ar])
                            if j == i:
                                kb.op(kb.pool, lambda: nc.gpsimd.tensor_tensor(out=A[:], in0=A[:], in1=trs, op=ALU.mult), reads=[ar, self.r_cst], writes=[ar])
                            kb.op(kb.pe, lambda: nc.tensor.matmul(self.ps[bO][:, 0:128], lhsT=A[:], rhs=V[:, j, 0:128], start=(idx == 0), stop=last),
                                  reads=[ar, vr], writes=[self.pr[bO]])
                            if not last:
                                kb.op(kb.pe, lambda: nc.tensor.matmul(self.ps[7][:, 0:128], lhsT=ones_b, rhs=SP[:], start=True, stop=True), reads=[spr, self.r_cst], writes=[self.pr[7]])
                                if idx == 0:
                                    kb.op(kb.dve, lambda: nc.vector.tensor_copy(Racc[:], self.ps[7][:, 0:128]), reads=[self.pr[7]], writes=[rr])
                                else:
                                    kb.op(kb.dve, lambda: nc.vector.tensor_tensor(out=Racc[:], in0=self.ps[7][:, 0:128], in1=Racc[:], op=ALU.add), reads=[self.pr[7], rr], writes=[rr])
                        osb, osr = rosb.next()
                        kb.op(kb.act, lambda: nc.scalar.copy(osb[:], self.ps[bO][:, 0:128]), reads=[self.pr[bO]], writes=[osr])
                    pv = self.ps[5][:].bitcast(BF16)
                    kb.op(kb.pe, lambda: nc.tensor.transpose(pv[:, 0:128], osb[:], self.ident_b()), reads=[osr, self.r_cst], writes=[self.pr[5]])
                    kb.op(kb.dve, lambda: nc.vector.tensor_copy(oTs[:, i * 128:(i + 1) * 128], pv[:, 0:128]), reads=[self.pr[5]], writes=[oTr])
                kb.dma(kb.pool, self.oT[h, :, :], oTs[:], reads=[oTr])

    def stage_inproj1(self):
        kb, nc = self.kb, self.nc
        hTv = self.hT.rearrange("k p t -> p k t")
        wv = self.wb_in1.rearrange("(k p) c -> p k c", p=128)
        with contextlib.ExitStack() as ctx:
            T = lambda name, shape, dty: ctx.enter_context(_sbt(nc, name, shape, dty))
            sinq, cosq = T("sinq", [128, NT, 64], F32), T("cosq", [128, NT, 64], F32)
            r_tab = Res()
            with contextlib.ExitStack() as c2:
                T2 = lambda name, shape, dty: c2.enter_context(_sbt(nc, name, shape, dty))
                pi32, pf32, post = T2("pi32", [32, 128], I32), T2("pf32", [32, 128], F32), T2("post", [128, 32], F32)
                ang, u, ki, kf, m = (T2("ang", [128, NT * 64], F32), T2("ru", [128, NT * 64], F32), T2("rki", [128, NT * 64], I32),
                                     T2("rkf", [128, NT * 64], F32), T2("rm", [128, NT * 64], F32))
                npi = T2("npi", [128, 1], F32)
                r = Res()
                kb.dma(kb.sp, pi32[:], self.pos[:, :], writes=[r])
                kb.op(kb.dve, lambda: nc.vector.memset(npi[:], -3.14159), writes=[r])
                kb.op(kb.dve, lambda: nc.vector.tensor_copy(pf32[:], pi32[:]), reads=[r], writes=[r])
                kb.op(kb.pe, lambda: nc.tensor.transpose(self.ps[0][:, 0:32], pf32[:], self.ident_f(32)), reads=[r, self.r_cst], writes=[self.pr[0]])
                kb.op(kb.dve, lambda: nc.vector.tensor_copy(post[:], self.ps[0][:, 0:32]), reads=[self.pr[0]], writes=[r])
                for j in range(NT):
                    kb.op(kb.dve, lambda: nc.vector.tensor_scalar(out=ang[:, j * 64:(j + 1) * 64], in0=self.cst_sb[:, C_INV:C_INV + 64], scalar1=post[:, j:j + 1], scalar2=None, op0=ALU.mult),
                          reads=[r, self.r_cst], writes=[r])
                for tab, shift in ((sinq, 0.5), (cosq, 0.75)):
                    V = nc.vector
                    kb.op(kb.dve, lambda: V.tensor_scalar(out=u[:], in0=ang[:], scalar1=1.0 / (2 * np.pi), scalar2=shift, op0=ALU.mult, op1=ALU.add), reads=[r], writes=[r])
                    kb.op(kb.dve, lambda: V.tensor_copy(ki[:], u[:]), reads=[r], writes=[r])
                    kb.op(kb.dve, lambda: V.tensor_copy(kf[:], ki[:]), reads=[r], writes=[r])
                    kb.op(kb.dve, lambda: V.tensor_tensor(out=u[:], in0=u[:], in1=kf[:], op=ALU.subtract), reads=[r], writes=[r])
                    kb.op(kb.dve, lambda: V.tensor_scalar(out=m[:], in0=u[:], scalar1=0.0, scalar2=None, op0=ALU.is_lt), reads=[r], writes=[r])
                    kb.op(kb.dve, lambda: V.tensor_tensor(out=u[:], in0=u[:], in1=m[:], op=ALU.add), reads=[r], writes=[r])
                    kb.op(kb.dve, lambda: V.tensor_scalar(out=m[:], in0=u[:], scalar1=1.0, scalar2=None, op0=ALU.is_ge), reads=[r], writes=[r])
                    kb.op(kb.dve, lambda: V.tensor_tensor(out=u[:], in0=u[:], in1=m[:], op=ALU.subtract), reads=[r], writes=[r])
                    kb.op(kb.act, lambda: nc.scalar.activation(out=tab[:].rearrange("p j k -> p (j k)"), in_=u[:], func=ACT.Sin, bias=npi[:], scale=6.28318), reads=[r], writes=[r_tab])
                kb.barrier()
            rh = Ring(ctx, nc, "jh", 2, [128, KC, 512], BF16)
            rw = Ring(ctx, nc, "jw", 3, [128, KC, 512], BF16)
            qtile = [T(f"jq{tt}", [128, 16, 128], BF16) for tt in range(4)]
            qres = [Res() for _ in range(4)]
            rta = Ring(ctx, nc, "jta", 2, [128, 512], F32)
            rtb = Ring(ctx, nc, "jtb", 2, [128, 512], F32)
            rqT = Ring(ctx, nc, "jqT", 2, [128, 16, 512], BF16)
            rqiT = Ring(ctx, nc, "jqiT", 2, [128, 8, 512], BF16)
            rkT = Ring(ctx, nc, "jkT", 2, [128, 512], BF16)
            rkiT = Ring(ctx, nc, "jkiT", 2, [128, 512], BF16)
            rkr = Ring(ctx, nc, "jkr", 2, [128, 128], BF16)
            rvs = Ring(ctx, nc, "jvs", 2, [128, 128], BF16)
            rws = Ring(ctx, nc, "jws", 2, [128, 16], F32)
            pbank = [0]
            tbank = [0]
            evc = [0]

            def proj(ht, hres, w, wr, tt, ncols):
                b = pbank[0] % 6
                pbank[0] += 1
                for kc in range(KC):
                    kb.op(kb.pe, lambda: nc.tensor.matmul(self.ps[b][:, 0:ncols], lhsT=ht[:, kc, tt * 128:(tt + 1) * 128], rhs=w[:, kc, 0:ncols], start=(kc == 0), stop=(kc == KC - 1)),
                          reads=[wr, hres], writes=[self.pr[b]])
                return b

            def rope(b, c0, nh, half, j, dst, dres):
                x = self.ps[b][:, c0:c0 + nh * 2 * half].rearrange("p (h two d) -> p h two d", h=nh, two=2)
                x1, x2 = x[:, :, 0, :], x[:, :, 1, :]
                st = 64 // half
                cs = cosq[:, j, 0:64:st].unsqueeze(1).to_broadcast([128, nh, half])
                sn = sinq[:, j, 0:64:st].unsqueeze(1).to_broadcast([128, nh, half])
                ta, tar = rta.next()
                tb, tbr = rtb.next()
                av = ta[:, 0:nh * half].rearrange("p (h d) -> p h d", h=nh)
                bv = tb[:, 0:nh * half].rearrange("p (h d) -> p h d", h=nh)
                V = nc.vector
                kb.op(kb.dve, lambda: V.tensor_tensor(out=av, in0=x1, in1=cs, op=ALU.mult), reads=[self.pr[b], r_tab], writes=[tar])
                kb.op(kb.dve, lambda: V.tensor_tensor(out=bv, in0=x2, in1=sn, op=ALU.mult), reads=[self.pr[b], r_tab], writes=[tbr])
                kb.op(kb.pool, lambda: nc.gpsimd.tensor_tensor(out=dst[:, :, 0:half], in0=av, in1=bv, op=ALU.subtract), reads=[tar, tbr], writes=[dres])
                ta, tar = rta.next()
                tb, tbr = rtb.next()
                av = ta[:, 0:nh * half].rearrange("p (h d) -> p h d", h=nh)
                bv = tb[:, 0:nh * half].rearrange("p (h d) -> p h d", h=nh)
                kb.op(kb.dve, lambda: V.tensor_tensor(out=av, in0=x2, in1=cs, op=ALU.mult), reads=[self.pr[b], r_tab], writes=[tar])
                kb.op(kb.dve, lambda: V.tensor_tensor(out=bv, in0=x1, in1=sn, op=ALU.mult), reads=[self.pr[b], r_tab], writes=[tbr])
                kb.op(kb.pool, lambda: nc.gpsimd.tensor_tensor(out=dst[:, :, half:2 * half], in0=av, in1=bv, op=ALU.add), reads=[tar, tbr], writes=[dres])

            def transp(src_list, sres, dst_fn, dres):
                k = 0
                while k < len(src_list):
                    grp = src_list[k:k + 8]
                    b = 6 + tbank[0] % 2
                    tbank[0] += 1
                    pv = self.ps[b][:].bitcast(BF16)
                    for g, src in enumerate(grp):
                        kb.op(kb.pe, lambda: nc.tensor.transpose(pv[:, g * 128:(g + 1) * 128], src, self.ident_b()), reads=[sres, self.r_cst], writes=[self.pr[b]])
                    for g, src in enumerate(grp):
                        if evc[0] % 2 == 0:
                            kb.op(kb.act, lambda: nc.scalar.copy(dst_fn(k + g), pv[:, g * 128:(g + 1) * 128]), reads=[self.pr[b]], writes=[dres])
                        else:
                            kb.op(kb.dve, lambda: nc.vector.tensor_copy(dst_fn(k + g), pv[:, g * 128:(g + 1) * 128]), reads=[self.pr[b]], writes=[dres])
                        evc[0] += 1
                    k += 8

            for t4 in range(NT // 4):
                ht, hres = rh.next()
                kb.dma(kb.sp, ht[:], hTv[:, :, t4 * 512:(t4 + 1) * 512], writes=[hres])
                qTs, qTr = rqT.next()
                qiTs, qiTr = rqiT.next()
                kTs, kTr = rkT.next()
                kiTs, kiTr = rkiT.next()
                for cbk in range(4):
                    w, wr = rw.next()
                    kb.dma(kb.sp, w[:], wv[:, :, cbk * 512:(cbk + 1) * 512], writes=[wr])
                    for tt in range(4):
                        j = t4 * 4 + tt
                        b = proj(ht, hres, w, wr, tt, 512)
                        rope(b, 0, 4, 64, j, qtile[tt][:, cbk * 4:(cbk + 1) * 4, :], qres[tt])
                for tt in range(4):
                    transp([qtile[tt][:, h, :] for h in range(16)], qres[tt], lambda h: qTs[:, h, tt * 128:(tt + 1) * 128], qTr)
                w, wr = rw.next()
                kb.dma(kb.sp, w[:, :, 0:256], wv[:, :, 2048:2304], writes=[wr])
                for tt in range(4):
                    j = t4 * 4 + tt
                    b = proj(ht, hres, w, wr, tt, 256)
                    kr_, krr = rkr.next()
                    rope(b, 0, 1, 64, j, kr_[:].rearrange("p (h d) -> p h d", h=1), krr)
                    vs, vsr = rvs.next()
                    kb.op(kb.act, lambda: nc.scalar.copy(vs[:], self.ps[b][:, 128:256]), reads=[self.pr[b]], writes=[vsr])
                    kb.dma(kb.pool, self.v1d[:, j, :], vs[:], reads=[vsr])
                    transp([kr_[:]], krr, lambda h: kTs[:, tt * 128:(tt + 1) * 128], kTr)
                for cbk in range(2):
                    w, wr = rw.next()
                    kb.dma(kb.sp, w[:], wv[:, :, 2304 + cbk * 512:2304 + (cbk + 1) * 512], writes=[wr])
                    for tt in range(4):
                        j = t4 * 4 + tt
                        b = proj(ht, hres, w, wr, tt, 512)
                        qv = qtile[tt][:].rearrange("p a b -> p (a b)")[:, cbk * 512:(cbk + 1) * 512].rearrange("p (h d) -> p h d", h=8)
                        rope(b, 0, 8, 32, j, qv, qres[tt])
                for tt in range(4):
                    flat = qtile[tt][:].rearrange("p a b -> p (a b)")
                    transp([flat[:, pr * 128:(pr + 1) * 128] for pr in range(8)], qres[tt], lambda pr: qiTs[:, pr, tt * 128:(tt + 1) * 128], qiTr)
                w, wr = rw.next()
                kb.dma(kb.sp, w[:, :, 0:80], wv[:, :, 3328:3408], writes=[wr])
                for tt in range(4):
                    j = t4 * 4 + tt
                    b = proj(ht, hres, w, wr, tt, 80)
                    kr_, krr = rkr.next()
                    rope(b, 0, 1, 32, j, kr_[:, 0:64].rearrange("p (h d) -> p h d", h=1), krr)
                    kb.op(kb.pool, lambda: nc.gpsimd.tensor_copy(kr_[:, 64:128], kr_[:, 0:64]), reads=[krr], writes=[krr])
                    ws, wsr = rws.next()
                    kb.op(kb.act, lambda: nc.scalar.copy(ws[:], self.ps[b][:, 64:80]), reads=[self.pr[b]], writes=[wsr])
                    kb.dma(kb.pool, self.wd[:, j, :], ws[:], reads=[wsr])
                    transp([kr_[:]], krr, lambda h: kiTs[:, tt * 128:(tt + 1) * 128], kiTr)
                sl = slice(t4 * 512, (t4 + 1) * 512)
                kb.dma(kb.pool, self.qT1.rearrange("h p t -> p h t")[:, :, sl], qTs[:], reads=[qTr])
                kb.dma(kb.pool, self.qiT1.rearrange("h p t -> p h t")[:, :, sl], qiTs[:], reads=[qiTr])
                kb.dma(kb.pool, self.kT1d[:, sl], kTs[:], reads=[kTr])
                kb.dma(kb.pool, self.kiT2d[:, sl], kiTs[:], reads=[kiTr])

    def stage_dsa(self):
        kb, nc = self.kb, self.nc
        scale = HD ** -0.5
        import os
        ntiles = int(os.environ.get("DSA_TILES", NT))
        with contextlib.ExitStack() as ctx:
            T = lambda name, shape, dty: ctx.enter_context(_sbt(nc, name, shape, dty))
            kT1, kiT2 = T("kT1", [128, S], BF16), T("kiT2", [128, S], BF16)
            V1, wsb = T("V1", [128, NT, 129], BF16), T("wsb", [128, NT, 16], F32)
            id4 = T("id4", [128, 512], BF16)
            r_k = Res()
            kb.dma(kb.sp, kT1[:], self.kT1d[:, :], writes=[r_k])
            kb.dma(kb.sp, kiT2[:], self.kiT2d[:, :], writes=[r_k])
            kb.dma(kb.sp, V1[:, :, 0:128], self.v1d[:, :, :], writes=[r_k])
            kb.dma(kb.sp, wsb[:], self.wd[:, :, :], writes=[r_k])
            kb.op(kb.pool, lambda: nc.gpsimd.memset(V1[:, :, 128:129], 1.0), writes=[r_k])
            for g in range(4):
                kb.op(kb.pool, lambda: nc.gpsimd.tensor_copy(id4[:, g * 128:(g + 1) * 128], self.ident_b()), reads=[self.r_cst], writes=[r_k])
            rI = Ring(ctx, nc, "dI", 2, [128, S], F32)
            Wk = T("dWk", [128, S], F32)
            r_wk = Res()
            rNM = Ring(ctx, nc, "dNM", 2, [128, S], BF16)
            rqi = Ring(ctx, nc, "dqi", 2, [128, 8, 128], BF16)
            rq = Ring(ctx, nc, "dq", 2, [128, 16, 128], BF16)
            rWd = Ring(ctx, nc, "dWd", 2, [128, 16, 128], BF16)
            rR = Ring(ctx, nc, "dR", 4, [128, 512], BF16)
            rPT = Ring(ctx, nc, "dPT", 3, [128, 512], BF16)
            rosb = Ring(ctx, nc, "dosb", 2, [128, 16, 128], BF16)
            roT = Ring(ctx, nc, "doT", 2, [128, 16, 512], BF16)
            rm8 = Ring(ctx, nc, "dm8", 2, [128, 8], F32)
            rthr = Ring(ctx, nc, "dthr", 2, [128, 1], F32)
            rrd = Ring(ctx, nc, "drd", 4, [128, 1], F32)
            ibank = 0
            sbank = 0
            tb = 0
            oTs, oTr = None, None
            for i in range(ntiles):
                n_i = 128 * (i + 1)
                tt = i % 4
                if tt == 0:
                    oTs, oTr = roT.next()
                qiT, qir = rqi.next()
                qT, qr = rq.next()
                Wd, wdr = rWd.next()
                Isb, Ir = rI.next()
                kb.dma(kb.sp, qiT[:], self.qiT1.rearrange("h p t -> p h t")[:, :, i * 128:(i + 1) * 128], writes=[qir])
                kb.dma(kb.sp, qT[:], self.qT1.rearrange("h p t -> p h t")[:, :, i * 128:(i + 1) * 128], writes=[qr])
                for h in range(16):
                    kb.op(kb.pool, lambda: nc.gpsimd.tensor_scalar(out=Wd[:, h, :], in0=self.ident_b(), scalar1=wsb[:, i, h:h + 1], scalar2=0.25 * 0.125, op0=ALU.mult, op1=ALU.mult),
                          reads=[r_k, self.r_cst], writes=[wdr])
                for sb in range((n_i + 511) // 512):
                    nco = min(512, n_i - 512 * sb)
                    bA = 3 + (sb % 2)
                    for h in range(16):
                        hp, pr = h % 2, h // 2
                        bI = ibank % 3
                        ibank += 1
                        kb.op(kb.pe, lambda: nc.tensor.matmul(self.ps[bI][:, 0:nco], lhsT=qiT[hp * 64:(hp + 1) * 64, pr, :], rhs=kiT2[hp * 64:(hp + 1) * 64, sb * 512:sb * 512 + nco], start=True, stop=True),
                              reads=[qir, r_k], writes=[self.pr[bI]])
                        R, rr = rR.next()
                        kb.op(kb.act, lambda: nc.scalar.activation(out=R[:, 0:nco], in_=self.ps[bI][:, 0:nco], func=ACT.Relu), reads=[self.pr[bI]], writes=[rr])
                        kb.op(kb.pe, lambda: nc.tensor.matmul(self.ps[bA][:, 0:nco], lhsT=Wd[:, h, :], rhs=R[:, 0:nco], start=(h == 0), stop=(h == 15)),
                              reads=[wdr, rr], writes=[self.pr[bA]])
                    kb.op(kb.act, lambda: nc.scalar.copy(Isb[:, sb * 512:sb * 512 + nco], self.ps[bA][:, 0:nco]), reads=[self.pr[bA]], writes=[Ir])
                kb.op(kb.dve, lambda: nc.vector.memset(Isb[0:64, n_i - 64:n_i], NEG), writes=[Ir])
                thr, thr_r = rthr.next()
                if n_i <= 256:
                    kb.op(kb.dve, lambda: nc.vector.memset(thr[:], -1.0e29), writes=[thr_r])
                else:
                    cur = Isb
                    cur_r = Ir
                    for rnd in range(32):
                        m8, m8r = rm8.next()
                        kb.op(kb.dve, lambda: nc.vector.max(out=m8[:], in_=cur[:, 0:n_i]), reads=[cur_r], writes=[m8r])
                        if rnd < 31:
                            kb.op(kb.dve, lambda: nc.vector.match_replace(out=Wk[:, 0:n_i], in_to_replace=m8[:], in_values=cur[:, 0:n_i], imm_value=NEG), reads=[cur_r, m8r], writes=[r_wk])
                            cur, cur_r = Wk, r_wk
                        else:
                            kb.op(kb.dve, lambda: nc.vector.tensor_copy(thr[:], m8[:, 7:8]), reads=[m8r], writes=[thr_r])
                NM, nmr = rNM.next()
                kb.op(kb.dve, lambda: nc.vector.tensor_scalar(out=NM[:, 0:n_i], in0=Isb[:, 0:n_i], scalar1=thr[:, 0:1], scalar2=-30000.0, op0=ALU.is_lt, op1=ALU.mult),
                      reads=[Ir, thr_r], writes=[nmr])
                osb, osr = rosb.next()
                for hg in range(4):
                    for j in range(i + 1):
                        bS = sbank % 2
                        sbank += 1
                        kb.op(kb.pe, lambda: nc.tensor.matmul(self.ps[bS][:, :], lhsT=kT1[:, j * 128:(j + 1) * 128], rhs=qT[:, hg * 4:(hg + 1) * 4, :].rearrange("p h t -> p (h t)"), start=True, stop=False),
                              reads=[r_k, qr], writes=[self.pr[bS]])
                        kb.op(kb.pe, lambda: nc.tensor.matmul(self.ps[bS][:, :], lhsT=NM[:, j * 128:(j + 1) * 128], rhs=id4[:], start=False, stop=True),
                              reads=[nmr, r_k], writes=[self.pr[bS]])
                        PT, ptr = rPT.next()
                        kb.op(kb.act, lambda: nc.scalar.activation(out=PT[:], in_=self.ps[bS][:, :], func=ACT.Exp, scale=scale), reads=[self.pr[bS]], writes=[ptr])
                        for hh in range(4):
                            bO = 2 + hh
                            kb.op(kb.pe, lambda: nc.tensor.matmul(self.ps[bO][:, 0:129], lhsT=PT[:, hh * 128:(hh + 1) * 128], rhs=V1[:, j, :], start=(j == 0), stop=(j == i)),
                                  reads=[ptr, r_k], writes=[self.pr[bO]])
                    for hh in range(4):
                        bO = 2 + hh
                        rd, rdr = rrd.next()
                        kb.op(kb.dve, lambda: nc.vector.reciprocal(rd[:], self.ps[bO][:, 128:129]), reads=[self.pr[bO]], writes=[rdr])
                        kb.op(kb.act, lambda: nc.scalar.activation(out=osb[:, hg * 4 + hh, :], in_=self.ps[bO][:, 0:128], func=ACT.Copy, scale=rd[:, 0:1]), reads=[self.pr[bO], rdr], writes=[osr])
                for half in range(2):
                    b = 6 + tb % 2
                    tb += 1
                    pv = self.ps[b][:].bitcast(BF16)
                    for g in range(8):
                        kb.op(kb.pe, lambda: nc.tensor.transpose(pv[:, g * 128:(g + 1) * 128], osb[:, half * 8 + g, :], self.ident_b()), reads=[osr, self.r_cst], writes=[self.pr[b]])
                    for g in range(8):
                        kb.op(kb.act, lambda: nc.scalar.copy(oTs[:, half * 8 + g, tt * 128:(tt + 1) * 128], pv[:, g * 128:(g + 1) * 128]), reads=[self.pr[b]], writes=[oTr])
                if tt == 3 or i == ntiles - 1:
                    t4 = i // 4
                    kb.dma(kb.pool, self.oT.rearrange("k p t -> p k t")[:, :, t4 * 512:(t4 + 1) * 512], oTs[:], reads=[oTr])


def make_in_maps(inputs):
    f = lambda a: np.ascontiguousarray(a)
    cst = make_consts()
    maps = []
    for b in range(8):
        maps.append({
            "x": f(inputs["x"][b]), "c": f(inputs["c"][b].reshape(16, 128)),
            "pos": f(inputs["positions"][b].reshape(32, 128).astype(np.int32)),
            "ada_w": inputs["ada_w"], "ada_b": inputs["ada_b"], "norm_g": inputs["norm_g"],
            "mix_w_out": inputs["mix_w_out"], "even_w_in": f(inputs["even_w_in"][0]),
            "even_b": f(inputs["even_b_forget"].reshape(1, 8)), "odd_w_in": f(inputs["odd_w_in"][0]),
            "ff_w1": inputs["ff_w1"], "ff_w2": inputs["ff_w2"], "cst": cst,
        })
    return maps


def kernel(**inputs):
    inputs = {k: np.asarray(v) for k, v in inputs.items()}
    prog = Prog()
    res = run_bass_kernel_spmd(prog.nc, make_in_maps(inputs), core_ids=list(range(8)))
    return np.stack([r["out"] for r in res.results], axis=0).astype(np.float32)
```

```python
import contextlib
import numpy as np
import concourse.bass as bass
import concourse.mybir as mybir
from concourse.bass_utils import run_bass_kernel_spmd

ACT = mybir.ActivationFunctionType
ALU = mybir.AluOpType
AX = mybir.AxisListType
F32, BF16, I32 = mybir.dt.float32, mybir.dt.bfloat16, mybir.dt.int32

S, D, DFF, HD = 4096, 2048, 8192, 128
NT = S // 128
KC = D // 128
EVEN_W, ODD_W = 6152, 3408
EPS = 1e-6
NEG = -1.0e30
SB_WIN = 3
NSLOT = 8

C_ID, C_TRI, C_TRS, C_LOW, C_ONE, C_INV = 0, 128, 256, 384, 512, 640
C_W = 704


def make_consts():
    c = np.zeros((128, C_W), np.float32)
    a = np.arange(128)
    c[:, C_ID:C_ID + 128] = np.eye(128)
    c[:, C_TRI:C_TRI + 128] = (a[:, None] <= a[None, :])
    c[:, C_TRS:C_TRS + 128] = (a[:, None] < a[None, :])
    c[:, C_LOW:C_LOW + 128] = (a[:, None] >= a[None, :])
    c[:, C_ONE:C_ONE + 128] = 1.0
    inv = (10000.0 ** (-np.arange(64, dtype=np.float32) / 64)).astype(np.float32)
    c[:, C_INV:C_INV + 64] = inv[None, :]
    return c


_uid = [0]


def _sbt(nc, name, shape, dty):
    _uid[0] += 1
    return nc.sbuf_tensor(f"{name}_u{_uid[0]}", shape, dty)


class Res:
    __slots__ = ("w", "r", "x")

    def __init__(self, x=False):
        self.w = None
        self.r = {}
        self.x = x


class Eng:
    def __init__(self, name, obj, sem, is_pe=False):
        self.name, self.obj, self.sem, self.is_pe = name, obj, sem, is_pe
        self.n = 0
        self.waited = {}
        self.dma_sems, self.dma_cnt, self.dma_i = [], [], 0


class KB:
    def __init__(self, nc):
        self.nc = nc
        self.stack = contextlib.ExitStack()
        mk = lambda nm: self.stack.enter_context(nc.semaphore(nm))
        self.pe = Eng("pe", nc.tensor, mk("s_pe"), True)
        self.act = Eng("act", nc.scalar, mk("s_act"))
        self.dve = Eng("dve", nc.vector, mk("s_dve"))
        self.pool = Eng("pool", nc.gpsimd, mk("s_pool"))
        self.sp = Eng("sp", nc.sync, mk("s_sp"))
        self.engs = [self.pe, self.act, self.dve, self.pool, self.sp]
        self.queues = [self.sp, self.pool, self.act]
        for q in self.queues:
            q.dma_sems = [mk(f"d_{q.name}{i}") for i in range(NSLOT)]
            q.dma_cnt = [0] * NSLOT
        self.n_ins = 0

    def _wait(self, eng, tk):
        sem, val, src = tk
        if src is eng and eng.is_pe:
            return
        key = sem.name
        if eng.waited.get(key, 0) >= val:
            return
        eng.obj.wait_ge(sem, val)
        eng.waited[key] = val

    def _deps(self, eng, reads, writes):
        for r in reads:
            if r.w is not None:
                self._wait(eng, r.w)
            if r.x:
                for k, tk in r.r.items():
                    if k != eng.name:
                        self._wait(eng, tk)
        for w in writes:
            if w.w is not None:
                self._wait(eng, w.w)
            for tk in w.r.values():
                self._wait(eng, tk)

    def _mark(self, key, tk, reads, writes):
        for r in reads:
            r.r[key] = tk
        for w in writes:
            w.w = tk
            w.r = {}

    def op(self, eng, fn, reads=(), writes=()):
        self._deps(eng, reads, writes)
        ins = fn()
        eng.n += 1
        ins.then_inc(eng.sem, 1)
        tk = (eng.sem, eng.n, eng)
        self._mark(eng.name, tk, reads, writes)
        self.n_ins += 1
        return tk

    def dma(self, q, out, in_, reads=(), writes=()):
        slot = q.dma_i % NSLOT
        q.dma_i += 1
        sem = q.dma_sems[slot]
        if q.dma_cnt[slot] > 0:
            self._wait(q, (sem, 16 * q.dma_cnt[slot], None))
        self._deps(q, reads, writes)
        q.obj.dma_start(out=out, in_=in_).then_inc(sem, 16)
        q.dma_cnt[slot] += 1
        tk = (sem, 16 * q.dma_cnt[slot], None)
        self._mark(sem.name, tk, reads, writes)
        self.n_ins += 1
        return tk

    def barrier(self):
        for e in self.engs:
            for f in self.engs:
                if f is not e and f.n > 0:
                    self._wait(e, (f.sem, f.n, f))
            for q in self.queues:
                for i in range(NSLOT):
                    if q.dma_cnt[i] > 0:
                        self._wait(e, (q.dma_sems[i], 16 * q.dma_cnt[i], None))


class Ring:
    def __init__(self, ctx, nc, name, n, shape, dtype):
        self.t = [ctx.enter_context(_sbt(nc, f"{name}{i}", shape, dtype)) for i in range(n)]
        self.r = [Res() for _ in range(n)]
        self.i = 0

    def next(self):
        k = self.i % len(self.t)
        self.i += 1
        return self.t[k], self.r[k]


class Prog:
    def __init__(self, stages=None, xsrc_is_out=False):
        self.stages = stages
        nc = self.nc = bass.Bass("TRN2", target_bir_lowering=False)
        kb = self.kb = KB(nc)
        import os
        dbg = set(os.environ.get("DEBUG_OUT", "").split(","))
        dt = lambda name, shape, dty, kind: nc.dram_tensor(name, shape, dty, kind=("ExternalOutput" if name in dbg else kind)).ap()
        I, O, N = "ExternalInput", "ExternalOutput", "Internal"
        self.x = dt("x", [S, D], F32, I)
        self.c = dt("c", [16, 128], F32, I)
        self.pos = dt("pos", [32, 128], I32, I)
        self.ada_w = dt("ada_w", [2, D, 6 * D], F32, I)
        self.ada_b = dt("ada_b", [2, 6 * D], F32, I)
        self.norm_g = dt("norm_g", [2, 4, D], F32, I)
        self.mix_w_out = dt("mix_w_out", [2, D, D], F32, I)
        self.even_w_in = dt("even_w_in", [D, EVEN_W], F32, I)
        self.even_b = dt("even_b", [1, 8], F32, I)
        self.odd_w_in = dt("odd_w_in", [D, ODD_W], F32, I)
        self.ff_w1 = dt("ff_w1", [2, D, DFF], F32, I)
        self.ff_w2 = dt("ff_w2", [2, DFF, D], F32, I)
        self.cst = dt("cst", [128, C_W], F32, I)
        self.out = dt("out", [S, D], F32, O)
        self.wb_in0 = dt("wb_in0", [D, EVEN_W], BF16, N)
        self.wb_in1 = dt("wb_in1", [D, ODD_W], BF16, N)
        self.wb_out = [dt(f"wb_out{l}", [D, D], BF16, N) for l in range(2)]
        self.wb_f1 = [dt(f"wb_f1{l}", [D, DFF], BF16, N) for l in range(2)]
        self.wb_f2 = [dt(f"wb_f2{l}", [DFF, D], BF16, N) for l in range(2)]
        self.modd = dt("modd", [2, 6 * D], F32, N)
        self.hT = dt("hT", [KC, 128, S], BF16, N)
        self.oT = dt("oT", [KC, 128, S], BF16, N)
        self.qkT = dt("qkT", [32, 128, S], BF16, N)
        self.vt = dt("vt", [16, 128, NT, 128], BF16, N)
        self.qT1 = dt("qT1", [16, 128, S], BF16, N)
        self.qiT1 = dt("qiT1", [8, 128, S], BF16, N)
        self.dbg = dt("dbg", [128, 512], F32, N)
        self.kT1d = dt("kT1d", [128, S], BF16, N)
        self.kiT2d = dt("kiT2d", [128, S], BF16, N)
        self.v1d = dt("v1d", [128, NT, 128], BF16, N)
        self.wd = dt("wd", [128, NT, 16], F32, N)
        self.ps = [nc.alloc_psum_tensor(f"ps{b}", [128, 512], F32) for b in range(8)]
        self.pr = [Res(True) for _ in range(8)]
        A = nc.alloc_sbuf_tensor
        self.cst_sb = A("cst_sb", [128, C_W], F32)
        self.cb = A("cb", [128, 640], BF16)
        self.eps_t = A("eps_t", [128, 1], F32)
        self.one_t = A("one_t", [128, 1], F32)
        self.zero_t = A("zero_t", [128, 1], F32)
        self.acol = A("acol", [128, 16], F32)
        self.bcol = A("bcol", [128, 16], F32)
        self.G = A("G", [128, D], F32)
        self.Pcol = A("Pcol", [128, 256], F32)
        self.Pb = A("Pb", [128, 256], F32)
        self.r_cst, self.r_cols, self.r_G, self.r_P = Res(), Res(), Res(), Res()
        self.build()

    def ident_f(self, n=128):
        return self.cst_sb[0:n, C_ID:C_ID + n]

    def ident_b(self):
        return self.cb[:, C_ID:C_ID + 128]

    def on(self, name):
        return self.stages is None or name in self.stages

    def build(self):
        kb, nc = self.kb, self.nc
        kb.dma(kb.sp, self.cst_sb[:], self.cst[:, :], writes=[self.r_cst])
        kb.op(kb.dve, lambda: nc.vector.tensor_copy(self.cb[:], self.cst_sb[:, 0:640]), reads=[self.r_cst], writes=[self.r_cst])
        kb.op(kb.dve, lambda: nc.vector.memset(self.eps_t[:], EPS), writes=[self.r_cst])
        kb.op(kb.dve, lambda: nc.vector.memset(self.one_t[:], 1.0), writes=[self.r_cst])
        kb.op(kb.dve, lambda: nc.vector.memset(self.zero_t[:], 0.0), writes=[self.r_cst])
        kb.barrier()
        if self.on("cast"):
            self.stage_cast()
            kb.barrier()
        if self.on("ada"):
            self.stage_ada()
            kb.barrier()
        xsrc = self.x
        for l in range(2):
            if self.on(f"mix{l}"):
                self.stage_normT(l, "a", xsrc)
                kb.barrier()
                if l == 0:
                    self.stage_inproj0()
                    kb.barrier()
                    self.stage_attn0()
                    kb.barrier()
                else:
                    self.stage_inproj1()
                    kb.barrier()
                    self.stage_dsa()
                    kb.barrier()
                self.stage_outproj(l, xsrc)
                kb.barrier()
                xsrc = self.out
            if self.on(f"ffn{l}"):
                self.stage_normT(l, "m", xsrc)
                kb.barrier()
                if self.stages is None or "skipffn" not in self.stages:
                    self.stage_ffn(l, xsrc)
                    kb.barrier()
                    xsrc = self.out
        kb.barrier()

    def stage_cast(self):
        kb, nc = self.kb, self.nc
        jobs = [(self.even_w_in, self.wb_in0)]
        for l in range(2):
            pass
        jobs += [(self.mix_w_out[0], self.wb_out[0]), (self.ff_w1[0], self.wb_f1[0]), (self.ff_w2[0], self.wb_f2[0]),
                 (self.odd_w_in, self.wb_in1), (self.mix_w_out[1], self.wb_out[1]), (self.ff_w1[1], self.wb_f1[1]),
                 (self.ff_w2[1], self.wb_f2[1])]
        CW = 2048
        with contextlib.ExitStack() as ctx:
            rf = Ring(ctx, nc, "cf", 4, [128, CW], F32)
            rb = Ring(ctx, nc, "cbf", 4, [128, CW], BF16)
            k = 0
            for src, dst in jobs:
                R, C = src.shape
                for rbk in range(R // 128):
                    for c0 in range(0, C, CW):
                        cw = min(CW, C - c0)
                        tf, rfr = rf.next()
                        tb, rbr = rb.next()
                        kb.dma(kb.sp, tf[:, 0:cw], src[rbk * 128:(rbk + 1) * 128, c0:c0 + cw], writes=[rfr])
                        if k % 2 == 0:
                            kb.op(kb.act, lambda: nc.scalar.copy(tb[:, 0:cw], tf[:, 0:cw]), reads=[rfr], writes=[rbr])
                        else:
                            kb.op(kb.dve, lambda: nc.vector.tensor_copy(tb[:, 0:cw], tf[:, 0:cw]), reads=[rfr], writes=[rbr])
                        kb.dma(kb.pool, dst[rbk * 128:(rbk + 1) * 128, c0:c0 + cw], tb[:, 0:cw], reads=[rbr])
                        k += 1

    def stage_ada(self):
        kb, nc = self.kb, self.nc
        with contextlib.ExitStack() as ctx:
            T = lambda name, shape, dty: ctx.enter_context(_sbt(nc, name, shape, dty))
            c16, sg16, csT = T("c16", [16, 128], F32), T("sg16", [16, 128], F32), T("csT", [128, 16], F32)
            brow, mrow = T("brow", [1, 6 * D], F32), T("mrow", [1, 6 * D], F32)
            r_c, r_b, r_m = Res(), Res(), Res()
            wr = Ring(ctx, nc, "adaw", 3, [128, 3072], F32)
            kb.dma(kb.sp, c16[:], self.c[:, :], writes=[r_c])
            kb.op(kb.act, lambda: nc.scalar.activation(out=sg16[:], in_=c16[:], func=ACT.Sigmoid), reads=[r_c], writes=[r_c])
            kb.op(kb.dve, lambda: nc.vector.tensor_mul(c16[:], c16[:], sg16[:]), reads=[r_c], writes=[r_c])
            kb.op(kb.pe, lambda: nc.tensor.transpose(self.ps[0][:, 0:16], c16[:], self.ident_f(16)), reads=[r_c, self.r_cst], writes=[self.pr[0]])
            kb.op(kb.dve, lambda: nc.vector.tensor_copy(csT[:], self.ps[0][:, 0:16]), reads=[self.pr[0]], writes=[r_c])
            for l in range(2):
                kb.dma(kb.sp, brow[:], self.ada_b[l:l + 1, :], writes=[r_b])
                for g in range(4):
                    for kc in range(KC):
                        wt, wres = wr.next()
                        kb.dma(kb.sp, wt[:], self.ada_w[l, kc * 128:(kc + 1) * 128, g * 3072:(g + 1) * 3072], writes=[wres])
                        for b in range(6):
                            kb.op(kb.pe, lambda b=b: nc.tensor.matmul(self.ps[b][0:1, :], lhsT=csT[:, kc:kc + 1], rhs=wt[:, b * 512:(b + 1) * 512],
                                                                   start=(kc == 0), stop=(kc == KC - 1)),
                                  reads=[r_c, wres], writes=[self.pr[b]])
                    for b in range(6):
                        c0 = g * 3072 + b * 512
                        kb.op(kb.dve, lambda b=b, c0=c0: nc.vector.tensor_tensor(out=mrow[0:1, c0:c0 + 512], in0=self.ps[b][0:1, :], in1=brow[0:1, c0:c0 + 512], op=ALU.add),
                              reads=[self.pr[b], r_b], writes=[r_m])
                kb.dma(kb.pool, self.modd[l:l + 1, :], mrow[:], reads=[r_m])

    def prep_cols(self, ctx, l, which):
        kb, nc = self.kb, self.nc
        off = 0 if which == "a" else 3
        gi = 0 if which == "a" else 2
        T = lambda name, shape, dty: ctx.enter_context(_sbt(nc, name, shape, dty))
        sc16, sh16, gm16 = T("sc16", [16, 128], F32), T("sh16", [16, 128], F32), T("gm16", [16, 128], F32)
        r = Res()
        v16 = lambda ap: ap.rearrange("(c p) -> c p", p=128)
        kb.dma(kb.sp, sh16[:], v16(self.modd[l, off * D:(off + 1) * D]), writes=[r])
        kb.dma(kb.sp, sc16[:], v16(self.modd[l, (off + 1) * D:(off + 2) * D]), writes=[r])
        kb.dma(kb.sp, gm16[:], v16(self.norm_g[l, gi, :]), writes=[r])
        kb.op(kb.dve, lambda: nc.vector.scalar_tensor_tensor(out=sc16[:], in0=sc16[:], scalar=1.0, in1=gm16[:], op0=ALU.add, op1=ALU.mult), reads=[r], writes=[r])
        kb.op(kb.pe, lambda: nc.tensor.transpose(self.ps[0][:, 0:16], sc16[:], self.ident_f(16)), reads=[r, self.r_cst], writes=[self.pr[0]])
        kb.op(kb.pe, lambda: nc.tensor.transpose(self.ps[1][:, 0:16], sh16[:], self.ident_f(16)), reads=[r, self.r_cst], writes=[self.pr[1]])
        kb.op(kb.dve, lambda: nc.vector.tensor_copy(self.acol[:], self.ps[0][:, 0:16]), reads=[self.pr[0]], writes=[self.r_cols])
        kb.op(kb.dve, lambda: nc.vector.tensor_copy(self.bcol[:], self.ps[1][:, 0:16]), reads=[self.pr[1]], writes=[self.r_cols])

    def prep_G(self, ctx, l, which):
        kb, nc = self.kb, self.nc
        off = 2 if which == "a" else 5
        gi = 1 if which == "a" else 3
        T = lambda name, shape, dty: ctx.enter_context(_sbt(nc, name, shape, dty))
        grow, gmrow = T("grow", [1, D], F32), T("gmrow", [1, D], F32)
        r = Res()
        kb.dma(kb.sp, grow[:], self.modd[l:l + 1, off * D:(off + 1) * D], writes=[r])
        kb.dma(kb.sp, gmrow[:], self.norm_g[l, gi:gi + 1, :], writes=[r])
        kb.op(kb.dve, lambda: nc.vector.tensor_mul(grow[:], grow[:], gmrow[:]), reads=[r], writes=[r])
        for n in range(4):
            kb.op(kb.pe, lambda n=n: nc.tensor.matmul(self.ps[n][:, :], lhsT=self.cst_sb[0:1, C_ONE:C_ONE + 128], rhs=grow[0:1, n * 512:(n + 1) * 512], start=True, stop=True),
                  reads=[r, self.r_cst], writes=[self.pr[n]])
            kb.op(kb.dve, lambda n=n: nc.vector.tensor_copy(self.G[:, n * 512:(n + 1) * 512], self.ps[n][:, :]), reads=[self.pr[n]], writes=[self.r_G])

    def stage_normT(self, l, which, xsrc):
        kb, nc = self.kb, self.nc
        with contextlib.ExitStack() as ctx:
            T = lambda name, shape, dty: ctx.enter_context(_sbt(nc, name, shape, dty))
            with contextlib.ExitStack() as c2:
                self.prep_cols(c2, l, which)
            kb.barrier()
            rx = Ring(ctx, nc, "nx", 3, [128, D], F32)
            rxn = Ring(ctx, nc, "nxn", 2, [128, D], BF16)
            rst = Ring(ctx, nc, "nst", 4, [128, 4], F32)
            rh = Ring(ctx, nc, "nh", 2, [128, KC, 512], BF16)
            junk = T("njunk", [128, D], BF16)
            rj = Res()
            pbank = 0
            for t4 in range(NT // 4):
                hts, hres = rh.next()
                for tt in range(4):
                    t = t4 * 4 + tt
                    xt, xr = rx.next()
                    xn, xnr = rxn.next()
                    st, sr = rst.next()
                    kb.dma(kb.sp, xt[:], xsrc[t * 128:(t + 1) * 128, :], writes=[xr])
                    kb.op(kb.act, lambda: nc.scalar.activation(out=junk[:], in_=xt[:], func=ACT.Square, accum_out=st[:, 0:1]), reads=[xr], writes=[rj, sr])
                    kb.op(kb.act, lambda: nc.scalar.activation(out=st[:, 1:2], in_=st[:, 0:1], func=ACT.Sqrt, bias=self.eps_t[:], scale=1.0 / D), reads=[sr], writes=[sr])
                    kb.op(kb.dve, lambda: nc.vector.reciprocal(st[:, 2:3], st[:, 1:2]), reads=[sr], writes=[sr])
                    kb.op(kb.dve, lambda: nc.vector.tensor_scalar(out=xn[:], in0=xt[:], scalar1=st[:, 2:3], scalar2=None, op0=ALU.mult), reads=[xr, sr], writes=[xnr])
                    for half in range(2):
                        b = pbank % 4
                        pbank += 1
                        pv = self.ps[b][:].bitcast(BF16)
                        for k8 in range(8):
                            kc = half * 8 + k8
                            kb.op(kb.pe, lambda kc=kc, k8=k8, pv=pv: nc.tensor.transpose(pv[:, k8 * 128:(k8 + 1) * 128], xn[:, kc * 128:(kc + 1) * 128], self.ident_b()),
                                  reads=[xnr, self.r_cst], writes=[self.pr[b]])
                        for k8 in range(8):
                            kc = half * 8 + k8
                            dst = hts[:, kc, tt * 128:(tt + 1) * 128]
                            src = pv[:, k8 * 128:(k8 + 1) * 128]
                            if k8 % 2 == 0:
                                kb.op(kb.act, lambda dst=dst, src=src, kc=kc: nc.scalar.activation(out=dst, in_=src, func=ACT.Identity, bias=self.bcol[:, kc:kc + 1], scale=self.acol[:, kc:kc + 1]),
                                      reads=[self.pr[b], self.r_cols], writes=[hres])
                            else:
                                kb.op(kb.dve, lambda dst=dst, src=src, kc=kc: nc.vector.tensor_scalar(out=dst, in0=src, scalar1=self.acol[:, kc:kc + 1], scalar2=self.bcol[:, kc:kc + 1], op0=ALU.mult, op1=ALU.add),
                                      reads=[self.pr[b], self.r_cols], writes=[hres])
                kb.dma(kb.pool, self.hT.rearrange("k p t -> p k t")[:, :, t4 * 512:(t4 + 1) * 512], hts[:], reads=[hres])

    def rstd_from_ss(self, st, sr, ncols):
        kb, nc = self.kb, self.nc
        kb.op(kb.dve, lambda: nc.vector.tensor_reduce(out=st[:, 4:5], in_=st[:, 0:ncols], axis=AX.X, op=ALU.add), reads=[sr], writes=[sr])
        kb.op(kb.act, lambda: nc.scalar.activation(out=st[:, 5:6], in_=st[:, 4:5], func=ACT.Sqrt, bias=self.eps_t[:], scale=1.0 / D), reads=[sr], writes=[sr])
        kb.op(kb.dve, lambda: nc.vector.reciprocal(st[:, 6:7], st[:, 5:6]), reads=[sr], writes=[sr])

    def stage_outproj(self, l, xsrc):
        kb, nc = self.kb, self.nc
        with contextlib.ExitStack() as ctx:
            T = lambda name, shape, dty: ctx.enter_context(_sbt(nc, name, shape, dty))
            with contextlib.ExitStack() as c2:
                self.prep_G(c2, l, "a")
            kb.barrier()
            wo = T("wo", [128, KC, D], BF16)
            r_wo = Res()
            wv = self.wb_out[l].rearrange("(k p) c -> p k c", p=128)
            for n in range(4):
                kb.dma(kb.sp, wo[:, :, n * 512:(n + 1) * 512], wv[:, :, n * 512:(n + 1) * 512], writes=[r_wo])
            ro = Ring(ctx, nc, "oo", 2, [128, KC, 512], BF16)
            rx = Ring(ctx, nc, "ox", 2, [128, D], F32)
            rt1 = Ring(ctx, nc, "ot1", 2, [128, D], F32)
            rst = Ring(ctx, nc, "ost", 4, [128, 8], F32)
            junk = T("ojunk", [128, 512], BF16)
            rj = Res()
            for t4 in range(NT // 4):
                ot, ores = ro.next()
                kb.dma(kb.sp, ot[:], self.oT.rearrange("k p t -> p k t")[:, :, t4 * 512:(t4 + 1) * 512], writes=[ores])
                for tt in range(4):
                    t = t4 * 4 + tt
                    xt, xr = rx.next()
                    t1, t1r = rt1.next()
                    st, sr = rst.next()
                    kb.dma(kb.sp, xt[:], xsrc[t * 128:(t + 1) * 128, :], writes=[xr])
                    base = (t % 2) * 4
                    for n in range(4):
                        b = base + n
                        for kc in range(KC):
                            kb.op(kb.pe, lambda kc=kc, b=b, n=n: nc.tensor.matmul(self.ps[b][:, :], lhsT=ot[:, kc, tt * 128:(tt + 1) * 128], rhs=wo[:, kc, n * 512:(n + 1) * 512],
                                                                             start=(kc == 0), stop=(kc == KC - 1)),
                                  reads=[ores, r_wo], writes=[self.pr[b]])
                        kb.op(kb.act, lambda b=b, n=n: nc.scalar.activation(out=junk[:], in_=self.ps[b][:, :], func=ACT.Square, accum_out=st[:, n:n + 1]), reads=[self.pr[b]], writes=[rj, sr])
                        kb.op(kb.dve, lambda b=b, n=n: nc.vector.tensor_tensor(out=t1[:, n * 512:(n + 1) * 512], in0=self.ps[b][:, :], in1=self.G[:, n * 512:(n + 1) * 512], op=ALU.mult),
                              reads=[self.pr[b], self.r_G], writes=[t1r])
                    self.rstd_from_ss(st, sr, 4)
                    kb.op(kb.dve, lambda: nc.vector.scalar_tensor_tensor(out=t1[:], in0=t1[:], scalar=st[:, 6:7], in1=xt[:], op0=ALU.mult, op1=ALU.add), reads=[t1r, sr, xr], writes=[t1r])
                    kb.dma(kb.pool, self.out[t * 128:(t + 1) * 128, :], t1[:], reads=[t1r])

    def stage_ffn(self, l, xsrc):
        kb, nc = self.kb, self.nc
        with contextlib.ExitStack() as ctx:
            T = lambda name, shape, dty: ctx.enter_context(_sbt(nc, name, shape, dty))
            with contextlib.ExitStack() as c2:
                self.prep_G(c2, l, "m")
            kb.barrier()
            rh = Ring(ctx, nc, "fh", 1, [128, KC, 512], BF16)
            uT = T("uT", [128, 64, 512], BF16)
            r_u = [Res() for _ in range(64)]
            rw1 = Ring(ctx, nc, "fw1", 3, [128, KC, 256], BF16)
            rw2 = Ring(ctx, nc, "fw2", 3, [128, 8, 512], BF16)
            rtmp = Ring(ctx, nc, "ftmp", 3, [128, 512], F32)
            ysb = T("ysb", [128, 4, D], F32)
            r_y = [Res() for _ in range(4)]
            rx = Ring(ctx, nc, "fx", 2, [128, D], F32)
            rst = Ring(ctx, nc, "fst", 8, [128, 8], F32)
            junk = T("fjunk", [128, 512], BF16)
            rj = Res()
            w1v = self.wb_f1[l].rearrange("(k p) c -> p k c", p=128)
            w2v = self.wb_f2[l].rearrange("(f p) c -> p f c", p=128)
            pb1 = 0
            import os
            for t4 in range(int(os.environ.get("FFN_BLOCKS", NT // 4))):
                ht, hres = rh.next()
                kb.dma(kb.sp, ht[:], self.hT.rearrange("k p t -> p k t")[:, :, t4 * 512:(t4 + 1) * 512], writes=[hres])
                for fb in range(32):
                    w1, w1r = rw1.next()
                    kb.dma(kb.sp, w1[:], w1v[:, :, fb * 256:(fb + 1) * 256], writes=[w1r])
                    for fi in range(2):
                        f = fb * 2 + fi
                        b = pb1 % 4
                        pb1 += 1
                        for kc in range(KC):
                            kb.op(kb.pe, lambda kc=kc, b=b, fi=fi: nc.tensor.matmul(self.ps[b][:, :], lhsT=w1[:, kc, fi * 128:(fi + 1) * 128], rhs=ht[:, kc, :],
                                                                               start=(kc == 0), stop=(kc == KC - 1)),
                                  reads=[w1r, hres], writes=[self.pr[b]])
                        tmp, tr = rtmp.next()
                        kb.op(kb.act, lambda b=b: nc.scalar.activation(out=tmp[:], in_=self.ps[b][:, :], func=ACT.Relu), reads=[self.pr[b]], writes=[tr])
                        kb.op(kb.dve, lambda f=f: nc.vector.tensor_tensor(out=uT[:, f, :], in0=tmp[:], in1=tmp[:], op=ALU.mult), reads=[tr], writes=[r_u[f]])
                if os.environ.get("FFN_PHASE") == "1":
                    continue
                sts = [rst.next() for _ in range(4)]
                for n in range(4):
                    for fg in range(8):
                        w2, w2r = rw2.next()
                        kb.dma(kb.sp, w2[:], w2v[:, fg * 8:(fg + 1) * 8, n * 512:(n + 1) * 512], writes=[w2r])
                        for j in range(8):
                            f = fg * 8 + j
                            for tt in range(4):
                                b = 4 + tt
                                kb.op(kb.pe, lambda f=f, j=j, tt=tt, b=b: nc.tensor.matmul(self.ps[b][:, :], lhsT=uT[:, f, tt * 128:(tt + 1) * 128], rhs=w2[:, j, :],
                                                                                       start=(f == 0), stop=(f == 63)),
                                      reads=[r_u[f], w2r], writes=[self.pr[b]])
                    for tt in range(4):
                        b = 4 + tt
                        st, sr = sts[tt]
                        if os.environ.get("FFN_NOEV") == "1":
                            continue
                        if os.environ.get("FFN_NOEV") != "2":
                            kb.op(kb.act, lambda b=b, st=st: nc.scalar.activation(out=junk[:], in_=self.ps[b][:, :], func=ACT.Square, accum_out=st[:, n:n + 1]), reads=[self.pr[b]], writes=[rj, sr])
                        kb.op(kb.dve, lambda b=b, tt=tt: nc.vector.tensor_tensor(out=ysb[:, tt, n * 512:(n + 1) * 512], in0=self.ps[b][:, :], in1=self.G[:, n * 512:(n + 1) * 512], op=ALU.mult),
                              reads=[self.pr[b], self.r_G], writes=[r_y[tt]])
                if os.environ.get("FFN_PHASE") == "2":
                    continue
                for tt in range(4):
                    t = t4 * 4 + tt
                    st, sr = sts[tt]
                    xt, xr = rx.next()
                    kb.dma(kb.sp, xt[:], xsrc[t * 128:(t + 1) * 128, :], writes=[xr])
                    self.rstd_from_ss(st, sr, 4)
                    kb.op(kb.dve, lambda tt=tt, st=st: nc.vector.scalar_tensor_tensor(out=xt[:], in0=ysb[:, tt, :], scalar=st[:, 6:7], in1=xt[:], op0=ALU.mult, op1=ALU.add),
                          reads=[r_y[tt], sr, xr], writes=[xr])
                    kb.dma(kb.pool, self.out[t * 128:(t + 1) * 128, :], xt[:], reads=[xr])

    def stage_inproj0(self):
        kb, nc = self.kb, self.nc
        hTv = self.hT.rearrange("k p t -> p k t")
        wv = self.wb_in0.rearrange("(k p) c -> p k c", p=128)
        qk_blocks = [(0, 0, 0), (512, 4, 0), (1024, 0, 1), (1536, 4, 1), (3080, 8, 0), (3592, 12, 0), (4104, 8, 1), (4616, 12, 1)]
        v_blocks = [(2048, 0), (2560, 4), (5128, 8), (5640, 12)]
        with contextlib.ExitStack() as ctx:
            T = lambda name, shape, dty: ctx.enter_context(_sbt(nc, name, shape, dty))
            rh = Ring(ctx, nc, "ih", 2, [128, KC, 512], BF16)
            rw = Ring(ctx, nc, "iw", 3, [128, KC, 512], BF16)
            rs = Ring(ctx, nc, "is", 4, [128, 512], BF16)
            wg = T("iwg", [128, KC, 128], BF16)
            nlf = T("nlf", [8, S], F32)
            brow, nb = T("ibrow", [1, 8], F32), T("inb", [8, 1], F32)
            etmp = T("ietmp", [8, 512], F32)
            r_wg, r_nlf, r_b, r_e = Res(), Res(), Res(), Res()
            kb.dma(kb.sp, wg[:], wv[:, :, 3008:3136], writes=[r_wg])
            kb.dma(kb.sp, brow[:], self.even_b[:, :], writes=[r_b])
            kb.op(kb.pe, lambda: nc.tensor.matmul(self.ps[7][0:8, 0:1], lhsT=brow[0:1, 0:8], rhs=self.cst_sb[0:1, C_ONE:C_ONE + 1], start=True, stop=True),
                  reads=[r_b, self.r_cst], writes=[self.pr[7]])
            kb.op(kb.dve, lambda: nc.vector.tensor_scalar(out=nb[:], in0=self.ps[7][0:8, 0:1], scalar1=-1.0, scalar2=None, op0=ALU.mult), reads=[self.pr[7]], writes=[r_b])
            pb = 0
            ev = 0
            for t4 in range(NT // 4):
                ht, hres = rh.next()
                kb.dma(kb.sp, ht[:], hTv[:, :, t4 * 512:(t4 + 1) * 512], writes=[hres])
                b = pb % 7
                pb += 1
                for kc in range(KC):
                    kb.op(kb.pe, lambda: nc.tensor.matmul(self.ps[b][0:8, :], lhsT=wg[:, kc, 64:72], rhs=ht[:, kc, :], start=(kc == 0), stop=(kc == KC - 1)),
                          reads=[r_wg, hres], writes=[self.pr[b]])
                kb.op(kb.act, lambda: nc.scalar.activation(out=etmp[:], in_=self.ps[b][0:8, :], func=ACT.Exp, bias=nb[:], scale=-1.0), reads=[self.pr[b], r_b], writes=[r_e])
                kb.op(kb.act, lambda: nc.scalar.activation(out=nlf[:, t4 * 512:(t4 + 1) * 512], in_=etmp[:], func=ACT.Ln, bias=self.one_t[0:8, :], scale=1.0), reads=[r_e], writes=[r_nlf])
                for (c0, h0, isk) in qk_blocks:
                    w, wr = rw.next()
                    kb.dma(kb.sp, w[:], wv[:, :, c0:c0 + 512], writes=[wr])
                    for hi in range(4):
                        b = pb % 7
                        pb += 1
                        for kc in range(KC):
                            kb.op(kb.pe, lambda: nc.tensor.matmul(self.ps[b][:, :], lhsT=w[:, kc, hi * 128:(hi + 1) * 128], rhs=ht[:, kc, :], start=(kc == 0), stop=(kc == KC - 1)),
                                  reads=[wr, hres], writes=[self.pr[b]])
                        stg, sr = rs.next()
                        if ev % 2 == 0:
                            kb.op(kb.act, lambda: nc.scalar.copy(stg[:], self.ps[b][:, :]), reads=[self.pr[b]], writes=[sr])
                        else:
                            kb.op(kb.dve, lambda: nc.vector.tensor_copy(stg[:], self.ps[b][:, :]), reads=[self.pr[b]], writes=[sr])
                        ev += 1
                        kb.dma(kb.pool, self.qkT[2 * (h0 + hi) + isk, :, t4 * 512:(t4 + 1) * 512], stg[:], reads=[sr])
                for (c0, h0) in v_blocks:
                    w, wr = rw.next()
                    kb.dma(kb.sp, w[:], wv[:, :, c0:c0 + 512], writes=[wr])
                    for tt in range(4):
                        j = t4 * 4 + tt
                        b = pb % 7
                        pb += 1
                        for kc in range(KC):
                            kb.op(kb.pe, lambda: nc.tensor.matmul(self.ps[b][:, :], lhsT=ht[:, kc, tt * 128:(tt + 1) * 128], rhs=w[:, kc, :], start=(kc == 0), stop=(kc == KC - 1)),
                                  reads=[wr, hres], writes=[self.pr[b]])
                        stg, sr = rs.next()
                        if ev % 2 == 0:
                            kb.op(kb.act, lambda: nc.scalar.copy(stg[:], self.ps[b][:, :]), reads=[self.pr[b]], writes=[sr])
                        else:
                            kb.op(kb.dve, lambda: nc.vector.tensor_copy(stg[:], self.ps[b][:, :]), reads=[self.pr[b]], writes=[sr])
                        ev += 1
                        kb.dma(kb.pool, self.vt[h0:h0 + 4, :, j, :].rearrange("h p d -> p h d"), stg[:].rearrange("p (h d) -> p h d", h=4), reads=[sr])
            Pt = T("iP", [8, S], F32)
            R = T("iR", [8, 8, 32], F32)
            r_P, r_R = Res(), Res()
            kb.op(kb.dve, lambda: nc.vector.tensor_tensor_scan(out=Pt[:], data0=self.one_t[0:8, 0:1].to_broadcast([8, S]), data1=nlf[:], initial=0.0, op0=ALU.mult, op1=ALU.add),
                  reads=[r_nlf], writes=[r_P])
            for j in range(NT):
                kb.op(kb.pe, lambda: nc.tensor.transpose(self.ps[0][:, j * 8:(j + 1) * 8], Pt[0:8, j * 128:(j + 1) * 128], self.ident_f(8)), reads=[r_P, self.r_cst], writes=[self.pr[0]])
            kb.op(kb.dve, lambda: nc.vector.tensor_copy(self.Pcol[:], self.ps[0][:, 0:256]), reads=[self.pr[0]], writes=[self.r_P])
            for h in range(8):
                kb.op(kb.dve, lambda: nc.vector.tensor_scalar(out=R[:, h, :], in0=Pt[0:8, 0:S:128], scalar1=self.cst_sb[0:8, C_ID + h:C_ID + h + 1], scalar2=None, op0=ALU.mult),
                      reads=[r_P, self.r_cst], writes=[r_R])
            kb.op(kb.pe, lambda: nc.tensor.matmul(self.ps[1][:, 0:256], lhsT=self.cst_sb[0:8, C_ONE:C_ONE + 128], rhs=R[:].rearrange("k h i -> k (h i)"), start=True, stop=True),
                  reads=[r_R, self.r_cst], writes=[self.pr[1]])
            kb.op(kb.dve, lambda: nc.vector.tensor_copy(self.Pb[:], self.ps[1][:, 0:256]), reads=[self.pr[1]], writes=[self.r_P])
            kb.dma(kb.pool, self.dbg[:, 0:256], self.Pcol[:], reads=[self.r_P])
            kb.dma(kb.pool, self.dbg[:, 256:512], self.Pb[:], reads=[self.r_P])

    def stage_attn0(self):
        kb, nc = self.kb, self.nc
        scale = HD ** -0.5
        import os
        heads = [int(v) for v in os.environ["ATTN_HEADS"].split(",")] if "ATTN_HEADS" in os.environ else range(16)
        with contextlib.ExitStack() as ctx:
            T = lambda name, shape, dty: ctx.enter_context(_sbt(nc, name, shape, dty))
            rq = Ring(ctx, nc, "aq", 2, [128, S], BF16)
            rk = Ring(ctx, nc, "ak", 2, [128, S], BF16)
            rnk = Ring(ctx, nc, "ank", 1, [128, S], BF16)
            rv = Ring(ctx, nc, "av", 2, [128, NT, 129], BF16)
            roT = Ring(ctx, nc, "aoT", 2, [128, S], BF16)
            rp = Ring(ctx, nc, "ap", 5, [128, 128], BF16)
            rsp = Ring(ctx, nc, "asp", 4, [128, 128], BF16)
            re_ = Ring(ctx, nc, "ae", 3, [128, 128], F32)
            rT = Ring(ctx, nc, "aT", 2, [128, 128], F32)
            rR = Ring(ctx, nc, "aR", 4, [128, 128], F32)
            rbias = Ring(ctx, nc, "ab", 5, [128, 32], F32)
            rosb = Ring(ctx, nc, "aosb", 3, [128, 128], BF16)
            rrd = Ring(ctx, nc, "ard", 3, [128, 1], F32)
            for vt_, vr_ in zip(rv.t, rv.r):
                kb.op(kb.pool, lambda: nc.gpsimd.memset(vt_[:, :, 128:129], 1.0), writes=[vr_])
            tri, trs = self.cb[:, C_TRI:C_TRI + 128], self.cb[:, C_TRS:C_TRS + 128]
            low, ones_b = self.cb[:, C_LOW:C_LOW + 128], self.cb[:, C_ONE:C_ONE + 128]
            Pcol = self.Pcol[:].rearrange("p (j h) -> p j h", h=8)
            Pb = self.Pb[:].rearrange("p (h i) -> p h i", i=32)
            sb_ = [0]
            xb_ = [0]
            for h in heads:
                fox = h < 8
                qT, qr = rq.next()
                kT, kr = rk.next()
                V, vr = rv.next()
                oTs, oTr = roT.next()
                kb.dma(kb.sp, qT[:], self.qkT[2 * h, :, :], writes=[qr])
                kb.dma(kb.sp, kT[:], self.qkT[2 * h + 1, :, :], writes=[kr])
                kb.dma(kb.sp, V[:, :, 0:128], self.vt[h, :, :, :], writes=[vr])
                if not fox:
                    nkT, nkr = rnk.next()
                    kb.op(kb.dve, lambda: nc.vector.tensor_scalar(out=nkT[:], in0=kT[:], scalar1=-scale, scalar2=None, op0=ALU.mult), reads=[kr], writes=[nkr])
                def finalize(i, normalize):
                    bO = 3 + (i % 2)
                    osb, osr = rosb.next()
                    if normalize:
                        rd, rdr = rrd.next()
                        kb.op(kb.dve, lambda: nc.vector.reciprocal(rd[:], self.ps[bO][:, 128:129]), reads=[self.pr[bO]], writes=[rdr])
                        kb.op(kb.act, lambda: nc.scalar.activation(out=osb[:], in_=self.ps[bO][:, 0:128], func=ACT.Copy, scale=rd[:, 0:1]), reads=[self.pr[bO], rdr], writes=[osr])
                    else:
                        kb.op(kb.act, lambda: nc.scalar.copy(osb[:], self.ps[bO][:, 0:128]), reads=[self.pr[bO]], writes=[osr])
                    pv = self.ps[5][:].bitcast(BF16)
                    kb.op(kb.pe, lambda: nc.tensor.transpose(pv[:, 0:128], osb[:], self.ident_b()), reads=[osr, self.r_cst], writes=[self.pr[5]])
                    kb.op(kb.dve, lambda: nc.vector.tensor_copy(oTs[:, i * 128:(i + 1) * 128], pv[:, 0:128]), reads=[self.pr[5]], writes=[oTr])

                if fox:
                    biases = {}

                    def f1(i, j):
                        if j == 0:
                            bias, br = rbias.next()
                            kb.op(kb.dve, lambda: nc.vector.tensor_scalar(out=bias[:], in0=Pcol[:, :, h], scalar1=Pb[:, h, i:i + 1], scalar2=None, op0=ALU.subtract),
                                  reads=[self.r_P], writes=[br])
                            biases[i] = (bias, br)
                        bS = sb_[0] % 3
                        sb_[0] += 1
                        kb.op(kb.pe, lambda: nc.tensor.matmul(self.ps[bS][:, 0:128], lhsT=kT[:, j * 128:(j + 1) * 128], rhs=qT[:, i * 128:(i + 1) * 128], start=True, stop=True),
                              reads=[kr, qr], writes=[self.pr[bS]])
                        return bS

                    def f2(i, j, bS):
                        bO = 3 + (i % 2)
                        bias, br = biases[i]
                        PT, pr_ = rp.next()
                        kb.op(kb.act, lambda: nc.scalar.activation(out=PT[:], in_=self.ps[bS][:, 0:128], func=ACT.Exp, bias=bias[:, j:j + 1], scale=scale),
                              reads=[self.pr[bS], br], writes=[pr_])
                        if j == i:
                            kb.op(kb.pool, lambda: nc.gpsimd.tensor_tensor(out=PT[:], in0=PT[:], in1=tri, op=ALU.mult), reads=[pr_, self.r_cst], writes=[pr_])
                        kb.op(kb.pe, lambda: nc.tensor.matmul(self.ps[bO][:, 0:129], lhsT=PT[:], rhs=V[:, j, :], start=(j == 0), stop=(j == i)),
                              reads=[pr_, vr], writes=[self.pr[bO]])
                        if j == i:
                            finalize(i, True)

                    pend = []
                    for i in range(NT):
                        for j in range(i + 1):
                            pend.append((i, j, f1(i, j)))
                            if len(pend) > 2:
                                f2(*pend.pop(0))
                    while pend:
                        f2(*pend.pop(0))
                else:
                    raccs = {}
                    sps = {}

                    def g1(i, idx, j, last):
                        bS = sb_[0] % 2
                        sb_[0] += 1
                        kb.op(kb.pe, lambda: nc.tensor.matmul(self.ps[bS][:, 0:128], lhsT=kT[:, j * 128:(j + 1) * 128], rhs=qT[:, i * 128:(i + 1) * 128], start=True, stop=True),
                              reads=[kr, qr], writes=[self.pr[bS]])
                        e, er = re_.next()
                        SP, spr = rsp.next()
                        kb.op(kb.act, lambda: nc.scalar.activation(out=e[:], in_=self.ps[bS][:, 0:128], func=ACT.Exp, scale=scale), reads=[self.pr[bS]], writes=[er])
                        kb.op(kb.act, lambda: nc.scalar.activation(out=SP[:], in_=e[:], func=ACT.Ln, bias=self.one_t[:], scale=1.0), reads=[er], writes=[spr])
                        if j == i:
                            kb.op(kb.pool, lambda: nc.gpsimd.tensor_tensor(out=SP[:], in0=SP[:], in1=trs, op=ALU.mult), reads=[spr, self.r_cst], writes=[spr])
                        sps[(i, idx)] = (SP, spr)

                    def g2(i, idx, j, last):
                        SP, spr = sps.pop((i, idx))
                        bX = (2, 6)[xb_[0] % 2]
                        xb_[0] += 1
                        kb.op(kb.pe, lambda: nc.tensor.matmul(self.ps[bX][:, 0:128], lhsT=low, rhs=SP[:], start=True, stop=False), reads=[spr, self.r_cst], writes=[self.pr[bX]])
                        kb.op(kb.pe, lambda: nc.tensor.matmul(self.ps[bX][:, 0:128], lhsT=nkT[:, j * 128:(j + 1) * 128], rhs=qT[:, i * 128:(i + 1) * 128], start=False, stop=True),
                              reads=[nkr, qr], writes=[self.pr[bX]])
                        A, ar = rp.next()
                        if idx == 0:
                            raccs[i] = rR.next()
                            kb.op(kb.act, lambda: nc.scalar.activation(out=A[:], in_=self.ps[bX][:, 0:128], func=ACT.Exp, scale=-1.0), reads=[self.pr[bX]], writes=[ar])
                        else:
                            Racc, rr = raccs[i]
                            Tt, tr_ = rT.next()
                            kb.op(kb.dve, lambda: nc.vector.tensor_tensor(out=Tt[:], in0=self.ps[bX][:, 0:128], in1=Racc[:], op=ALU.add), reads=[self.pr[bX], rr], writes=[tr_])
                            kb.op(kb.act, lambda: nc.scalar.activation(out=A[:], in_=Tt[:], func=ACT.Exp, scale=-1.0), reads=[tr_], writes=[ar])
                        if j == i:
                            kb.op(kb.pool, lambda: nc.gpsimd.tensor_tensor(out=A[:], in0=A[:], in1=trs, op=ALU.mult), reads=[ar, self.r_cst], writes=[ar])
                        if not last:
                            Racc, rr = raccs[i]
                            kb.op(kb.pe, lambda: nc.tensor.matmul(self.ps[7][:, 0:128], lhsT=ones_b, rhs=SP[:], start=True, stop=True), reads=[spr, self.r_cst], writes=[self.pr[7]])
                            if idx == 0:
                                kb.op(kb.dve, lambda: nc.vector.tensor_copy(Racc[:], self.ps[7][:, 0:128]), reads=[self.pr[7]], writes=[rr])
                            else:
                                kb.op(kb.dve, lambda: nc.vector.tensor_tensor(out=Racc[:], in0=self.ps[7][:, 0:128], in1=Racc[:], op=ALU.add), reads=[self.pr[7], rr], writes=[rr])
                        sps[("A", i, idx)] = (A, ar)

                    def g3(i, idx, j, last):
                        A, ar = sps.pop(("A", i, idx))
                        bO = 3 + (i % 2)
                        kb.op(kb.pe, lambda: nc.tensor.matmul(self.ps[bO][:, 0:128], lhsT=A[:], rhs=V[:, j, 0:128], start=(idx == 0), stop=last),
                              reads=[ar, vr], writes=[self.pr[bO]])
                        if last:
                            finalize(i, False)

                    stream = []
                    for i in range(NT):
                        js = [j for j in range(i, i - SB_WIN, -1) if j >= 0]
                        for idx, j in enumerate(js):
                            stream.append((i, idx, j, idx == len(js) - 1))
                    n = len(stream)
                    for k in range(n + 2):
                        if k < n:
                            g1(*stream[k])
                        if 0 <= k - 1 < n:
                            g2(*stream[k - 1])
                        if 0 <= k - 2 < n:
                            g3(*stream[k - 2])
                kb.dma(kb.pool, self.oT[h, :, :], oTs[:], reads=[oTr])

    def stage_inproj1(self):
        kb, nc = self.kb, self.nc
        hTv = self.hT.rearrange("k p t -> p k t")
        wv = self.wb_in1.rearrange("(k p) c -> p k c", p=128)
        with contextlib.ExitStack() as ctx:
            T = lambda name, shape, dty: ctx.enter_context(_sbt(nc, name, shape, dty))
            sinq, cosq = T("sinq", [128, NT, 64], F32), T("cosq", [128, NT, 64], F32)
            r_tab = Res()
            with contextlib.ExitStack() as c2:
                T2 = lambda name, shape, dty: c2.enter_context(_sbt(nc, name, shape, dty))
                pi32, pf32, post = T2("pi32", [32, 128], I32), T2("pf32", [32, 128], F32), T2("post", [128, 32], F32)
                ang, u, ki, kf, m = (T2("ang", [128, NT * 64], F32), T2("ru", [128, NT * 64], F32), T2("rki", [128, NT * 64], I32),
                                     T2("rkf", [128, NT * 64], F32), T2("rm", [128, NT * 64], F32))
                npi = T2("npi", [128, 1], F32)
                r = Res()
                kb.dma(kb.sp, pi32[:], self.pos[:, :], writes=[r])
                kb.op(kb.dve, lambda: nc.vector.memset(npi[:], -3.14159), writes=[r])
                kb.op(kb.dve, lambda: nc.vector.tensor_copy(pf32[:], pi32[:]), reads=[r], writes=[r])
                kb.op(kb.pe, lambda: nc.tensor.transpose(self.ps[0][:, 0:32], pf32[:], self.ident_f(32)), reads=[r, self.r_cst], writes=[self.pr[0]])
                kb.op(kb.dve, lambda: nc.vector.tensor_copy(post[:], self.ps[0][:, 0:32]), reads=[self.pr[0]], writes=[r])
                for j in range(NT):
                    kb.op(kb.dve, lambda: nc.vector.tensor_scalar(out=ang[:, j * 64:(j + 1) * 64], in0=self.cst_sb[:, C_INV:C_INV + 64], scalar1=post[:, j:j + 1], scalar2=None, op0=ALU.mult),
                          reads=[r, self.r_cst], writes=[r])
                for tab, shift in ((sinq, 0.5), (cosq, 0.75)):
                    V = nc.vector
                    kb.op(kb.dve, lambda: V.tensor_scalar(out=u[:], in0=ang[:], scalar1=1.0 / (2 * np.pi), scalar2=shift, op0=ALU.mult, op1=ALU.add), reads=[r], writes=[r])
                    kb.op(kb.dve, lambda: V.tensor_copy(ki[:], u[:]), reads=[r], writes=[r])
                    kb.op(kb.dve, lambda: V.tensor_copy(kf[:], ki[:]), reads=[r], writes=[r])
                    kb.op(kb.dve, lambda: V.tensor_tensor(out=u[:], in0=u[:], in1=kf[:], op=ALU.subtract), reads=[r], writes=[r])
                    kb.op(kb.dve, lambda: V.tensor_scalar(out=m[:], in0=u[:], scalar1=0.0, scalar2=None, op0=ALU.is_lt), reads=[r], writes=[r])
                    kb.op(kb.dve, lambda: V.tensor_tensor(out=u[:], in0=u[:], in1=m[:], op=ALU.add), reads=[r], writes=[r])
                    kb.op(kb.dve, lambda: V.tensor_scalar(out=m[:], in0=u[:], scalar1=1.0, scalar2=None, op0=ALU.is_ge), reads=[r], writes=[r])
                    kb.op(kb.dve, lambda: V.tensor_tensor(out=u[:], in0=u[:], in1=m[:], op=ALU.subtract), reads=[r], writes=[r])
                    kb.op(kb.act, lambda: nc.scalar.activation(out=tab[:].rearrange("p j k -> p (j k)"), in_=u[:], func=ACT.Sin, bias=npi[:], scale=6.28318), reads=[r], writes=[r_tab])
                kb.barrier()
            rh = Ring(ctx, nc, "jh", 2, [128, KC, 512], BF16)
            rw = Ring(ctx, nc, "jw", 3, [128, KC, 512], BF16)
            qtile = [T(f"jq{tt}", [128, 16, 128], BF16) for tt in range(4)]
            qres = [Res() for _ in range(4)]
            rta = Ring(ctx, nc, "jta", 2, [128, 512], F32)
            rtb = Ring(ctx, nc, "jtb", 2, [128, 512], F32)
            rqT = Ring(ctx, nc, "jqT", 2, [128, 16, 512], BF16)
            rqiT = Ring(ctx, nc, "jqiT", 2, [128, 8, 512], BF16)
            rkT = Ring(ctx, nc, "jkT", 2, [128, 512], BF16)
            rkiT = Ring(ctx, nc, "jkiT", 2, [128, 512], BF16)
            rkr = Ring(ctx, nc, "jkr", 2, [128, 128], BF16)
            rvs = Ring(ctx, nc, "jvs", 2, [128, 128], BF16)
            rws = Ring(ctx, nc, "jws", 2, [128, 16], F32)
            pbank = [0]
            tbank = [0]
            evc = [0]

            def proj(ht, hres, w, wr, tt, ncols):
                b = pbank[0] % 6
                pbank[0] += 1
                for kc in range(KC):
                    kb.op(kb.pe, lambda: nc.tensor.matmul(self.ps[b][:, 0:ncols], lhsT=ht[:, kc, tt * 128:(tt + 1) * 128], rhs=w[:, kc, 0:ncols], start=(kc == 0), stop=(kc == KC - 1)),
                          reads=[wr, hres], writes=[self.pr[b]])
                return b

            def rope(b, c0, nh, half, j, dst, dres):
                x = self.ps[b][:, c0:c0 + nh * 2 * half].rearrange("p (h two d) -> p h two d", h=nh, two=2)
                x1, x2 = x[:, :, 0, :], x[:, :, 1, :]
                st = 64 // half
                cs = cosq[:, j, 0:64:st].unsqueeze(1).to_broadcast([128, nh, half])
                sn = sinq[:, j, 0:64:st].unsqueeze(1).to_broadcast([128, nh, half])
                ta, tar = rta.next()
                tb, tbr = rtb.next()
                av = ta[:, 0:nh * half].rearrange("p (h d) -> p h d", h=nh)
                bv = tb[:, 0:nh * half].rearrange("p (h d) -> p h d", h=nh)
                V = nc.vector
                kb.op(kb.dve, lambda: V.tensor_tensor(out=av, in0=x1, in1=cs, op=ALU.mult), reads=[self.pr[b], r_tab], writes=[tar])
                kb.op(kb.dve, lambda: V.tensor_tensor(out=bv, in0=x2, in1=sn, op=ALU.mult), reads=[self.pr[b], r_tab], writes=[tbr])
                kb.op(kb.pool, lambda: nc.gpsimd.tensor_tensor(out=dst[:, :, 0:half], in0=av, in1=bv, op=ALU.subtract), reads=[tar, tbr], writes=[dres])
                ta, tar = rta.next()
                tb, tbr = rtb.next()
                av = ta[:, 0:nh * half].rearrange("p (h d) -> p h d", h=nh)
                bv = tb[:, 0:nh * half].rearrange("p (h d) -> p h d", h=nh)
                kb.op(kb.dve, lambda: V.tensor_tensor(out=av, in0=x2, in1=cs, op=ALU.mult), reads=[self.pr[b], r_tab], writes=[tar])
                kb.op(kb.dve, lambda: V.tensor_tensor(out=bv, in0=x1, in1=sn, op=ALU.mult), reads=[self.pr[b], r_tab], writes=[tbr])
                kb.op(kb.pool, lambda: nc.gpsimd.tensor_tensor(out=dst[:, :, half:2 * half], in0=av, in1=bv, op=ALU.add), reads=[tar, tbr], writes=[dres])

            def transp(src_list, sres, dst_fn, dres):
                k = 0
                while k < len(src_list):
                    grp = src_list[k:k + 8]
                    b = 6 + tbank[0] % 2
                    tbank[0] += 1
                    pv = self.ps[b][:].bitcast(BF16)
                    for g, src in enumerate(grp):
                        kb.op(kb.pe, lambda: nc.tensor.transpose(pv[:, g * 128:(g + 1) * 128], src, self.ident_b()), reads=[sres, self.r_cst], writes=[self.pr[b]])
                    for g, src in enumerate(grp):
                        if evc[0] % 2 == 0:
                            kb.op(kb.act, lambda: nc.scalar.copy(dst_fn(k + g), pv[:, g * 128:(g + 1) * 128]), reads=[self.pr[b]], writes=[dres])
                        else:
                            kb.op(kb.dve, lambda: nc.vector.tensor_copy(dst_fn(k + g), pv[:, g * 128:(g + 1) * 128]), reads=[self.pr[b]], writes=[dres])
                        evc[0] += 1
                    k += 8

            for t4 in range(NT // 4):
                ht, hres = rh.next()
                kb.dma(kb.sp, ht[:], hTv[:, :, t4 * 512:(t4 + 1) * 512], writes=[hres])
                qTs, qTr = rqT.next()
                qiTs, qiTr = rqiT.next()
                kTs, kTr = rkT.next()
                kiTs, kiTr = rkiT.next()
                for cbk in range(4):
                    w, wr = rw.next()
                    kb.dma(kb.sp, w[:], wv[:, :, cbk * 512:(cbk + 1) * 512], writes=[wr])
                    for tt in range(4):
                        j = t4 * 4 + tt
                        b = proj(ht, hres, w, wr, tt, 512)
                        rope(b, 0, 4, 64, j, qtile[tt][:, cbk * 4:(cbk + 1) * 4, :], qres[tt])
                for tt in range(4):
                    transp([qtile[tt][:, h, :] for h in range(16)], qres[tt], lambda h: qTs[:, h, tt * 128:(tt + 1) * 128], qTr)
                w, wr = rw.next()
                kb.dma(kb.sp, w[:, :, 0:256], wv[:, :, 2048:2304], writes=[wr])
                for tt in range(4):
                    j = t4 * 4 + tt
                    b = proj(ht, hres, w, wr, tt, 256)
                    kr_, krr = rkr.next()
                    rope(b, 0, 1, 64, j, kr_[:].rearrange("p (h d) -> p h d", h=1), krr)
                    vs, vsr = rvs.next()
                    kb.op(kb.act, lambda: nc.scalar.copy(vs[:], self.ps[b][:, 128:256]), reads=[self.pr[b]], writes=[vsr])
                    kb.dma(kb.pool, self.v1d[:, j, :], vs[:], reads=[vsr])
                    transp([kr_[:]], krr, lambda h: kTs[:, tt * 128:(tt + 1) * 128], kTr)
                for cbk in range(2):
                    w, wr = rw.next()
                    kb.dma(kb.sp, w[:], wv[:, :, 2304 + cbk * 512:2304 + (cbk + 1) * 512], writes=[wr])
                    for tt in range(4):
                        j = t4 * 4 + tt
                        b = proj(ht, hres, w, wr, tt, 512)
                        qv = qtile[tt][:].rearrange("p a b -> p (a b)")[:, cbk * 512:(cbk + 1) * 512].rearrange("p (h d) -> p h d", h=8)
                        rope(b, 0, 8, 32, j, qv, qres[tt])
                for tt in range(4):
                    flat = qtile[tt][:].rearrange("p a b -> p (a b)")
                    transp([flat[:, pr * 128:(pr + 1) * 128] for pr in range(8)], qres[tt], lambda pr: qiTs[:, pr, tt * 128:(tt + 1) * 128], qiTr)
                w, wr = rw.next()
                kb.dma(kb.sp, w[:, :, 0:80], wv[:, :, 3328:3408], writes=[wr])
                for tt in range(4):
                    j = t4 * 4 + tt
                    b = proj(ht, hres, w, wr, tt, 80)
                    kr_, krr = rkr.next()
                    rope(b, 0, 1, 32, j, kr_[:, 0:64].rearrange("p (h d) -> p h d", h=1), krr)
                    kb.op(kb.pool, lambda: nc.gpsimd.tensor_copy(kr_[:, 64:128], kr_[:, 0:64]), reads=[krr], writes=[krr])
                    ws, wsr = rws.next()
                    kb.op(kb.act, lambda: nc.scalar.copy(ws[:], self.ps[b][:, 64:80]), reads=[self.pr[b]], writes=[wsr])
                    kb.dma(kb.pool, self.wd[:, j, :], ws[:], reads=[wsr])
                    transp([kr_[:]], krr, lambda h: kiTs[:, tt * 128:(tt + 1) * 128], kiTr)
                sl = slice(t4 * 512, (t4 + 1) * 512)
                kb.dma(kb.pool, self.qT1.rearrange("h p t -> p h t")[:, :, sl], qTs[:], reads=[qTr])
                kb.dma(kb.pool, self.qiT1.rearrange("h p t -> p h t")[:, :, sl], qiTs[:], reads=[qiTr])
                kb.dma(kb.pool, self.kT1d[:, sl], kTs[:], reads=[kTr])
                kb.dma(kb.pool, self.kiT2d[:, sl], kiTs[:], reads=[kiTr])

    def stage_dsa(self):
        kb, nc = self.kb, self.nc
        scale = HD ** -0.5
        import os
        ntiles = int(os.environ.get("DSA_TILES", NT))
        with contextlib.ExitStack() as ctx:
            T = lambda name, shape, dty: ctx.enter_context(_sbt(nc, name, shape, dty))
            kT1, kiT2 = T("kT1", [128, S], BF16), T("kiT2", [128, S], BF16)
            V1, wsb = T("V1", [128, NT, 129], BF16), T("wsb", [128, NT, 16], F32)
            id4 = T("id4", [128, 512], BF16)
            r_k = Res()
            kb.dma(kb.sp, kT1[:], self.kT1d[:, :], writes=[r_k])
            kb.dma(kb.sp, kiT2[:], self.kiT2d[:, :], writes=[r_k])
            kb.dma(kb.sp, V1[:, :, 0:128], self.v1d[:, :, :], writes=[r_k])
            kb.dma(kb.sp, wsb[:], self.wd[:, :, :], writes=[r_k])
            kb.op(kb.pool, lambda: nc.gpsimd.memset(V1[:, :, 128:129], 1.0), writes=[r_k])
            for g in range(4):
                kb.op(kb.pool, lambda: nc.gpsimd.tensor_copy(id4[:, g * 128:(g + 1) * 128], self.ident_b()), reads=[self.r_cst], writes=[r_k])
            rI = Ring(ctx, nc, "dI", 2, [128, S], F32)
            rWk = Ring(ctx, nc, "dWk", 2, [128, S], F32)
            rNM = Ring(ctx, nc, "dNM", 4, [128, S], BF16)
            rqi = Ring(ctx, nc, "dqi", 2, [128, 8, 128], BF16)
            rq = Ring(ctx, nc, "dq", 4, [128, 16, 128], BF16)
            rWd = Ring(ctx, nc, "dWd", 2, [128, 16, 128], BF16)
            rR = Ring(ctx, nc, "dR", 4, [128, 512], BF16)
            rPT = Ring(ctx, nc, "dPT", 4, [128, 512], BF16)
            rosb = Ring(ctx, nc, "dosb", 2, [128, 16, 128], BF16)
            roT = Ring(ctx, nc, "doT", 1, [128, 16, 512], BF16)
            rm8 = Ring(ctx, nc, "dm8", 4, [128, 8], F32)
            rthr = Ring(ctx, nc, "dthr", 4, [128, 1], F32)
            rrd = Ring(ctx, nc, "drd", 4, [128, 2], F32)
            ib_ = [0]
            sb_ = [0]
            tiles = {}
            oT_cur = [None, None]

            def phase_a_index(i):
                n_i = 128 * (i + 1)
                qiT, qir = rqi.next()
                qT, qr = rq.next()
                Wd, wdr = rWd.next()
                Isb, Ir = rI.next()
                kb.dma(kb.sp, qiT[:], self.qiT1.rearrange("h p t -> p h t")[:, :, i * 128:(i + 1) * 128], writes=[qir])
                kb.dma(kb.sp, qT[:], self.qT1.rearrange("h p t -> p h t")[:, :, i * 128:(i + 1) * 128], writes=[qr])
                for h in range(16):
                    kb.op(kb.pool, lambda: nc.gpsimd.tensor_scalar(out=Wd[:, h, :], in0=self.ident_b(), scalar1=wsb[:, i, h:h + 1], scalar2=0.25 * 0.125, op0=ALU.mult, op1=ALU.mult),
                          reads=[r_k, self.r_cst], writes=[wdr])

                def i1(sb, h):
                    nco = min(512, n_i - 512 * sb)
                    hp, pr = h % 2, h // 2
                    bI = ib_[0] % 3
                    ib_[0] += 1
                    kb.op(kb.pe, lambda: nc.tensor.matmul(self.ps[bI][:, 0:nco], lhsT=qiT[hp * 64:(hp + 1) * 64, pr, :], rhs=kiT2[hp * 64:(hp + 1) * 64, sb * 512:sb * 512 + nco], start=True, stop=True),
                          reads=[qir, r_k], writes=[self.pr[bI]])
                    return bI

                def i2(sb, h, bI):
                    nco = min(512, n_i - 512 * sb)
                    bA = 3 + (sb % 2)
                    R, rr = rR.next()
                    kb.op(kb.act, lambda: nc.scalar.activation(out=R[:, 0:nco], in_=self.ps[bI][:, 0:nco], func=ACT.Relu), reads=[self.pr[bI]], writes=[rr])
                    kb.op(kb.pe, lambda: nc.tensor.matmul(self.ps[bA][:, 0:nco], lhsT=Wd[:, h, :], rhs=R[:, 0:nco], start=(h == 0), stop=(h == 15)),
                          reads=[wdr, rr], writes=[self.pr[bA]])
                    if h == 15:
                        kb.op(kb.act, lambda: nc.scalar.copy(Isb[:, sb * 512:sb * 512 + nco], self.ps[bA][:, 0:nco]), reads=[self.pr[bA]], writes=[Ir])

                pend = []
                for sb in range((n_i + 511) // 512):
                    for h in range(16):
                        pend.append((sb, h, i1(sb, h)))
                        if len(pend) > 2:
                            i2(*pend.pop(0))
                while pend:
                    i2(*pend.pop(0))
                kb.op(kb.dve, lambda: nc.vector.memset(Isb[0:64, n_i - 64:n_i], NEG), writes=[Ir])
                tiles[i] = dict(qT=qT, qr=qr, Isb=Isb, Ir=Ir, n=n_i)

            def phase_a_topk(group):
                chains = []
                for i in group:
                    t = tiles[i]
                    thr, thr_r = rthr.next()
                    t["thr"], t["thr_r"] = thr, thr_r
                    if t["n"] <= 256:
                        kb.op(kb.dve, lambda: nc.vector.memset(thr[:], -1.0e29), writes=[thr_r])
                    else:
                        Wk, wkr = rWk.next()
                        chains.append(dict(t=t, cur=t["Isb"], cur_r=t["Ir"], Wk=Wk, wkr=wkr))
                for rnd in range(32):
                    for c in chains:
                        t, n_i = c["t"], c["t"]["n"]
                        m8, m8r = rm8.next()
                        c["m8"], c["m8r"] = m8, m8r
                        cur, cur_r = c["cur"], c["cur_r"]
                        kb.op(kb.dve, lambda: nc.vector.max(out=m8[:], in_=cur[:, 0:n_i]), reads=[cur_r], writes=[m8r])
                    for c in chains:
                        t, n_i = c["t"], c["t"]["n"]
                        m8, m8r, cur, cur_r, Wk, wkr = c["m8"], c["m8r"], c["cur"], c["cur_r"], c["Wk"], c["wkr"]
                        if rnd < 31:
                            kb.op(kb.dve, lambda: nc.vector.match_replace(out=Wk[:, 0:n_i], in_to_replace=m8[:], in_values=cur[:, 0:n_i], imm_value=NEG), reads=[cur_r, m8r], writes=[wkr])
                            c["cur"], c["cur_r"] = Wk, wkr
                        else:
                            kb.op(kb.dve, lambda: nc.vector.tensor_copy(t["thr"][:], m8[:, 7:8]), reads=[m8r], writes=[t["thr_r"]])
                for i in group:
                    t = tiles[i]
                    NM, nmr = rNM.next()
                    kb.op(kb.dve, lambda: nc.vector.tensor_scalar(out=NM[:, 0:t["n"]], in0=t["Isb"][:, 0:t["n"]], scalar1=t["thr"][:, 0:1], scalar2=-30000.0, op0=ALU.is_lt, op1=ALU.mult),
                          reads=[t["Ir"], t["thr_r"]], writes=[nmr])
                    t["NM"], t["nmr"] = NM, nmr

            def phase_b(i):
                t = tiles.pop(i)
                qT, qr, NM, nmr = t["qT"], t["qr"], t["NM"], t["nmr"]
                tt = i % 4
                if tt == 0:
                    oT_cur[0], oT_cur[1] = roT.next()
                oTs, oTr = oT_cur
                osb, osr = rosb.next()

                def b1(hg, j):
                    bS = (0, 1, 6)[sb_[0] % 3]
                    sb_[0] += 1
                    kb.op(kb.pe, lambda: nc.tensor.matmul(self.ps[bS][:, :], lhsT=kT1[:, j * 128:(j + 1) * 128], rhs=qT[:, hg * 4:(hg + 1) * 4, :].rearrange("p h t -> p (h t)"), start=True, stop=False),
                          reads=[r_k, qr], writes=[self.pr[bS]])
                    kb.op(kb.pe, lambda: nc.tensor.matmul(self.ps[bS][:, :], lhsT=NM[:, j * 128:(j + 1) * 128], rhs=id4[:], start=False, stop=True),
                          reads=[nmr, r_k], writes=[self.pr[bS]])
                    return bS

                def b2(hg, j, bS):
                    PT, ptr = rPT.next()
                    kb.op(kb.act, lambda: nc.scalar.activation(out=PT[:], in_=self.ps[bS][:, :], func=ACT.Exp, scale=scale), reads=[self.pr[bS]], writes=[ptr])
                    for hh in range(4):
                        bO = 2 + hh
                        kb.op(kb.pe, lambda: nc.tensor.matmul(self.ps[bO][:, 0:129], lhsT=PT[:, hh * 128:(hh + 1) * 128], rhs=V1[:, j, :], start=(j == 0), stop=(j == i)),
                              reads=[ptr, r_k], writes=[self.pr[bO]])
                    if j == i:
                        for hh in range(4):
                            bO = 2 + hh
                            rd, rdr = rrd.next()
                            kb.op(kb.act, lambda: nc.scalar.activation(out=rd[:, 0:1], in_=self.ps[bO][:, 128:129], func=ACT.Ln), reads=[self.pr[bO]], writes=[rdr])
                            kb.op(kb.act, lambda: nc.scalar.activation(out=rd[:, 1:2], in_=rd[:, 0:1], func=ACT.Exp, scale=-1.0), reads=[rdr], writes=[rdr])
                            kb.op(kb.act, lambda: nc.scalar.activation(out=osb[:, hg * 4 + hh, :], in_=self.ps[bO][:, 0:128], func=ACT.Copy, scale=rd[:, 1:2]), reads=[self.pr[bO], rdr], writes=[osr])

                pend = []
                for hg in range(4):
                    for j in range(i + 1):
                        pend.append((hg, j, b1(hg, j)))
                        if len(pend) > 2:
                            b2(*pend.pop(0))
                while pend:
                    b2(*pend.pop(0))
                for half in range(2):
                    pv = self.ps[7][:].bitcast(BF16)
                    for g in range(8):
                        kb.op(kb.pe, lambda: nc.tensor.transpose(pv[:, g * 128:(g + 1) * 128], osb[:, half * 8 + g, :], self.ident_b()), reads=[osr, self.r_cst], writes=[self.pr[7]])
                    for g in range(8):
                        kb.op(kb.act, lambda: nc.scalar.copy(oTs[:, half * 8 + g, tt * 128:(tt + 1) * 128], pv[:, g * 128:(g + 1) * 128]), reads=[self.pr[7]], writes=[oTr])
                if tt == 3 or i == ntiles - 1:
                    t4 = i // 4
                    kb.dma(kb.pool, self.oT.rearrange("k p t -> p k t")[:, :, t4 * 512:(t4 + 1) * 512], oTs[:], reads=[oTr])

            groups = [list(range(g, min(g + 2, ntiles))) for g in range(0, ntiles, 2)]
            for gi, grp in enumerate(groups):
                for i in grp:
                    phase_a_index(i)
                phase_a_topk(grp)
                if gi > 0:
                    for i in groups[gi - 1]:
                        phase_b(i)
            for i in groups[-1]:
                phase_b(i)


def make_in_maps(inputs):
    f = lambda a: np.ascontiguousarray(a)
    cst = make_consts()
    maps = []
    for b in range(8):
        maps.append({
            "x": f(inputs["x"][b]), "c": f(inputs["c"][b].reshape(16, 128)),
            "pos": f(inputs["positions"][b].reshape(32, 128).astype(np.int32)),
            "ada_w": inputs["ada_w"], "ada_b": inputs["ada_b"], "norm_g": inputs["norm_g"],
            "mix_w_out": inputs["mix_w_out"], "even_w_in": f(inputs["even_w_in"][0]),
            "even_b": f(inputs["even_b_forget"].reshape(1, 8)), "odd_w_in": f(inputs["odd_w_in"][0]),
            "ff_w1": inputs["ff_w1"], "ff_w2": inputs["ff_w2"], "cst": cst,
        })
    return maps


def kernel(**inputs):
    inputs = {k: np.asarray(v) for k, v in inputs.items()}
    prog = Prog()
    res = run_bass_kernel_spmd(prog.nc, make_in_maps(inputs), core_ids=list(range(8)))
    return np.stack([r["out"] for r in res.results], axis=0).astype(np.float32)
```

```python
import contextlib
import numpy as np
import concourse.bass as bass
import concourse.mybir as mybir
from concourse.bass_utils import run_bass_kernel_spmd

ACT = mybir.ActivationFunctionType
ALU = mybir.AluOpType
AX = mybir.AxisListType
F32, BF16, I32 = mybir.dt.float32, mybir.dt.bfloat16, mybir.dt.int32

S, D, DFF, HD = 4096, 2048, 8192, 128
NT = S // 128
KC = D // 128
EVEN_W, ODD_W = 6152, 3408
EPS = 1e-6
NEG = -1.0e30
SB_WIN = 3
NSLOT = 8
CAST_OVERLAP = True

C_ID, C_TRI, C_TRS, C_LOW, C_ONE, C_INV = 0, 128, 256, 384, 512, 640
C_W = 704


def make_consts():
    c = np.zeros((128, C_W), np.float32)
    a = np.arange(128)
    c[:, C_ID:C_ID + 128] = np.eye(128)
    c[:, C_TRI:C_TRI + 128] = (a[:, None] <= a[None, :])
    c[:, C_TRS:C_TRS + 128] = (a[:, None] < a[None, :])
    c[:, C_LOW:C_LOW + 128] = (a[:, None] >= a[None, :])
    c[:, C_ONE:C_ONE + 128] = 1.0
    inv = (10000.0 ** (-np.arange(64, dtype=np.float32) / 64)).astype(np.float32)
    c[:, C_INV:C_INV + 64] = inv[None, :]
    return c


_uid = [0]


def _sbt(nc, name, shape, dty):
    _uid[0] += 1
    return nc.sbuf_tensor(f"{name}_u{_uid[0]}", shape, dty)


class Res:
    __slots__ = ("w", "r", "x")

    def __init__(self, x=False):
        self.w = None
        self.r = {}
        self.x = x


class Eng:
    def __init__(self, name, obj, sem, is_pe=False):
        self.name, self.obj, self.sem, self.is_pe = name, obj, sem, is_pe
        self.n = 0
        self.waited = {}
        self.dma_sems, self.dma_cnt, self.dma_i = [], [], 0


class KB:
    def __init__(self, nc):
        self.nc = nc
        self.stack = contextlib.ExitStack()
        mk = lambda nm: self.stack.enter_context(nc.semaphore(nm))
        self.pe = Eng("pe", nc.tensor, mk("s_pe"), True)
        self.act = Eng("act", nc.scalar, mk("s_act"))
        self.dve = Eng("dve", nc.vector, mk("s_dve"))
        self.pool = Eng("pool", nc.gpsimd, mk("s_pool"))
        self.sp = Eng("sp", nc.sync, mk("s_sp"))
        self.engs = [self.pe, self.act, self.dve, self.pool, self.sp]
        self.queues = [self.sp, self.pool, self.act]
        for q in self.queues:
            q.dma_sems = [mk(f"d_{q.name}{i}") for i in range(NSLOT)]
            q.dma_cnt = [0] * NSLOT
        self.n_ins = 0

    def _wait(self, eng, tk):
        sem, val, src = tk
        if src is eng and eng.is_pe:
            return
        key = sem.name
        if eng.waited.get(key, 0) >= val:
            return
        eng.obj.wait_ge(sem, val)
        eng.waited[key] = val

    def _deps(self, eng, reads, writes):
        for r in reads:
            if r.w is not None:
                self._wait(eng, r.w)
            if r.x:
                for k, tk in r.r.items():
                    if k != eng.name:
                        self._wait(eng, tk)
        for w in writes:
            if w.w is not None:
                self._wait(eng, w.w)
            for tk in w.r.values():
                self._wait(eng, tk)

    def _mark(self, key, tk, reads, writes):
        for r in reads:
            r.r[key] = tk
        for w in writes:
            w.w = tk
            w.r = {}

    def op(self, eng, fn, reads=(), writes=()):
        self._deps(eng, reads, writes)
        ins = fn()
        eng.n += 1
        ins.then_inc(eng.sem, 1)
        tk = (eng.sem, eng.n, eng)
        self._mark(eng.name, tk, reads, writes)
        self.n_ins += 1
        return tk

    def dma(self, q, out, in_, reads=(), writes=()):
        slot = q.dma_i % NSLOT
        q.dma_i += 1
        sem = q.dma_sems[slot]
        if q.dma_cnt[slot] > 0:
            self._wait(q, (sem, 16 * q.dma_cnt[slot], None))
        self._deps(q, reads, writes)
        q.obj.dma_start(out=out, in_=in_).then_inc(sem, 16)
        q.dma_cnt[slot] += 1
        tk = (sem, 16 * q.dma_cnt[slot], None)
        self._mark(sem.name, tk, reads, writes)
        self.n_ins += 1
        return tk

    def barrier(self):
        for e in self.engs:
            for f in self.engs:
                if f is not e and f.n > 0:
                    self._wait(e, (f.sem, f.n, f))
            for q in self.queues:
                for i in range(NSLOT):
                    if q.dma_cnt[i] > 0:
                        self._wait(e, (q.dma_sems[i], 16 * q.dma_cnt[i], None))


class Ring:
    def __init__(self, ctx, nc, name, n, shape, dtype):
        self.t = [ctx.enter_context(_sbt(nc, f"{name}{i}", shape, dtype)) for i in range(n)]
        self.r = [Res() for _ in range(n)]
        self.i = 0

    def next(self):
        k = self.i % len(self.t)
        self.i += 1
        return self.t[k], self.r[k]


class Prog:
    def __init__(self, stages=None, xsrc_is_out=False):
        self.stages = stages
        nc = self.nc = bass.Bass("TRN2", target_bir_lowering=False)
        kb = self.kb = KB(nc)
        import os
        dbg = set(os.environ.get("DEBUG_OUT", "").split(","))
        dt = lambda name, shape, dty, kind: nc.dram_tensor(name, shape, dty, kind=("ExternalOutput" if name in dbg else kind)).ap()
        I, O, N = "ExternalInput", "ExternalOutput", "Internal"
        self.x = dt("x", [S, D], F32, I)
        self.c = dt("c", [16, 128], F32, I)
        self.pos = dt("pos", [32, 128], I32, I)
        self.ada_w = dt("ada_w", [2, D, 6 * D], F32, I)
        self.ada_b = dt("ada_b", [2, 6 * D], F32, I)
        self.norm_g = dt("norm_g", [2, 4, D], F32, I)
        self.mix_w_out = dt("mix_w_out", [2, D, D], F32, I)
        self.even_w_in = dt("even_w_in", [D, EVEN_W], F32, I)
        self.even_b = dt("even_b", [1, 8], F32, I)
        self.odd_w_in = dt("odd_w_in", [D, ODD_W], F32, I)
        self.ff_w1 = dt("ff_w1", [2, D, DFF], F32, I)
        self.ff_w2 = dt("ff_w2", [2, DFF, D], F32, I)
        self.cst = dt("cst", [128, C_W], F32, I)
        self.out = dt("out", [S, D], F32, O)
        self.wb_in0 = dt("wb_in0", [D, EVEN_W], BF16, N)
        self.wb_in1 = dt("wb_in1", [D, ODD_W], BF16, N)
        self.wb_out = [dt(f"wb_out{l}", [D, D], BF16, N) for l in range(2)]
        self.wb_f1 = [dt(f"wb_f1{l}", [D, DFF], BF16, N) for l in range(2)]
        self.wb_f2 = [dt(f"wb_f2{l}", [DFF, D], BF16, N) for l in range(2)]
        self.modd = dt("modd", [2, 6 * D], F32, N)
        self.hT = dt("hT", [KC, 128, S], BF16, N)
        self.oT = dt("oT", [KC, 128, S], BF16, N)
        self.qkT = dt("qkT", [32, 128, S], BF16, N)
        self.vt = dt("vt", [16, 128, NT, 128], BF16, N)
        self.qT1 = dt("qT1", [16, 128, S], BF16, N)
        self.qiT1 = dt("qiT1", [8, 128, S], BF16, N)
        self.dbg = dt("dbg", [128, 512], F32, N)
        self.kT1d = dt("kT1d", [128, S], BF16, N)
        self.kiT2d = dt("kiT2d", [128, S], BF16, N)
        self.v1d = dt("v1d", [128, NT, 128], BF16, N)
        self.wd = dt("wd", [128, NT, 16], F32, N)
        self.ps = [nc.alloc_psum_tensor(f"ps{b}", [128, 512], F32) for b in range(8)]
        self.pr = [Res(True) for _ in range(8)]
        A = nc.alloc_sbuf_tensor
        self.cst_sb = A("cst_sb", [128, C_W], F32)
        self.cb = A("cb", [128, 640], BF16)
        self.eps_t = A("eps_t", [128, 1], F32)
        self.one_t = A("one_t", [128, 1], F32)
        self.zero_t = A("zero_t", [128, 1], F32)
        self.acol = A("acol", [128, 16], F32)
        self.bcol = A("bcol", [128, 16], F32)
        self.G = A("G", [128, D], F32)
        self.Pcol = A("Pcol", [128, 256], F32)
        self.Pb = A("Pb", [128, 256], F32)
        self.r_cst, self.r_cols, self.r_G, self.r_P = Res(), Res(), Res(), Res()
        self.build()

    def ident_f(self, n=128):
        return self.cst_sb[0:n, C_ID:C_ID + n]

    def ident_b(self):
        return self.cb[:, C_ID:C_ID + 128]

    def on(self, name):
        return self.stages is None or name in self.stages

    def build(self):
        kb, nc = self.kb, self.nc
        kb.dma(kb.sp, self.cst_sb[:], self.cst[:, :], writes=[self.r_cst])
        kb.op(kb.dve, lambda: nc.vector.tensor_copy(self.cb[:], self.cst_sb[:, 0:640]), reads=[self.r_cst], writes=[self.r_cst])
        kb.op(kb.dve, lambda: nc.vector.memset(self.eps_t[:], EPS), writes=[self.r_cst])
        kb.op(kb.dve, lambda: nc.vector.memset(self.one_t[:], 1.0), writes=[self.r_cst])
        kb.op(kb.dve, lambda: nc.vector.memset(self.zero_t[:], 0.0), writes=[self.r_cst])
        kb.barrier()
        if self.on("cast"):
            self.stage_cast()
            kb.barrier()
        if self.on("ada"):
            self.stage_ada()
            kb.barrier()
        xsrc = self.x
        for l in range(2):
            if self.on(f"mix{l}"):
                self.stage_normT(l, "a", xsrc)
                kb.barrier()
                if l == 0:
                    self.stage_inproj0()
                    kb.barrier()
                    self.stage_attn0()
                    kb.barrier()
                else:
                    self.stage_inproj1()
                    kb.barrier()
                    self.stage_dsa()
                    kb.barrier()
                self.stage_outproj(l, xsrc)
                kb.barrier()
                xsrc = self.out
            if self.on(f"ffn{l}"):
                self.stage_normT(l, "m", xsrc)
                kb.barrier()
                if self.stages is None or "skipffn" not in self.stages:
                    self.stage_ffn(l, xsrc)
                    kb.barrier()
                    xsrc = self.out
        kb.barrier()

    def stage_cast(self):
        kb, nc = self.kb, self.nc
        jobs = [(self.even_w_in, self.wb_in0)]
        if not (CAST_OVERLAP and self.on("mix0")):
            jobs += self.late_cast_jobs()
        CW = 2048
        with contextlib.ExitStack() as ctx:
            rf = Ring(ctx, nc, "cf", 4, [128, CW], F32)
            rb = Ring(ctx, nc, "cbf", 4, [128, CW], BF16)
            k = 0
            for src, dst in jobs:
                R, C = src.shape
                for rbk in range(R // 128):
                    for c0 in range(0, C, CW):
                        cw = min(CW, C - c0)
                        tf, rfr = rf.next()
                        tb, rbr = rb.next()
                        kb.dma(kb.sp, tf[:, 0:cw], src[rbk * 128:(rbk + 1) * 128, c0:c0 + cw], writes=[rfr])
                        if k % 2 == 0:
                            kb.op(kb.act, lambda: nc.scalar.copy(tb[:, 0:cw], tf[:, 0:cw]), reads=[rfr], writes=[rbr])
                        else:
                            kb.op(kb.dve, lambda: nc.vector.tensor_copy(tb[:, 0:cw], tf[:, 0:cw]), reads=[rfr], writes=[rbr])
                        kb.dma(kb.pool, dst[rbk * 128:(rbk + 1) * 128, c0:c0 + cw], tb[:, 0:cw], reads=[rbr])
                        k += 1

    def late_cast_jobs(self):
        return [(self.mix_w_out[0], self.wb_out[0]), (self.ff_w1[0], self.wb_f1[0]), (self.ff_w2[0], self.wb_f2[0]),
                (self.odd_w_in, self.wb_in1), (self.mix_w_out[1], self.wb_out[1]), (self.ff_w1[1], self.wb_f1[1]),
                (self.ff_w2[1], self.wb_f2[1])]

    def cast_gen(self, ctx, jobs):
        kb, nc = self.kb, self.nc
        CW = 2048
        rf = Ring(ctx, nc, "cgf", 3, [128, CW], F32)
        rb = Ring(ctx, nc, "cgb", 3, [128, CW], BF16)
        pieces = []
        for src, dst in jobs:
            R, C = src.shape
            for rbk in range(R // 128):
                for c0 in range(0, C, CW):
                    pieces.append((src, dst, rbk, c0, min(CW, C - c0)))
        loaded = []
        for k in range(len(pieces) + 2):
            if k < len(pieces):
                src, dst, rbk, c0, cw = pieces[k]
                tf, rfr = rf.next()
                kb.dma(kb.sp, tf[:, 0:cw], src[rbk * 128:(rbk + 1) * 128, c0:c0 + cw], writes=[rfr])
                loaded.append((pieces[k], tf, rfr))
            if k >= 2:
                (src, dst, rbk, c0, cw), tf, rfr = loaded.pop(0)
                tb, rbr = rb.next()
                kb.op(kb.dve, lambda: nc.vector.tensor_copy(tb[:, 0:cw], tf[:, 0:cw]), reads=[rfr], writes=[rbr])
                kb.dma(kb.sp, dst[rbk * 128:(rbk + 1) * 128, c0:c0 + cw], tb[:, 0:cw], reads=[rbr])
            yield

    def stage_ada(self):
        kb, nc = self.kb, self.nc
        with contextlib.ExitStack() as ctx:
            T = lambda name, shape, dty: ctx.enter_context(_sbt(nc, name, shape, dty))
            c16, sg16, csT = T("c16", [16, 128], F32), T("sg16", [16, 128], F32), T("csT", [128, 16], F32)
            brow, mrow = T("brow", [1, 6 * D], F32), T("mrow", [1, 6 * D], F32)
            r_c, r_b, r_m = Res(), Res(), Res()
            wr = Ring(ctx, nc, "adaw", 3, [128, 3072], F32)
            kb.dma(kb.sp, c16[:], self.c[:, :], writes=[r_c])
            kb.op(kb.act, lambda: nc.scalar.activation(out=sg16[:], in_=c16[:], func=ACT.Sigmoid), reads=[r_c], writes=[r_c])
            kb.op(kb.dve, lambda: nc.vector.tensor_mul(c16[:], c16[:], sg16[:]), reads=[r_c], writes=[r_c])
            kb.op(kb.pe, lambda: nc.tensor.transpose(self.ps[0][:, 0:16], c16[:], self.ident_f(16)), reads=[r_c, self.r_cst], writes=[self.pr[0]])
            kb.op(kb.dve, lambda: nc.vector.tensor_copy(csT[:], self.ps[0][:, 0:16]), reads=[self.pr[0]], writes=[r_c])
            for l in range(2):
                kb.dma(kb.sp, brow[:], self.ada_b[l:l + 1, :], writes=[r_b])
                for g in range(4):
                    for kc in range(KC):
                        wt, wres = wr.next()
                        kb.dma(kb.sp, wt[:], self.ada_w[l, kc * 128:(kc + 1) * 128, g * 3072:(g + 1) * 3072], writes=[wres])
                        for b in range(6):
                            kb.op(kb.pe, lambda b=b: nc.tensor.matmul(self.ps[b][0:1, :], lhsT=csT[:, kc:kc + 1], rhs=wt[:, b * 512:(b + 1) * 512],
                                                                   start=(kc == 0), stop=(kc == KC - 1)),
                                  reads=[r_c, wres], writes=[self.pr[b]])
                    for b in range(6):
                        c0 = g * 3072 + b * 512
                        kb.op(kb.dve, lambda b=b, c0=c0: nc.vector.tensor_tensor(out=mrow[0:1, c0:c0 + 512], in0=self.ps[b][0:1, :], in1=brow[0:1, c0:c0 + 512], op=ALU.add),
                              reads=[self.pr[b], r_b], writes=[r_m])
                kb.dma(kb.pool, self.modd[l:l + 1, :], mrow[:], reads=[r_m])

    def prep_cols(self, ctx, l, which):
        kb, nc = self.kb, self.nc
        off = 0 if which == "a" else 3
        gi = 0 if which == "a" else 2
        T = lambda name, shape, dty: ctx.enter_context(_sbt(nc, name, shape, dty))
        sc16, sh16, gm16 = T("sc16", [16, 128], F32), T("sh16", [16, 128], F32), T("gm16", [16, 128], F32)
        r = Res()
        v16 = lambda ap: ap.rearrange("(c p) -> c p", p=128)
        kb.dma(kb.sp, sh16[:], v16(self.modd[l, off * D:(off + 1) * D]), writes=[r])
        kb.dma(kb.sp, sc16[:], v16(self.modd[l, (off + 1) * D:(off + 2) * D]), writes=[r])
        kb.dma(kb.sp, gm16[:], v16(self.norm_g[l, gi, :]), writes=[r])
        kb.op(kb.dve, lambda: nc.vector.scalar_tensor_tensor(out=sc16[:], in0=sc16[:], scalar=1.0, in1=gm16[:], op0=ALU.add, op1=ALU.mult), reads=[r], writes=[r])
        kb.op(kb.pe, lambda: nc.tensor.transpose(self.ps[0][:, 0:16], sc16[:], self.ident_f(16)), reads=[r, self.r_cst], writes=[self.pr[0]])
        kb.op(kb.pe, lambda: nc.tensor.transpose(self.ps[1][:, 0:16], sh16[:], self.ident_f(16)), reads=[r, self.r_cst], writes=[self.pr[1]])
        kb.op(kb.dve, lambda: nc.vector.tensor_copy(self.acol[:], self.ps[0][:, 0:16]), reads=[self.pr[0]], writes=[self.r_cols])
        kb.op(kb.dve, lambda: nc.vector.tensor_copy(self.bcol[:], self.ps[1][:, 0:16]), reads=[self.pr[1]], writes=[self.r_cols])

    def prep_G(self, ctx, l, which):
        kb, nc = self.kb, self.nc
        off = 2 if which == "a" else 5
        gi = 1 if which == "a" else 3
        T = lambda name, shape, dty: ctx.enter_context(_sbt(nc, name, shape, dty))
        grow, gmrow = T("grow", [1, D], F32), T("gmrow", [1, D], F32)
        r = Res()
        kb.dma(kb.sp, grow[:], self.modd[l:l + 1, off * D:(off + 1) * D], writes=[r])
        kb.dma(kb.sp, gmrow[:], self.norm_g[l, gi:gi + 1, :], writes=[r])
        kb.op(kb.dve, lambda: nc.vector.tensor_mul(grow[:], grow[:], gmrow[:]), reads=[r], writes=[r])
        for n in range(4):
            kb.op(kb.pe, lambda n=n: nc.tensor.matmul(self.ps[n][:, :], lhsT=self.cst_sb[0:1, C_ONE:C_ONE + 128], rhs=grow[0:1, n * 512:(n + 1) * 512], start=True, stop=True),
                  reads=[r, self.r_cst], writes=[self.pr[n]])
            kb.op(kb.dve, lambda n=n: nc.vector.tensor_copy(self.G[:, n * 512:(n + 1) * 512], self.ps[n][:, :]), reads=[self.pr[n]], writes=[self.r_G])

    def stage_normT(self, l, which, xsrc):
        kb, nc = self.kb, self.nc
        with contextlib.ExitStack() as ctx:
            T = lambda name, shape, dty: ctx.enter_context(_sbt(nc, name, shape, dty))
            with contextlib.ExitStack() as c2:
                self.prep_cols(c2, l, which)
            kb.barrier()
            rx = Ring(ctx, nc, "nx", 3, [128, D], F32)
            rxn = Ring(ctx, nc, "nxn", 2, [128, D], BF16)
            rst = Ring(ctx, nc, "nst", 4, [128, 4], F32)
            rh = Ring(ctx, nc, "nh", 2, [128, KC, 512], BF16)
            junk = T("njunk", [128, D], BF16)
            rj = Res()
            pbank = 0
            for t4 in range(NT // 4):
                hts, hres = rh.next()
                for tt in range(4):
                    t = t4 * 4 + tt
                    xt, xr = rx.next()
                    xn, xnr = rxn.next()
                    st, sr = rst.next()
                    kb.dma(kb.sp, xt[:], xsrc[t * 128:(t + 1) * 128, :], writes=[xr])
                    kb.op(kb.act, lambda: nc.scalar.activation(out=junk[:], in_=xt[:], func=ACT.Square, accum_out=st[:, 0:1]), reads=[xr], writes=[rj, sr])
                    kb.op(kb.act, lambda: nc.scalar.activation(out=st[:, 1:2], in_=st[:, 0:1], func=ACT.Sqrt, bias=self.eps_t[:], scale=1.0 / D), reads=[sr], writes=[sr])
                    kb.op(kb.dve, lambda: nc.vector.reciprocal(st[:, 2:3], st[:, 1:2]), reads=[sr], writes=[sr])
                    kb.op(kb.dve, lambda: nc.vector.tensor_scalar(out=xn[:], in0=xt[:], scalar1=st[:, 2:3], scalar2=None, op0=ALU.mult), reads=[xr, sr], writes=[xnr])
                    for half in range(2):
                        b = pbank % 4
                        pbank += 1
                        pv = self.ps[b][:].bitcast(BF16)
                        for k8 in range(8):
                            kc = half * 8 + k8
                            kb.op(kb.pe, lambda kc=kc, k8=k8, pv=pv: nc.tensor.transpose(pv[:, k8 * 128:(k8 + 1) * 128], xn[:, kc * 128:(kc + 1) * 128], self.ident_b()),
                                  reads=[xnr, self.r_cst], writes=[self.pr[b]])
                        for k8 in range(8):
                            kc = half * 8 + k8
                            dst = hts[:, kc, tt * 128:(tt + 1) * 128]
                            src = pv[:, k8 * 128:(k8 + 1) * 128]
                            if k8 % 2 == 0:
                                kb.op(kb.act, lambda dst=dst, src=src, kc=kc: nc.scalar.activation(out=dst, in_=src, func=ACT.Identity, bias=self.bcol[:, kc:kc + 1], scale=self.acol[:, kc:kc + 1]),
                                      reads=[self.pr[b], self.r_cols], writes=[hres])
                            else:
                                kb.op(kb.dve, lambda dst=dst, src=src, kc=kc: nc.vector.tensor_scalar(out=dst, in0=src, scalar1=self.acol[:, kc:kc + 1], scalar2=self.bcol[:, kc:kc + 1], op0=ALU.mult, op1=ALU.add),
                                      reads=[self.pr[b], self.r_cols], writes=[hres])
                kb.dma(kb.pool, self.hT.rearrange("k p t -> p k t")[:, :, t4 * 512:(t4 + 1) * 512], hts[:], reads=[hres])

    def rstd_from_ss(self, st, sr, ncols):
        kb, nc = self.kb, self.nc
        kb.op(kb.dve, lambda: nc.vector.tensor_reduce(out=st[:, 4:5], in_=st[:, 0:ncols], axis=AX.X, op=ALU.add), reads=[sr], writes=[sr])
        kb.op(kb.act, lambda: nc.scalar.activation(out=st[:, 5:6], in_=st[:, 4:5], func=ACT.Sqrt, bias=self.eps_t[:], scale=1.0 / D), reads=[sr], writes=[sr])
        kb.op(kb.dve, lambda: nc.vector.reciprocal(st[:, 6:7], st[:, 5:6]), reads=[sr], writes=[sr])

    def stage_outproj(self, l, xsrc):
        kb, nc = self.kb, self.nc
        with contextlib.ExitStack() as ctx:
            T = lambda name, shape, dty: ctx.enter_context(_sbt(nc, name, shape, dty))
            with contextlib.ExitStack() as c2:
                self.prep_G(c2, l, "a")
            kb.barrier()
            wo = T("wo", [128, KC, D], BF16)
            r_wo = Res()
            wv = self.wb_out[l].rearrange("(k p) c -> p k c", p=128)
            for n in range(4):
                kb.dma(kb.sp, wo[:, :, n * 512:(n + 1) * 512], wv[:, :, n * 512:(n + 1) * 512], writes=[r_wo])
            ro = Ring(ctx, nc, "oo", 2, [128, KC, 512], BF16)
            rx = Ring(ctx, nc, "ox", 2, [128, D], F32)
            rt1 = Ring(ctx, nc, "ot1", 2, [128, D], F32)
            rst = Ring(ctx, nc, "ost", 4, [128, 8], F32)
            junk = T("ojunk", [128, 512], BF16)
            rj = Res()
            for t4 in range(NT // 4):
                ot, ores = ro.next()
                kb.dma(kb.sp, ot[:], self.oT.rearrange("k p t -> p k t")[:, :, t4 * 512:(t4 + 1) * 512], writes=[ores])
                for tt in range(4):
                    t = t4 * 4 + tt
                    xt, xr = rx.next()
                    t1, t1r = rt1.next()
                    st, sr = rst.next()
                    kb.dma(kb.sp, xt[:], xsrc[t * 128:(t + 1) * 128, :], writes=[xr])
                    base = (t % 2) * 4
                    for n in range(4):
                        b = base + n
                        for kc in range(KC):
                            kb.op(kb.pe, lambda kc=kc, b=b, n=n: nc.tensor.matmul(self.ps[b][:, :], lhsT=ot[:, kc, tt * 128:(tt + 1) * 128], rhs=wo[:, kc, n * 512:(n + 1) * 512],
                                                                             start=(kc == 0), stop=(kc == KC - 1)),
                                  reads=[ores, r_wo], writes=[self.pr[b]])
                        kb.op(kb.act, lambda b=b, n=n: nc.scalar.activation(out=junk[:], in_=self.ps[b][:, :], func=ACT.Square, accum_out=st[:, n:n + 1]), reads=[self.pr[b]], writes=[rj, sr])
                        kb.op(kb.dve, lambda b=b, n=n: nc.vector.tensor_tensor(out=t1[:, n * 512:(n + 1) * 512], in0=self.ps[b][:, :], in1=self.G[:, n * 512:(n + 1) * 512], op=ALU.mult),
                              reads=[self.pr[b], self.r_G], writes=[t1r])
                    self.rstd_from_ss(st, sr, 4)
                    kb.op(kb.dve, lambda: nc.vector.scalar_tensor_tensor(out=t1[:], in0=t1[:], scalar=st[:, 6:7], in1=xt[:], op0=ALU.mult, op1=ALU.add), reads=[t1r, sr, xr], writes=[t1r])
                    kb.dma(kb.pool, self.out[t * 128:(t + 1) * 128, :], t1[:], reads=[t1r])

    def stage_ffn(self, l, xsrc):
        kb, nc = self.kb, self.nc
        with contextlib.ExitStack() as ctx:
            T = lambda name, shape, dty: ctx.enter_context(_sbt(nc, name, shape, dty))
            with contextlib.ExitStack() as c2:
                self.prep_G(c2, l, "m")
            kb.barrier()
            rh = Ring(ctx, nc, "fh", 1, [128, KC, 512], BF16)
            uT = T("uT", [128, 64, 512], BF16)
            r_u = [Res() for _ in range(64)]
            rw1 = Ring(ctx, nc, "fw1", 3, [128, KC, 256], BF16)
            rw2 = Ring(ctx, nc, "fw2", 3, [128, 8, 512], BF16)
            rtmp = Ring(ctx, nc, "ftmp", 3, [128, 512], F32)
            ysb = T("ysb", [128, 4, D], F32)
            r_y = [Res() for _ in range(4)]
            rx = Ring(ctx, nc, "fx", 2, [128, D], F32)
            rst = Ring(ctx, nc, "fst", 8, [128, 8], F32)
            junk = T("fjunk", [128, 512], BF16)
            rj = Res()
            w1v = self.wb_f1[l].rearrange("(k p) c -> p k c", p=128)
            w2v = self.wb_f2[l].rearrange("(f p) c -> p f c", p=128)
            pb1 = 0
            import os
            for t4 in range(int(os.environ.get("FFN_BLOCKS", NT // 4))):
                ht, hres = rh.next()
                kb.dma(kb.sp, ht[:], self.hT.rearrange("k p t -> p k t")[:, :, t4 * 512:(t4 + 1) * 512], writes=[hres])
                for fb in range(32):
                    w1, w1r = rw1.next()
                    kb.dma(kb.sp, w1[:], w1v[:, :, fb * 256:(fb + 1) * 256], writes=[w1r])
                    for fi in range(2):
                        f = fb * 2 + fi
                        b = pb1 % 4
                        pb1 += 1
                        for kc in range(KC):
                            kb.op(kb.pe, lambda kc=kc, b=b, fi=fi: nc.tensor.matmul(self.ps[b][:, :], lhsT=w1[:, kc, fi * 128:(fi + 1) * 128], rhs=ht[:, kc, :],
                                                                               start=(kc == 0), stop=(kc == KC - 1)),
                                  reads=[w1r, hres], writes=[self.pr[b]])
                        tmp, tr = rtmp.next()
                        kb.op(kb.act, lambda b=b: nc.scalar.activation(out=tmp[:], in_=self.ps[b][:, :], func=ACT.Relu), reads=[self.pr[b]], writes=[tr])
                        kb.op(kb.dve, lambda f=f: nc.vector.tensor_tensor(out=uT[:, f, :], in0=tmp[:], in1=tmp[:], op=ALU.mult), reads=[tr], writes=[r_u[f]])
                if os.environ.get("FFN_PHASE") == "1":
                    continue
                sts = [rst.next() for _ in range(4)]
                for n in range(4):
                    for fg in range(8):
                        w2, w2r = rw2.next()
                        kb.dma(kb.sp, w2[:], w2v[:, fg * 8:(fg + 1) * 8, n * 512:(n + 1) * 512], writes=[w2r])
                        for j in range(8):
                            f = fg * 8 + j
                            for tt in range(4):
                                b = 4 + tt
                                kb.op(kb.pe, lambda f=f, j=j, tt=tt, b=b: nc.tensor.matmul(self.ps[b][:, :], lhsT=uT[:, f, tt * 128:(tt + 1) * 128], rhs=w2[:, j, :],
                                                                                       start=(f == 0), stop=(f == 63)),
                                      reads=[r_u[f], w2r], writes=[self.pr[b]])
                    for tt in range(4):
                        b = 4 + tt
                        st, sr = sts[tt]
                        if os.environ.get("FFN_NOEV") == "1":
                            continue
                        if os.environ.get("FFN_NOEV") != "2":
                            kb.op(kb.act, lambda b=b, st=st: nc.scalar.activation(out=junk[:], in_=self.ps[b][:, :], func=ACT.Square, accum_out=st[:, n:n + 1]), reads=[self.pr[b]], writes=[rj, sr])
                        kb.op(kb.dve, lambda b=b, tt=tt: nc.vector.tensor_tensor(out=ysb[:, tt, n * 512:(n + 1) * 512], in0=self.ps[b][:, :], in1=self.G[:, n * 512:(n + 1) * 512], op=ALU.mult),
                              reads=[self.pr[b], self.r_G], writes=[r_y[tt]])
                if os.environ.get("FFN_PHASE") == "2":
                    continue
                for tt in range(4):
                    t = t4 * 4 + tt
                    st, sr = sts[tt]
                    xt, xr = rx.next()
                    kb.dma(kb.sp, xt[:], xsrc[t * 128:(t + 1) * 128, :], writes=[xr])
                    self.rstd_from_ss(st, sr, 4)
                    kb.op(kb.dve, lambda tt=tt, st=st: nc.vector.scalar_tensor_tensor(out=xt[:], in0=ysb[:, tt, :], scalar=st[:, 6:7], in1=xt[:], op0=ALU.mult, op1=ALU.add),
                          reads=[r_y[tt], sr, xr], writes=[xr])
                    kb.dma(kb.pool, self.out[t * 128:(t + 1) * 128, :], xt[:], reads=[xr])

    def stage_inproj0(self):
        kb, nc = self.kb, self.nc
        hTv = self.hT.rearrange("k p t -> p k t")
        wv = self.wb_in0.rearrange("(k p) c -> p k c", p=128)
        qk_blocks = [(0, 0, 0), (512, 4, 0), (1024, 0, 1), (1536, 4, 1), (3080, 8, 0), (3592, 12, 0), (4104, 8, 1), (4616, 12, 1)]
        v_blocks = [(2048, 0), (2560, 4), (5128, 8), (5640, 12)]
        with contextlib.ExitStack() as ctx:
            T = lambda name, shape, dty: ctx.enter_context(_sbt(nc, name, shape, dty))
            rh = Ring(ctx, nc, "ih", 2, [128, KC, 512], BF16)
            rw = Ring(ctx, nc, "iw", 3, [128, KC, 512], BF16)
            rs = Ring(ctx, nc, "is", 4, [128, 512], BF16)
            wg = T("iwg", [128, KC, 128], BF16)
            nlf = T("nlf", [8, S], F32)
            brow, nb = T("ibrow", [1, 8], F32), T("inb", [8, 1], F32)
            etmp = T("ietmp", [8, 512], F32)
            r_wg, r_nlf, r_b, r_e = Res(), Res(), Res(), Res()
            kb.dma(kb.sp, wg[:], wv[:, :, 3008:3136], writes=[r_wg])
            kb.dma(kb.sp, brow[:], self.even_b[:, :], writes=[r_b])
            kb.op(kb.pe, lambda: nc.tensor.matmul(self.ps[7][0:8, 0:1], lhsT=brow[0:1, 0:8], rhs=self.cst_sb[0:1, C_ONE:C_ONE + 1], start=True, stop=True),
                  reads=[r_b, self.r_cst], writes=[self.pr[7]])
            kb.op(kb.dve, lambda: nc.vector.tensor_scalar(out=nb[:], in0=self.ps[7][0:8, 0:1], scalar1=-1.0, scalar2=None, op0=ALU.mult), reads=[self.pr[7]], writes=[r_b])
            pb = 0
            ev = 0
            for t4 in range(NT // 4):
                ht, hres = rh.next()
                kb.dma(kb.sp, ht[:], hTv[:, :, t4 * 512:(t4 + 1) * 512], writes=[hres])
                b = pb % 7
                pb += 1
                for kc in range(KC):
                    kb.op(kb.pe, lambda: nc.tensor.matmul(self.ps[b][0:8, :], lhsT=wg[:, kc, 64:72], rhs=ht[:, kc, :], start=(kc == 0), stop=(kc == KC - 1)),
                          reads=[r_wg, hres], writes=[self.pr[b]])
                kb.op(kb.act, lambda: nc.scalar.activation(out=etmp[:], in_=self.ps[b][0:8, :], func=ACT.Exp, bias=nb[:], scale=-1.0), reads=[self.pr[b], r_b], writes=[r_e])
                kb.op(kb.act, lambda: nc.scalar.activation(out=nlf[:, t4 * 512:(t4 + 1) * 512], in_=etmp[:], func=ACT.Ln, bias=self.one_t[0:8, :], scale=1.0), reads=[r_e], writes=[r_nlf])
                for (c0, h0, isk) in qk_blocks:
                    w, wr = rw.next()
                    kb.dma(kb.sp, w[:], wv[:, :, c0:c0 + 512], writes=[wr])
                    for hi in range(4):
                        b = pb % 7
                        pb += 1
                        for kc in range(KC):
                            kb.op(kb.pe, lambda: nc.tensor.matmul(self.ps[b][:, :], lhsT=w[:, kc, hi * 128:(hi + 1) * 128], rhs=ht[:, kc, :], start=(kc == 0), stop=(kc == KC - 1)),
                                  reads=[wr, hres], writes=[self.pr[b]])
                        stg, sr = rs.next()
                        if ev % 2 == 0:
                            kb.op(kb.act, lambda: nc.scalar.copy(stg[:], self.ps[b][:, :]), reads=[self.pr[b]], writes=[sr])
                        else:
                            kb.op(kb.dve, lambda: nc.vector.tensor_copy(stg[:], self.ps[b][:, :]), reads=[self.pr[b]], writes=[sr])
                        ev += 1
                        kb.dma(kb.pool, self.qkT[2 * (h0 + hi) + isk, :, t4 * 512:(t4 + 1) * 512], stg[:], reads=[sr])
                for (c0, h0) in v_blocks:
                    w, wr = rw.next()
                    kb.dma(kb.sp, w[:], wv[:, :, c0:c0 + 512], writes=[wr])
                    for tt in range(4):
                        j = t4 * 4 + tt
                        b = pb % 7
                        pb += 1
                        for kc in range(KC):
                            kb.op(kb.pe, lambda: nc.tensor.matmul(self.ps[b][:, :], lhsT=ht[:, kc, tt * 128:(tt + 1) * 128], rhs=w[:, kc, :], start=(kc == 0), stop=(kc == KC - 1)),
                                  reads=[wr, hres], writes=[self.pr[b]])
                        stg, sr = rs.next()
                        if ev % 2 == 0:
                            kb.op(kb.act, lambda: nc.scalar.copy(stg[:], self.ps[b][:, :]), reads=[self.pr[b]], writes=[sr])
                        else:
                            kb.op(kb.dve, lambda: nc.vector.tensor_copy(stg[:], self.ps[b][:, :]), reads=[self.pr[b]], writes=[sr])
                        ev += 1
                        kb.dma(kb.pool, self.vt[h0:h0 + 4, :, j, :].rearrange("h p d -> p h d"), stg[:].rearrange("p (h d) -> p h d", h=4), reads=[sr])
            Pt = T("iP", [8, S], F32)
            R = T("iR", [8, 8, 32], F32)
            r_P, r_R = Res(), Res()
            kb.op(kb.dve, lambda: nc.vector.tensor_tensor_scan(out=Pt[:], data0=self.one_t[0:8, 0:1].to_broadcast([8, S]), data1=nlf[:], initial=0.0, op0=ALU.mult, op1=ALU.add),
                  reads=[r_nlf], writes=[r_P])
            for j in range(NT):
                kb.op(kb.pe, lambda: nc.tensor.transpose(self.ps[0][:, j * 8:(j + 1) * 8], Pt[0:8, j * 128:(j + 1) * 128], self.ident_f(8)), reads=[r_P, self.r_cst], writes=[self.pr[0]])
            kb.op(kb.dve, lambda: nc.vector.tensor_copy(self.Pcol[:], self.ps[0][:, 0:256]), reads=[self.pr[0]], writes=[self.r_P])
            for h in range(8):
                kb.op(kb.dve, lambda: nc.vector.tensor_scalar(out=R[:, h, :], in0=Pt[0:8, 0:S:128], scalar1=self.cst_sb[0:8, C_ID + h:C_ID + h + 1], scalar2=None, op0=ALU.mult),
                      reads=[r_P, self.r_cst], writes=[r_R])
            kb.op(kb.pe, lambda: nc.tensor.matmul(self.ps[1][:, 0:256], lhsT=self.cst_sb[0:8, C_ONE:C_ONE + 128], rhs=R[:].rearrange("k h i -> k (h i)"), start=True, stop=True),
                  reads=[r_R, self.r_cst], writes=[self.pr[1]])
            kb.op(kb.dve, lambda: nc.vector.tensor_copy(self.Pb[:], self.ps[1][:, 0:256]), reads=[self.pr[1]], writes=[self.r_P])
            kb.dma(kb.pool, self.dbg[:, 0:256], self.Pcol[:], reads=[self.r_P])
            kb.dma(kb.pool, self.dbg[:, 256:512], self.Pb[:], reads=[self.r_P])

    def stage_attn0(self):
        kb, nc = self.kb, self.nc
        scale = HD ** -0.5
        import os
        heads = [int(v) for v in os.environ["ATTN_HEADS"].split(",")] if "ATTN_HEADS" in os.environ else range(16)
        with contextlib.ExitStack() as ctx:
            T = lambda name, shape, dty: ctx.enter_context(_sbt(nc, name, shape, dty))
            rq = Ring(ctx, nc, "aq", 2, [128, S], BF16)
            rk = Ring(ctx, nc, "ak", 2, [128, S], BF16)
            rnk = Ring(ctx, nc, "ank", 1, [128, S], BF16)
            rv = Ring(ctx, nc, "av", 2, [128, NT, 129], BF16)
            roT = Ring(ctx, nc, "aoT", 2, [128, S], BF16)
            rp = Ring(ctx, nc, "ap", 5, [128, 128], BF16)
            rsp = Ring(ctx, nc, "asp", 4, [128, 128], BF16)
            re_ = Ring(ctx, nc, "ae", 3, [128, 128], F32)
            rT = Ring(ctx, nc, "aT", 2, [128, 128], F32)
            rR = Ring(ctx, nc, "aR", 4, [128, 128], F32)
            rbias = Ring(ctx, nc, "ab", 5, [128, 32], F32)
            rosb = Ring(ctx, nc, "aosb", 3, [128, 128], BF16)
            rrd = Ring(ctx, nc, "ard", 3, [128, 1], F32)
            for vt_, vr_ in zip(rv.t, rv.r):
                kb.op(kb.pool, lambda: nc.gpsimd.memset(vt_[:, :, 128:129], 1.0), writes=[vr_])
            cg = self.cast_gen(ctx, self.late_cast_jobs()) if (CAST_OVERLAP and self.on("cast")) else iter(())
            tri, trs = self.cb[:, C_TRI:C_TRI + 128], self.cb[:, C_TRS:C_TRS + 128]
            low, ones_b = self.cb[:, C_LOW:C_LOW + 128], self.cb[:, C_ONE:C_ONE + 128]
            Pcol = self.Pcol[:].rearrange("p (j h) -> p j h", h=8)
            Pb = self.Pb[:].rearrange("p (h i) -> p h i", i=32)
            sb_ = [0]
            xb_ = [0]
            for h in heads:
                fox = h < 8
                qT, qr = rq.next()
                kT, kr = rk.next()
                V, vr = rv.next()
                oTs, oTr = roT.next()
                kb.dma(kb.sp, qT[:], self.qkT[2 * h, :, :], writes=[qr])
                kb.dma(kb.sp, kT[:], self.qkT[2 * h + 1, :, :], writes=[kr])
                kb.dma(kb.sp, V[:, :, 0:128], self.vt[h, :, :, :], writes=[vr])
                if not fox:
                    nkT, nkr = rnk.next()
                    kb.op(kb.dve, lambda: nc.vector.tensor_scalar(out=nkT[:], in0=kT[:], scalar1=-scale, scalar2=None, op0=ALU.mult), reads=[kr], writes=[nkr])
                def finalize(i, normalize):
                    bO = 3 + (i % 2)
                    osb, osr = rosb.next()
                    if normalize:
                        rd, rdr = rrd.next()
                        kb.op(kb.dve, lambda: nc.vector.reciprocal(rd[:], self.ps[bO][:, 128:129]), reads=[self.pr[bO]], writes=[rdr])
                        kb.op(kb.act, lambda: nc.scalar.activation(out=osb[:], in_=self.ps[bO][:, 0:128], func=ACT.Copy, scale=rd[:, 0:1]), reads=[self.pr[bO], rdr], writes=[osr])
                    else:
                        kb.op(kb.act, lambda: nc.scalar.copy(osb[:], self.ps[bO][:, 0:128]), reads=[self.pr[bO]], writes=[osr])
                    pv = self.ps[5][:].bitcast(BF16)
                    kb.op(kb.pe, lambda: nc.tensor.transpose(pv[:, 0:128], osb[:], self.ident_b()), reads=[osr, self.r_cst], writes=[self.pr[5]])
                    kb.op(kb.dve, lambda: nc.vector.tensor_copy(oTs[:, i * 128:(i + 1) * 128], pv[:, 0:128]), reads=[self.pr[5]], writes=[oTr])
                    next(cg, None)

                if fox:
                    biases = {}

                    def f1(i, j):
                        if j == 0:
                            bias, br = rbias.next()
                            kb.op(kb.dve, lambda: nc.vector.tensor_scalar(out=bias[:], in0=Pcol[:, :, h], scalar1=Pb[:, h, i:i + 1], scalar2=None, op0=ALU.subtract),
                                  reads=[self.r_P], writes=[br])
                            biases[i] = (bias, br)
                        bS = sb_[0] % 3
                        sb_[0] += 1
                        kb.op(kb.pe, lambda: nc.tensor.matmul(self.ps[bS][:, 0:128], lhsT=kT[:, j * 128:(j + 1) * 128], rhs=qT[:, i * 128:(i + 1) * 128], start=True, stop=True),
                              reads=[kr, qr], writes=[self.pr[bS]])
                        return bS

                    def f2(i, j, bS):
                        bO = 3 + (i % 2)
                        bias, br = biases[i]
                        PT, pr_ = rp.next()
                        kb.op(kb.act, lambda: nc.scalar.activation(out=PT[:], in_=self.ps[bS][:, 0:128], func=ACT.Exp, bias=bias[:, j:j + 1], scale=scale),
                              reads=[self.pr[bS], br], writes=[pr_])
                        if j == i:
                            kb.op(kb.pool, lambda: nc.gpsimd.tensor_tensor(out=PT[:], in0=PT[:], in1=tri, op=ALU.mult), reads=[pr_, self.r_cst], writes=[pr_])
                        kb.op(kb.pe, lambda: nc.tensor.matmul(self.ps[bO][:, 0:129], lhsT=PT[:], rhs=V[:, j, :], start=(j == 0), stop=(j == i)),
                              reads=[pr_, vr], writes=[self.pr[bO]])
                        if j == i:
                            finalize(i, True)

                    pend = []
                    for i in range(NT):
                        for j in range(i + 1):
                            pend.append((i, j, f1(i, j)))
                            if len(pend) > 2:
                                f2(*pend.pop(0))
                    while pend:
                        f2(*pend.pop(0))
                else:
                    raccs = {}
                    sps = {}

                    def g1(i, idx, j, last):
                        bS = sb_[0] % 2
                        sb_[0] += 1
                        kb.op(kb.pe, lambda: nc.tensor.matmul(self.ps[bS][:, 0:128], lhsT=kT[:, j * 128:(j + 1) * 128], rhs=qT[:, i * 128:(i + 1) * 128], start=True, stop=True),
                              reads=[kr, qr], writes=[self.pr[bS]])
                        e, er = re_.next()
                        SP, spr = rsp.next()
                        kb.op(kb.act, lambda: nc.scalar.activation(out=e[:], in_=self.ps[bS][:, 0:128], func=ACT.Exp, scale=scale), reads=[self.pr[bS]], writes=[er])
                        kb.op(kb.act, lambda: nc.scalar.activation(out=SP[:], in_=e[:], func=ACT.Ln, bias=self.one_t[:], scale=1.0), reads=[er], writes=[spr])
                        if j == i:
                            kb.op(kb.pool, lambda: nc.gpsimd.tensor_tensor(out=SP[:], in0=SP[:], in1=trs, op=ALU.mult), reads=[spr, self.r_cst], writes=[spr])
                        sps[(i, idx)] = (SP, spr)

                    def g2(i, idx, j, last):
                        SP, spr = sps.pop((i, idx))
                        bX = (2, 6)[xb_[0] % 2]
                        xb_[0] += 1
                        kb.op(kb.pe, lambda: nc.tensor.matmul(self.ps[bX][:, 0:128], lhsT=low, rhs=SP[:], start=True, stop=False), reads=[spr, self.r_cst], writes=[self.pr[bX]])
                        kb.op(kb.pe, lambda: nc.tensor.matmul(self.ps[bX][:, 0:128], lhsT=nkT[:, j * 128:(j + 1) * 128], rhs=qT[:, i * 128:(i + 1) * 128], start=False, stop=True),
                              reads=[nkr, qr], writes=[self.pr[bX]])
                        A, ar = rp.next()
                        if idx == 0:
                            raccs[i] = rR.next()
                            kb.op(kb.act, lambda: nc.scalar.activation(out=A[:], in_=self.ps[bX][:, 0:128], func=ACT.Exp, scale=-1.0), reads=[self.pr[bX]], writes=[ar])
                        else:
                            Racc, rr = raccs[i]
                            Tt, tr_ = rT.next()
                            kb.op(kb.dve, lambda: nc.vector.tensor_tensor(out=Tt[:], in0=self.ps[bX][:, 0:128], in1=Racc[:], op=ALU.add), reads=[self.pr[bX], rr], writes=[tr_])
                            kb.op(kb.act, lambda: nc.scalar.activation(out=A[:], in_=Tt[:], func=ACT.Exp, scale=-1.0), reads=[tr_], writes=[ar])
                        if j == i:
                            kb.op(kb.pool, lambda: nc.gpsimd.tensor_tensor(out=A[:], in0=A[:], in1=trs, op=ALU.mult), reads=[ar, self.r_cst], writes=[ar])
                        if not last:
                            Racc, rr = raccs[i]
                            kb.op(kb.pe, lambda: nc.tensor.matmul(self.ps[7][:, 0:128], lhsT=ones_b, rhs=SP[:], start=True, stop=True), reads=[spr, self.r_cst], writes=[self.pr[7]])
                            if idx == 0:
                                kb.op(kb.dve, lambda: nc.vector.tensor_copy(Racc[:], self.ps[7][:, 0:128]), reads=[self.pr[7]], writes=[rr])
                            else:
                                kb.op(kb.dve, lambda: nc.vector.tensor_tensor(out=Racc[:], in0=self.ps[7][:, 0:128], in1=Racc[:], op=ALU.add), reads=[self.pr[7], rr], writes=[rr])
                        sps[("A", i, idx)] = (A, ar)

                    def g3(i, idx, j, last):
                        A, ar = sps.pop(("A", i, idx))
                        bO = 3 + (i % 2)
                        kb.op(kb.pe, lambda: nc.tensor.matmul(self.ps[bO][:, 0:128], lhsT=A[:], rhs=V[:, j, 0:128], start=(idx == 0), stop=last),
                              reads=[ar, vr], writes=[self.pr[bO]])
                        if last:
                            finalize(i, False)

                    stream = []
                    for i in range(NT):
                        js = [j for j in range(i, i - SB_WIN, -1) if j >= 0]
                        for idx, j in enumerate(js):
                            stream.append((i, idx, j, idx == len(js) - 1))
                    n = len(stream)
                    for k in range(n + 2):
                        if k < n:
                            g1(*stream[k])
                        if 0 <= k - 1 < n:
                            g2(*stream[k - 1])
                        if 0 <= k - 2 < n:
                            g3(*stream[k - 2])
                kb.dma(kb.pool, self.oT[h, :, :], oTs[:], reads=[oTr])
            for _ in cg:
                pass

    def stage_inproj1(self):
        kb, nc = self.kb, self.nc
        hTv = self.hT.rearrange("k p t -> p k t")
        wv = self.wb_in1.rearrange("(k p) c -> p k c", p=128)
        with contextlib.ExitStack() as ctx:
            T = lambda name, shape, dty: ctx.enter_context(_sbt(nc, name, shape, dty))
            sinq, cosq = T("sinq", [128, NT, 64], F32), T("cosq", [128, NT, 64], F32)
            r_tab = Res()
            with contextlib.ExitStack() as c2:
                T2 = lambda name, shape, dty: c2.enter_context(_sbt(nc, name, shape, dty))
                pi32, pf32, post = T2("pi32", [32, 128], I32), T2("pf32", [32, 128], F32), T2("post", [128, 32], F32)
                ang, u, ki, kf, m = (T2("ang", [128, NT * 64], F32), T2("ru", [128, NT * 64], F32), T2("rki", [128, NT * 64], I32),
                                     T2("rkf", [128, NT * 64], F32), T2("rm", [128, NT * 64], F32))
                npi = T2("npi", [128, 1], F32)
                r = Res()
                kb.dma(kb.sp, pi32[:], self.pos[:, :], writes=[r])
                kb.op(kb.dve, lambda: nc.vector.memset(npi[:], -3.14159), writes=[r])
                kb.op(kb.dve, lambda: nc.vector.tensor_copy(pf32[:], pi32[:]), reads=[r], writes=[r])
                kb.op(kb.pe, lambda: nc.tensor.transpose(self.ps[0][:, 0:32], pf32[:], self.ident_f(32)), reads=[r, self.r_cst], writes=[self.pr[0]])
                kb.op(kb.dve, lambda: nc.vector.tensor_copy(post[:], self.ps[0][:, 0:32]), reads=[self.pr[0]], writes=[r])
                for j in range(NT):
                    kb.op(kb.dve, lambda: nc.vector.tensor_scalar(out=ang[:, j * 64:(j + 1) * 64], in0=self.cst_sb[:, C_INV:C_INV + 64], scalar1=post[:, j:j + 1], scalar2=None, op0=ALU.mult),
                          reads=[r, self.r_cst], writes=[r])
                for tab, shift in ((sinq, 0.5), (cosq, 0.75)):
                    V = nc.vector
                    kb.op(kb.dve, lambda: V.tensor_scalar(out=u[:], in0=ang[:], scalar1=1.0 / (2 * np.pi), scalar2=shift, op0=ALU.mult, op1=ALU.add), reads=[r], writes=[r])
                    kb.op(kb.dve, lambda: V.tensor_copy(ki[:], u[:]), reads=[r], writes=[r])
                    kb.op(kb.dve, lambda: V.tensor_copy(kf[:], ki[:]), reads=[r], writes=[r])
                    kb.op(kb.dve, lambda: V.tensor_tensor(out=u[:], in0=u[:], in1=kf[:], op=ALU.subtract), reads=[r], writes=[r])
                    kb.op(kb.dve, lambda: V.tensor_scalar(out=m[:], in0=u[:], scalar1=0.0, scalar2=None, op0=ALU.is_lt), reads=[r], writes=[r])
                    kb.op(kb.dve, lambda: V.tensor_tensor(out=u[:], in0=u[:], in1=m[:], op=ALU.add), reads=[r], writes=[r])
                    kb.op(kb.dve, lambda: V.tensor_scalar(out=m[:], in0=u[:], scalar1=1.0, scalar2=None, op0=ALU.is_ge), reads=[r], writes=[r])
                    kb.op(kb.dve, lambda: V.tensor_tensor(out=u[:], in0=u[:], in1=m[:], op=ALU.subtract), reads=[r], writes=[r])
                    kb.op(kb.act, lambda: nc.scalar.activation(out=tab[:].rearrange("p j k -> p (j k)"), in_=u[:], func=ACT.Sin, bias=npi[:], scale=6.28318), reads=[r], writes=[r_tab])
                kb.barrier()
            rh = Ring(ctx, nc, "jh", 2, [128, KC, 512], BF16)
            rw = Ring(ctx, nc, "jw", 3, [128, KC, 512], BF16)
            qtile = [T(f"jq{tt}", [128, 16, 128], BF16) for tt in range(4)]
            qres = [Res() for _ in range(4)]
            rta = Ring(ctx, nc, "jta", 2, [128, 512], F32)
            rtb = Ring(ctx, nc, "jtb", 2, [128, 512], F32)
            rqT = Ring(ctx, nc, "jqT", 2, [128, 16, 512], BF16)
            rqiT = Ring(ctx, nc, "jqiT", 2, [128, 8, 512], BF16)
            rkT = Ring(ctx, nc, "jkT", 2, [128, 512], BF16)
            rkiT = Ring(ctx, nc, "jkiT", 2, [128, 512], BF16)
            rkr = Ring(ctx, nc, "jkr", 2, [128, 128], BF16)
            rvs = Ring(ctx, nc, "jvs", 2, [128, 128], BF16)
            rws = Ring(ctx, nc, "jws", 2, [128, 16], F32)
            pbank = [0]
            tbank = [0]
            evc = [0]

            def proj(ht, hres, w, wr, tt, ncols):
                b = pbank[0] % 6
                pbank[0] += 1
                for kc in range(KC):
                    kb.op(kb.pe, lambda: nc.tensor.matmul(self.ps[b][:, 0:ncols], lhsT=ht[:, kc, tt * 128:(tt + 1) * 128], rhs=w[:, kc, 0:ncols], start=(kc == 0), stop=(kc == KC - 1)),
                          reads=[wr, hres], writes=[self.pr[b]])
                return b

            def rope(b, c0, nh, half, j, dst, dres):
                x = self.ps[b][:, c0:c0 + nh * 2 * half].rearrange("p (h two d) -> p h two d", h=nh, two=2)
                x1, x2 = x[:, :, 0, :], x[:, :, 1, :]
                st = 64 // half
                cs = cosq[:, j, 0:64:st].unsqueeze(1).to_broadcast([128, nh, half])
                sn = sinq[:, j, 0:64:st].unsqueeze(1).to_broadcast([128, nh, half])
                ta, tar = rta.next()
                tb, tbr = rtb.next()
                av = ta[:, 0:nh * half].rearrange("p (h d) -> p h d", h=nh)
                bv = tb[:, 0:nh * half].rearrange("p (h d) -> p h d", h=nh)
                V = nc.vector
                kb.op(kb.dve, lambda: V.tensor_tensor(out=av, in0=x1, in1=cs, op=ALU.mult), reads=[self.pr[b], r_tab], writes=[tar])
                kb.op(kb.dve, lambda: V.tensor_tensor(out=bv, in0=x2, in1=sn, op=ALU.mult), reads=[self.pr[b], r_tab], writes=[tbr])
                kb.op(kb.pool, lambda: nc.gpsimd.tensor_tensor(out=dst[:, :, 0:half], in0=av, in1=bv, op=ALU.subtract), reads=[tar, tbr], writes=[dres])
                ta, tar = rta.next()
                tb, tbr = rtb.next()
                av = ta[:, 0:nh * half].rearrange("p (h d) -> p h d", h=nh)
                bv = tb[:, 0:nh * half].rearrange("p (h d) -> p h d", h=nh)
                kb.op(kb.dve, lambda: V.tensor_tensor(out=av, in0=x2, in1=cs, op=ALU.mult), reads=[self.pr[b], r_tab], writes=[tar])
                kb.op(kb.dve, lambda: V.tensor_tensor(out=bv, in0=x1, in1=sn, op=ALU.mult), reads=[self.pr[b], r_tab], writes=[tbr])
                kb.op(kb.pool, lambda: nc.gpsimd.tensor_tensor(out=dst[:, :, half:2 * half], in0=av, in1=bv, op=ALU.add), reads=[tar, tbr], writes=[dres])

            def transp(src_list, sres, dst_fn, dres):
                k = 0
                while k < len(src_list):
                    grp = src_list[k:k + 8]
                    b = 6 + tbank[0] % 2
                    tbank[0] += 1
                    pv = self.ps[b][:].bitcast(BF16)
                    for g, src in enumerate(grp):
                        kb.op(kb.pe, lambda: nc.tensor.transpose(pv[:, g * 128:(g + 1) * 128], src, self.ident_b()), reads=[sres, self.r_cst], writes=[self.pr[b]])
                    for g, src in enumerate(grp):
                        if evc[0] % 2 == 0:
                            kb.op(kb.act, lambda: nc.scalar.copy(dst_fn(k + g), pv[:, g * 128:(g + 1) * 128]), reads=[self.pr[b]], writes=[dres])
                        else:
                            kb.op(kb.dve, lambda: nc.vector.tensor_copy(dst_fn(k + g), pv[:, g * 128:(g + 1) * 128]), reads=[self.pr[b]], writes=[dres])
                        evc[0] += 1
                    k += 8

            for t4 in range(NT // 4):
                ht, hres = rh.next()
                kb.dma(kb.sp, ht[:], hTv[:, :, t4 * 512:(t4 + 1) * 512], writes=[hres])
                qTs, qTr = rqT.next()
                qiTs, qiTr = rqiT.next()
                kTs, kTr = rkT.next()
                kiTs, kiTr = rkiT.next()
                for cbk in range(4):
                    w, wr = rw.next()
                    kb.dma(kb.sp, w[:], wv[:, :, cbk * 512:(cbk + 1) * 512], writes=[wr])
                    for tt in range(4):
                        j = t4 * 4 + tt
                        b = proj(ht, hres, w, wr, tt, 512)
                        rope(b, 0, 4, 64, j, qtile[tt][:, cbk * 4:(cbk + 1) * 4, :], qres[tt])
                for tt in range(4):
                    transp([qtile[tt][:, h, :] for h in range(16)], qres[tt], lambda h: qTs[:, h, tt * 128:(tt + 1) * 128], qTr)
                w, wr = rw.next()
                kb.dma(kb.sp, w[:, :, 0:256], wv[:, :, 2048:2304], writes=[wr])
                for tt in range(4):
                    j = t4 * 4 + tt
                    b = proj(ht, hres, w, wr, tt, 256)
                    kr_, krr = rkr.next()
                    rope(b, 0, 1, 64, j, kr_[:].rearrange("p (h d) -> p h d", h=1), krr)
                    vs, vsr = rvs.next()
                    kb.op(kb.act, lambda: nc.scalar.copy(vs[:], self.ps[b][:, 128:256]), reads=[self.pr[b]], writes=[vsr])
                    kb.dma(kb.pool, self.v1d[:, j, :], vs[:], reads=[vsr])
                    transp([kr_[:]], krr, lambda h: kTs[:, tt * 128:(tt + 1) * 128], kTr)
                for cbk in range(2):
                    w, wr = rw.next()
                    kb.dma(kb.sp, w[:], wv[:, :, 2304 + cbk * 512:2304 + (cbk + 1) * 512], writes=[wr])
                    for tt in range(4):
                        j = t4 * 4 + tt
                        b = proj(ht, hres, w, wr, tt, 512)
                        qv = qtile[tt][:].rearrange("p a b -> p (a b)")[:, cbk * 512:(cbk + 1) * 512].rearrange("p (h d) -> p h d", h=8)
                        rope(b, 0, 8, 32, j, qv, qres[tt])
                for tt in range(4):
                    flat = qtile[tt][:].rearrange("p a b -> p (a b)")
                    transp([flat[:, pr * 128:(pr + 1) * 128] for pr in range(8)], qres[tt], lambda pr: qiTs[:, pr, tt * 128:(tt + 1) * 128], qiTr)
                w, wr = rw.next()
                kb.dma(kb.sp, w[:, :, 0:80], wv[:, :, 3328:3408], writes=[wr])
                for tt in range(4):
                    j = t4 * 4 + tt
                    b = proj(ht, hres, w, wr, tt, 80)
                    kr_, krr = rkr.next()
                    rope(b, 0, 1, 32, j, kr_[:, 0:64].rearrange("p (h d) -> p h d", h=1), krr)
                    kb.op(kb.pool, lambda: nc.gpsimd.tensor_copy(kr_[:, 64:128], kr_[:, 0:64]), reads=[krr], writes=[krr])
                    ws, wsr = rws.next()
                    kb.op(kb.act, lambda: nc.scalar.copy(ws[:], self.ps[b][:, 64:80]), reads=[self.pr[b]], writes=[wsr])
                    kb.dma(kb.pool, self.wd[:, j, :], ws[:], reads=[wsr])
                    transp([kr_[:]], krr, lambda h: kiTs[:, tt * 128:(tt + 1) * 128], kiTr)
                sl = slice(t4 * 512, (t4 + 1) * 512)
                kb.dma(kb.pool, self.qT1.rearrange("h p t -> p h t")[:, :, sl], qTs[:], reads=[qTr])
                kb.dma(kb.pool, self.qiT1.rearrange("h p t -> p h t")[:, :, sl], qiTs[:], reads=[qiTr])
                kb.dma(kb.pool, self.kT1d[:, sl], kTs[:], reads=[kTr])
                kb.dma(kb.pool, self.kiT2d[:, sl], kiTs[:], reads=[kiTr])

    def stage_dsa(self):
        kb, nc = self.kb, self.nc
        scale = HD ** -0.5
        import os
        ntiles = int(os.environ.get("DSA_TILES", NT))
        with contextlib.ExitStack() as ctx:
            T = lambda name, shape, dty: ctx.enter_context(_sbt(nc, name, shape, dty))
            kT1, kiT2 = T("kT1", [128, S], BF16), T("kiT2", [128, S], BF16)
            V1, wsb = T("V1", [128, NT, 129], BF16), T("wsb", [128, NT, 16], F32)
            id4 = T("id4", [128, 512], BF16)
            r_k = Res()
            kb.dma(kb.sp, kT1[:], self.kT1d[:, :], writes=[r_k])
            kb.dma(kb.sp, kiT2[:], self.kiT2d[:, :], writes=[r_k])
            kb.dma(kb.sp, V1[:, :, 0:128], self.v1d[:, :, :], writes=[r_k])
            kb.dma(kb.sp, wsb[:], self.wd[:, :, :], writes=[r_k])
            kb.op(kb.pool, lambda: nc.gpsimd.memset(V1[:, :, 128:129], 1.0), writes=[r_k])
            for g in range(4):
                kb.op(kb.pool, lambda: nc.gpsimd.tensor_copy(id4[:, g * 128:(g + 1) * 128], self.ident_b()), reads=[self.r_cst], writes=[r_k])
            rI = Ring(ctx, nc, "dI", 2, [128, S], F32)
            rWk = Ring(ctx, nc, "dWk", 2, [128, S], F32)
            rNM = Ring(ctx, nc, "dNM", 4, [128, S], BF16)
            rqi = Ring(ctx, nc, "dqi", 2, [128, 8, 128], BF16)
            rq = Ring(ctx, nc, "dq", 4, [128, 16, 128], BF16)
            rWd = Ring(ctx, nc, "dWd", 2, [128, 16, 128], BF16)
            rR = Ring(ctx, nc, "dR", 4, [128, 512], BF16)
            rPT = Ring(ctx, nc, "dPT", 4, [128, 512], BF16)
            rosb = Ring(ctx, nc, "dosb", 2, [128, 16, 128], BF16)
            roT = Ring(ctx, nc, "doT", 1, [128, 16, 512], BF16)
            rm8 = Ring(ctx, nc, "dm8", 4, [128, 8], F32)
            rthr = Ring(ctx, nc, "dthr", 4, [128, 1], F32)
            rrd = Ring(ctx, nc, "drd", 4, [128, 2], F32)
            ib_ = [0]
            sb_ = [0]
            tiles = {}
            oT_cur = [None, None]

            def phase_a_index(i):
                n_i = 128 * (i + 1)
                qiT, qir = rqi.next()
                qT, qr = rq.next()
                Wd, wdr = rWd.next()
                Isb, Ir = rI.next()
                kb.dma(kb.sp, qiT[:], self.qiT1.rearrange("h p t -> p h t")[:, :, i * 128:(i + 1) * 128], writes=[qir])
                kb.dma(kb.sp, qT[:], self.qT1.rearrange("h p t -> p h t")[:, :, i * 128:(i + 1) * 128], writes=[qr])
                for h in range(16):
                    kb.op(kb.pool, lambda: nc.gpsimd.tensor_scalar(out=Wd[:, h, :], in0=self.ident_b(), scalar1=wsb[:, i, h:h + 1], scalar2=0.25 * 0.125, op0=ALU.mult, op1=ALU.mult),
                          reads=[r_k, self.r_cst], writes=[wdr])

                def i1(sb, h):
                    nco = min(512, n_i - 512 * sb)
                    hp, pr = h % 2, h // 2
                    bI = ib_[0] % 3
                    ib_[0] += 1
                    kb.op(kb.pe, lambda: nc.tensor.matmul(self.ps[bI][:, 0:nco], lhsT=qiT[hp * 64:(hp + 1) * 64, pr, :], rhs=kiT2[hp * 64:(hp + 1) * 64, sb * 512:sb * 512 + nco], start=True, stop=True),
                          reads=[qir, r_k], writes=[self.pr[bI]])
                    return bI

                def i2(sb, h, bI):
                    nco = min(512, n_i - 512 * sb)
                    bA = 3 + (sb % 2)
                    R, rr = rR.next()
                    kb.op(kb.act, lambda: nc.scalar.activation(out=R[:, 0:nco], in_=self.ps[bI][:, 0:nco], func=ACT.Relu), reads=[self.pr[bI]], writes=[rr])
                    kb.op(kb.pe, lambda: nc.tensor.matmul(self.ps[bA][:, 0:nco], lhsT=Wd[:, h, :], rhs=R[:, 0:nco], start=(h == 0), stop=(h == 15)),
                          reads=[wdr, rr], writes=[self.pr[bA]])
                    if h == 15:
                        kb.op(kb.act, lambda: nc.scalar.copy(Isb[:, sb * 512:sb * 512 + nco], self.ps[bA][:, 0:nco]), reads=[self.pr[bA]], writes=[Ir])

                pend = []
                for sb in range((n_i + 511) // 512):
                    for h in range(16):
                        pend.append((sb, h, i1(sb, h)))
                        if len(pend) > 2:
                            i2(*pend.pop(0))
                while pend:
                    i2(*pend.pop(0))
                kb.op(kb.dve, lambda: nc.vector.memset(Isb[0:64, n_i - 64:n_i], NEG), writes=[Ir])
                tiles[i] = dict(qT=qT, qr=qr, Isb=Isb, Ir=Ir, n=n_i)

            def phase_a_topk(group):
                chains = []
                for i in group:
                    t = tiles[i]
                    thr, thr_r = rthr.next()
                    t["thr"], t["thr_r"] = thr, thr_r
                    if t["n"] <= 256:
                        kb.op(kb.dve, lambda: nc.vector.memset(thr[:], -1.0e29), writes=[thr_r])
                    else:
                        Wk, wkr = rWk.next()
                        chains.append(dict(t=t, cur=t["Isb"], cur_r=t["Ir"], Wk=Wk, wkr=wkr))
                for rnd in range(32):
                    if rnd > 0:
                        yield
                    for c in chains:
                        t, n_i = c["t"], c["t"]["n"]
                        m8, m8r = rm8.next()
                        c["m8"], c["m8r"] = m8, m8r
                        cur, cur_r = c["cur"], c["cur_r"]
                        kb.op(kb.dve, lambda: nc.vector.max(out=m8[:], in_=cur[:, 0:n_i]), reads=[cur_r], writes=[m8r])
                    for c in chains:
                        t, n_i = c["t"], c["t"]["n"]
                        m8, m8r, cur, cur_r, Wk, wkr = c["m8"], c["m8r"], c["cur"], c["cur_r"], c["Wk"], c["wkr"]
                        if rnd < 31:
                            kb.op(kb.dve, lambda: nc.vector.match_replace(out=Wk[:, 0:n_i], in_to_replace=m8[:], in_values=cur[:, 0:n_i], imm_value=NEG), reads=[cur_r, m8r], writes=[wkr])
                            c["cur"], c["cur_r"] = Wk, wkr
                        else:
                            kb.op(kb.dve, lambda: nc.vector.tensor_copy(t["thr"][:], m8[:, 7:8]), reads=[m8r], writes=[t["thr_r"]])
                for i in group:
                    t = tiles[i]
                    NM, nmr = rNM.next()
                    kb.op(kb.dve, lambda: nc.vector.tensor_scalar(out=NM[:, 0:t["n"]], in0=t["Isb"][:, 0:t["n"]], scalar1=t["thr"][:, 0:1], scalar2=-30000.0, op0=ALU.is_lt, op1=ALU.mult),
                          reads=[t["Ir"], t["thr_r"]], writes=[nmr])
                    t["NM"], t["nmr"] = NM, nmr

            def phase_b(i):
                t = tiles.pop(i)
                qT, qr, NM, nmr = t["qT"], t["qr"], t["NM"], t["nmr"]
                tt = i % 4
                if tt == 0:
                    oT_cur[0], oT_cur[1] = roT.next()
                oTs, oTr = oT_cur
                osb, osr = rosb.next()

                def b1(hp_, j):
                    bS = (0, 1, 6)[sb_[0] % 3]
                    sb_[0] += 1
                    kb.op(kb.pe, lambda: nc.tensor.matmul(self.ps[bS][:, 0:256], lhsT=kT1[:, j * 128:(j + 1) * 128], rhs=qT[:, hp_ * 2:(hp_ + 1) * 2, :].rearrange("p h t -> p (h t)"), start=True, stop=False),
                          reads=[r_k, qr], writes=[self.pr[bS]])
                    kb.op(kb.pe, lambda: nc.tensor.matmul(self.ps[bS][:, 0:256], lhsT=NM[:, j * 128:(j + 1) * 128], rhs=id4[:, 0:256], start=False, stop=True),
                          reads=[nmr, r_k], writes=[self.pr[bS]])
                    return bS

                def b2(hp_, j, bS):
                    PT, ptr = rPT.next()
                    kb.op(kb.act, lambda: nc.scalar.activation(out=PT[:, 0:256], in_=self.ps[bS][:, 0:256], func=ACT.Exp, scale=scale), reads=[self.pr[bS]], writes=[ptr])
                    for hh in range(2):
                        bO = 2 + 2 * (hp_ % 2) + hh
                        kb.op(kb.pe, lambda: nc.tensor.matmul(self.ps[bO][:, 0:129], lhsT=PT[:, hh * 128:(hh + 1) * 128], rhs=V1[:, j, :], start=(j == 0), stop=(j == i)),
                              reads=[ptr, r_k], writes=[self.pr[bO]])

                def fin(hp_):
                    for hh in range(2):
                        bO = 2 + 2 * (hp_ % 2) + hh
                        rd, rdr = rrd.next()
                        kb.op(kb.dve, lambda: nc.vector.reciprocal(rd[:, 0:1], self.ps[bO][:, 128:129]), reads=[self.pr[bO]], writes=[rdr])
                        kb.op(kb.act, lambda: nc.scalar.activation(out=osb[:, hp_ * 2 + hh, :], in_=self.ps[bO][:, 0:128], func=ACT.Copy, scale=rd[:, 0:1]), reads=[self.pr[bO], rdr], writes=[osr])

                pend = []
                for hp_ in range(8):
                    for j in range(i + 1):
                        pend.append((hp_, j, b1(hp_, j)))
                        if len(pend) > 2:
                            p = pend.pop(0)
                            b2(*p)
                            if p[1] == i:
                                yield
                                fin(p[0])
                while pend:
                    p = pend.pop(0)
                    b2(*p)
                    if p[1] == i:
                        yield
                        fin(p[0])
                for half in range(2):
                    pv = self.ps[7][:].bitcast(BF16)
                    for g in range(8):
                        kb.op(kb.pe, lambda: nc.tensor.transpose(pv[:, g * 128:(g + 1) * 128], osb[:, half * 8 + g, :], self.ident_b()), reads=[osr, self.r_cst], writes=[self.pr[7]])
                    for g in range(8):
                        kb.op(kb.act, lambda: nc.scalar.copy(oTs[:, half * 8 + g, tt * 128:(tt + 1) * 128], pv[:, g * 128:(g + 1) * 128]), reads=[self.pr[7]], writes=[oTr])
                if tt == 3 or i == ntiles - 1:
                    t4 = i // 4
                    kb.dma(kb.pool, self.oT.rearrange("k p t -> p k t")[:, :, t4 * 512:(t4 + 1) * 512], oTs[:], reads=[oTr])

            groups = [list(range(g, min(g + 2, ntiles))) for g in range(0, ntiles, 2)]
            for gi, grp in enumerate(groups):
                for i in grp:
                    phase_a_index(i)
                tg = phase_a_topk(grp)
                if gi > 0:
                    for i in groups[gi - 1]:
                        for _ in phase_b(i):
                            next(tg, None)
                            next(tg, None)
                for _ in tg:
                    pass
            for i in groups[-1]:
                for _ in phase_b(i):
                    pass


def make_in_maps(inputs):
    f = lambda a: np.ascontiguousarray(a)
    cst = make_consts()
    maps = []
    for b in range(8):
        maps.append({
            "x": f(inputs["x"][b]), "c": f(inputs["c"][b].reshape(16, 128)),
            "pos": f(inputs["positions"][b].reshape(32, 128).astype(np.int32)),
            "ada_w": inputs["ada_w"], "ada_b": inputs["ada_b"], "norm_g": inputs["norm_g"],
            "mix_w_out": inputs["mix_w_out"], "even_w_in": f(inputs["even_w_in"][0]),
            "even_b": f(inputs["even_b_forget"].reshape(1, 8)), "odd_w_in": f(inputs["odd_w_in"][0]),
            "ff_w1": inputs["ff_w1"], "ff_w2": inputs["ff_w2"], "cst": cst,
        })
    return maps


def kernel(**inputs):
    inputs = {k: np.asarray(v) for k, v in inputs.items()}
    prog = Prog()
    res = run_bass_kernel_spmd(prog.nc, make_in_maps(inputs), core_ids=list(range(8)))
    return np.stack([r["out"] for r in res.results], axis=0).astype(np.float32)
```

```python
import contextlib
import numpy as np
import concourse.bass as bass
import concourse.mybir as mybir
from concourse.bass_utils import run_bass_kernel_spmd

ACT = mybir.ActivationFunctionType
ALU = mybir.AluOpType
AX = mybir.AxisListType
F32, BF16, I32 = mybir.dt.float32, mybir.dt.bfloat16, mybir.dt.int32

S, D, DFF, HD = 4096, 2048, 8192, 128
NT = S // 128
KC = D // 128
EVEN_W, ODD_W = 6152, 3408
EPS = 1e-6
NEG = -1.0e30
SB_WIN = 3
NSLOT = 8
CAST_OVERLAP = True

C_ID, C_TRI, C_TRS, C_LOW, C_ONE, C_INV = 0, 128, 256, 384, 512, 640
C_W = 704


def make_consts():
    c = np.zeros((128, C_W), np.float32)
    a = np.arange(128)
    c[:, C_ID:C_ID + 128] = np.eye(128)
    c[:, C_TRI:C_TRI + 128] = (a[:, None] <= a[None, :])
    c[:, C_TRS:C_TRS + 128] = (a[:, None] < a[None, :])
    c[:, C_LOW:C_LOW + 128] = (a[:, None] >= a[None, :])
    c[:, C_ONE:C_ONE + 128] = 1.0
    inv = (10000.0 ** (-np.arange(64, dtype=np.float32) / 64)).astype(np.float32)
    c[:, C_INV:C_INV + 64] = inv[None, :]
    return c


_uid = [0]


def _sbt(nc, name, shape, dty):
    _uid[0] += 1
    return nc.sbuf_tensor(f"{name}_u{_uid[0]}", shape, dty)


class Res:
    __slots__ = ("w", "r", "x")

    def __init__(self, x=False):
        self.w = None
        self.r = {}
        self.x = x


class Eng:
    def __init__(self, name, obj, sem, is_pe=False):
        self.name, self.obj, self.sem, self.is_pe = name, obj, sem, is_pe
        self.n = 0
        self.waited = {}
        self.dma_sems, self.dma_cnt, self.dma_i = [], [], 0


class KB:
    def __init__(self, nc):
        self.nc = nc
        self.stack = contextlib.ExitStack()
        mk = lambda nm: self.stack.enter_context(nc.semaphore(nm))
        self.pe = Eng("pe", nc.tensor, mk("s_pe"), True)
        self.act = Eng("act", nc.scalar, mk("s_act"))
        self.dve = Eng("dve", nc.vector, mk("s_dve"))
        self.pool = Eng("pool", nc.gpsimd, mk("s_pool"))
        self.sp = Eng("sp", nc.sync, mk("s_sp"))
        self.engs = [self.pe, self.act, self.dve, self.pool, self.sp]
        self.queues = [self.sp, self.pool, self.act]
        for q in self.queues:
            q.dma_sems = [mk(f"d_{q.name}{i}") for i in range(NSLOT)]
            q.dma_cnt = [0] * NSLOT
        self.n_ins = 0

    def _wait(self, eng, tk):
        sem, val, src = tk
        if src is eng and eng.is_pe:
            return
        key = sem.name
        if eng.waited.get(key, 0) >= val:
            return
        eng.obj.wait_ge(sem, val)
        eng.waited[key] = val

    def _deps(self, eng, reads, writes):
        for r in reads:
            if r.w is not None:
                self._wait(eng, r.w)
            if r.x:
                for k, tk in r.r.items():
                    if k != eng.name:
                        self._wait(eng, tk)
        for w in writes:
            if w.w is not None:
                self._wait(eng, w.w)
            for tk in w.r.values():
                self._wait(eng, tk)

    def _mark(self, key, tk, reads, writes):
        for r in reads:
            r.r[key] = tk
        for w in writes:
            w.w = tk
            w.r = {}

    def op(self, eng, fn, reads=(), writes=()):
        self._deps(eng, reads, writes)
        ins = fn()
        eng.n += 1
        ins.then_inc(eng.sem, 1)
        tk = (eng.sem, eng.n, eng)
        self._mark(eng.name, tk, reads, writes)
        self.n_ins += 1
        return tk

    def dma(self, q, out, in_, reads=(), writes=()):
        slot = q.dma_i % NSLOT
        q.dma_i += 1
        sem = q.dma_sems[slot]
        if q.dma_cnt[slot] > 0:
            self._wait(q, (sem, 16 * q.dma_cnt[slot], None))
        self._deps(q, reads, writes)
        q.obj.dma_start(out=out, in_=in_).then_inc(sem, 16)
        q.dma_cnt[slot] += 1
        tk = (sem, 16 * q.dma_cnt[slot], None)
        self._mark(sem.name, tk, reads, writes)
        self.n_ins += 1
        return tk

    def barrier(self):
        for e in self.engs:
            for f in self.engs:
                if f is not e and f.n > 0:
                    self._wait(e, (f.sem, f.n, f))
            for q in self.queues:
                for i in range(NSLOT):
                    if q.dma_cnt[i] > 0:
                        self._wait(e, (q.dma_sems[i], 16 * q.dma_cnt[i], None))


class Ring:
    def __init__(self, ctx, nc, name, n, shape, dtype):
        self.t = [ctx.enter_context(_sbt(nc, f"{name}{i}", shape, dtype)) for i in range(n)]
        self.r = [Res() for _ in range(n)]
        self.i = 0

    def next(self):
        k = self.i % len(self.t)
        self.i += 1
        return self.t[k], self.r[k]


class Prog:
    def __init__(self, stages=None, xsrc_is_out=False):
        self.stages = stages
        nc = self.nc = bass.Bass("TRN2", target_bir_lowering=False)
        kb = self.kb = KB(nc)
        import os
        dbg = set(os.environ.get("DEBUG_OUT", "").split(","))
        dt = lambda name, shape, dty, kind: nc.dram_tensor(name, shape, dty, kind=("ExternalOutput" if name in dbg else kind)).ap()
        I, O, N = "ExternalInput", "ExternalOutput", "Internal"
        self.x = dt("x", [S, D], F32, I)
        self.c = dt("c", [16, 128], F32, I)
        self.pos = dt("pos", [32, 128], I32, I)
        self.ada_w = dt("ada_w", [2, D, 6 * D], F32, I)
        self.ada_b = dt("ada_b", [2, 6 * D], F32, I)
        self.norm_g = dt("norm_g", [2, 4, D], F32, I)
        self.mix_w_out = dt("mix_w_out", [2, D, D], F32, I)
        self.even_w_in = dt("even_w_in", [D, EVEN_W], F32, I)
        self.even_b = dt("even_b", [1, 8], F32, I)
        self.odd_w_in = dt("odd_w_in", [D, ODD_W], F32, I)
        self.ff_w1 = dt("ff_w1", [2, D, DFF], F32, I)
        self.ff_w2 = dt("ff_w2", [2, DFF, D], F32, I)
        self.cst = dt("cst", [128, C_W], F32, I)
        self.out = dt("out", [S, D], F32, O)
        self.wb_in0 = dt("wb_in0", [D, EVEN_W], BF16, N)
        self.wb_in1 = dt("wb_in1", [D, ODD_W], BF16, N)
        self.wb_out = [dt(f"wb_out{l}", [D, D], BF16, N) for l in range(2)]
        self.wb_f1 = [dt(f"wb_f1{l}", [D, DFF], BF16, N) for l in range(2)]
        self.wb_f2 = [dt(f"wb_f2{l}", [DFF, D], BF16, N) for l in range(2)]
        self.modd = dt("modd", [2, 6 * D], F32, N)
        self.hT = dt("hT", [KC, 128, S], BF16, N)
        self.oT = dt("oT", [KC, 128, S], BF16, N)
        self.qkT = dt("qkT", [32, 128, S], BF16, N)
        self.vt = dt("vt", [16, 128, NT, 128], BF16, N)
        self.qT1 = dt("qT1", [16, 128, S], BF16, N)
        self.qiT1 = dt("qiT1", [8, 128, S], BF16, N)
        self.dbg = dt("dbg", [128, 512], F32, N)
        self.kT1d = dt("kT1d", [128, S], BF16, N)
        self.kiT2d = dt("kiT2d", [128, S], BF16, N)
        self.v1d = dt("v1d", [128, NT, 128], BF16, N)
        self.wd = dt("wd", [128, NT, 16], F32, N)
        self.ps = [nc.alloc_psum_tensor(f"ps{b}", [128, 512], F32) for b in range(8)]
        self.pr = [Res(True) for _ in range(8)]
        A = nc.alloc_sbuf_tensor
        self.cst_sb = A("cst_sb", [128, C_W], F32)
        self.cb = A("cb", [128, 640], BF16)
        self.eps_t = A("eps_t", [128, 1], F32)
        self.one_t = A("one_t", [128, 1], F32)
        self.zero_t = A("zero_t", [128, 1], F32)
        self.acol = A("acol", [128, 16], F32)
        self.bcol = A("bcol", [128, 16], F32)
        self.G = A("G", [128, D], F32)
        self.Pcol = A("Pcol", [128, 256], F32)
        self.Pb = A("Pb", [128, 256], F32)
        self.r_cst, self.r_cols, self.r_G, self.r_P = Res(), Res(), Res(), Res()
        self.build()

    def ident_f(self, n=128):
        return self.cst_sb[0:n, C_ID:C_ID + n]

    def ident_b(self):
        return self.cb[:, C_ID:C_ID + 128]

    def on(self, name):
        return self.stages is None or name in self.stages

    def build(self):
        kb, nc = self.kb, self.nc
        kb.dma(kb.sp, self.cst_sb[:], self.cst[:, :], writes=[self.r_cst])
        kb.op(kb.dve, lambda: nc.vector.tensor_copy(self.cb[:], self.cst_sb[:, 0:640]), reads=[self.r_cst], writes=[self.r_cst])
        kb.op(kb.dve, lambda: nc.vector.memset(self.eps_t[:], EPS), writes=[self.r_cst])
        kb.op(kb.dve, lambda: nc.vector.memset(self.one_t[:], 1.0), writes=[self.r_cst])
        kb.op(kb.dve, lambda: nc.vector.memset(self.zero_t[:], 0.0), writes=[self.r_cst])
        kb.barrier()
        if self.on("cast"):
            self.stage_cast()
            kb.barrier()
        if self.on("ada"):
            self.stage_ada()
            kb.barrier()
        xsrc = self.x
        for l in range(2):
            if self.on(f"mix{l}"):
                self.stage_normT(l, "a", xsrc)
                kb.barrier()
                if l == 0:
                    self.stage_inproj0()
                    kb.barrier()
                    self.stage_attn0()
                    kb.barrier()
                else:
                    self.stage_inproj1()
                    kb.barrier()
                    self.stage_dsa()
                    kb.barrier()
                self.stage_outproj(l, xsrc)
                kb.barrier()
                xsrc = self.out
            if self.on(f"ffn{l}"):
                self.stage_normT(l, "m", xsrc)
                kb.barrier()
                if self.stages is None or "skipffn" not in self.stages:
                    self.stage_ffn(l, xsrc)
                    kb.barrier()
                    xsrc = self.out
        kb.barrier()

    def stage_cast(self):
        kb, nc = self.kb, self.nc
        jobs = [(self.even_w_in, self.wb_in0)]
        if not (CAST_OVERLAP and self.on("mix0")):
            jobs += self.late_cast_jobs()
        CW = 2048
        with contextlib.ExitStack() as ctx:
            rf = Ring(ctx, nc, "cf", 4, [128, CW], F32)
            rb = Ring(ctx, nc, "cbf", 4, [128, CW], BF16)
            k = 0
            for src, dst in jobs:
                R, C = src.shape
                for rbk in range(R // 128):
                    for c0 in range(0, C, CW):
                        cw = min(CW, C - c0)
                        tf, rfr = rf.next()
                        tb, rbr = rb.next()
                        kb.dma(kb.sp, tf[:, 0:cw], src[rbk * 128:(rbk + 1) * 128, c0:c0 + cw], writes=[rfr])
                        if k % 2 == 0:
                            kb.op(kb.act, lambda: nc.scalar.copy(tb[:, 0:cw], tf[:, 0:cw]), reads=[rfr], writes=[rbr])
                        else:
                            kb.op(kb.dve, lambda: nc.vector.tensor_copy(tb[:, 0:cw], tf[:, 0:cw]), reads=[rfr], writes=[rbr])
                        kb.dma(kb.pool, dst[rbk * 128:(rbk + 1) * 128, c0:c0 + cw], tb[:, 0:cw], reads=[rbr])
                        k += 1

    def late_cast_jobs(self):
        return [(self.mix_w_out[0], self.wb_out[0]), (self.ff_w1[0], self.wb_f1[0]), (self.ff_w2[0], self.wb_f2[0]),
                (self.odd_w_in, self.wb_in1), (self.mix_w_out[1], self.wb_out[1]), (self.ff_w1[1], self.wb_f1[1]),
                (self.ff_w2[1], self.wb_f2[1])]

    def cast_gen(self, ctx, jobs):
        kb, nc = self.kb, self.nc
        CW = 2048
        rf = Ring(ctx, nc, "cgf", 3, [128, CW], F32)
        rb = Ring(ctx, nc, "cgb", 3, [128, CW], BF16)
        pieces = []
        for src, dst in jobs:
            R, C = src.shape
            for rbk in range(R // 128):
                for c0 in range(0, C, CW):
                    pieces.append((src, dst, rbk, c0, min(CW, C - c0)))
        loaded = []
        for k in range(len(pieces) + 2):
            if k < len(pieces):
                src, dst, rbk, c0, cw = pieces[k]
                tf, rfr = rf.next()
                kb.dma(kb.sp, tf[:, 0:cw], src[rbk * 128:(rbk + 1) * 128, c0:c0 + cw], writes=[rfr])
                loaded.append((pieces[k], tf, rfr))
            if k >= 2:
                (src, dst, rbk, c0, cw), tf, rfr = loaded.pop(0)
                tb, rbr = rb.next()
                kb.op(kb.dve, lambda: nc.vector.tensor_copy(tb[:, 0:cw], tf[:, 0:cw]), reads=[rfr], writes=[rbr])
                kb.dma(kb.sp, dst[rbk * 128:(rbk + 1) * 128, c0:c0 + cw], tb[:, 0:cw], reads=[rbr])
            yield

    def stage_ada(self):
        kb, nc = self.kb, self.nc
        with contextlib.ExitStack() as ctx:
            T = lambda name, shape, dty: ctx.enter_context(_sbt(nc, name, shape, dty))
            c16, sg16, csT = T("c16", [16, 128], F32), T("sg16", [16, 128], F32), T("csT", [128, 16], F32)
            brow, mrow = T("brow", [1, 6 * D], F32), T("mrow", [1, 6 * D], F32)
            r_c, r_b, r_m = Res(), Res(), Res()
            wr = Ring(ctx, nc, "adaw", 3, [128, 3072], F32)
            kb.dma(kb.sp, c16[:], self.c[:, :], writes=[r_c])
            kb.op(kb.act, lambda: nc.scalar.activation(out=sg16[:], in_=c16[:], func=ACT.Sigmoid), reads=[r_c], writes=[r_c])
            kb.op(kb.dve, lambda: nc.vector.tensor_mul(c16[:], c16[:], sg16[:]), reads=[r_c], writes=[r_c])
            kb.op(kb.pe, lambda: nc.tensor.transpose(self.ps[0][:, 0:16], c16[:], self.ident_f(16)), reads=[r_c, self.r_cst], writes=[self.pr[0]])
            kb.op(kb.dve, lambda: nc.vector.tensor_copy(csT[:], self.ps[0][:, 0:16]), reads=[self.pr[0]], writes=[r_c])
            for l in range(2):
                kb.dma(kb.sp, brow[:], self.ada_b[l:l + 1, :], writes=[r_b])
                for g in range(4):
                    for kc in range(KC):
                        wt, wres = wr.next()
                        kb.dma(kb.sp, wt[:], self.ada_w[l, kc * 128:(kc + 1) * 128, g * 3072:(g + 1) * 3072], writes=[wres])
                        for b in range(6):
                            kb.op(kb.pe, lambda b=b: nc.tensor.matmul(self.ps[b][0:1, :], lhsT=csT[:, kc:kc + 1], rhs=wt[:, b * 512:(b + 1) * 512],
                                                                   start=(kc == 0), stop=(kc == KC - 1)),
                                  reads=[r_c, wres], writes=[self.pr[b]])
                    for b in range(6):
                        c0 = g * 3072 + b * 512
                        kb.op(kb.dve, lambda b=b, c0=c0: nc.vector.tensor_tensor(out=mrow[0:1, c0:c0 + 512], in0=self.ps[b][0:1, :], in1=brow[0:1, c0:c0 + 512], op=ALU.add),
                              reads=[self.pr[b], r_b], writes=[r_m])
                kb.dma(kb.pool, self.modd[l:l + 1, :], mrow[:], reads=[r_m])

    def prep_cols(self, ctx, l, which):
        kb, nc = self.kb, self.nc
        off = 0 if which == "a" else 3
        gi = 0 if which == "a" else 2
        T = lambda name, shape, dty: ctx.enter_context(_sbt(nc, name, shape, dty))
        sc16, sh16, gm16 = T("sc16", [16, 128], F32), T("sh16", [16, 128], F32), T("gm16", [16, 128], F32)
        r = Res()
        v16 = lambda ap: ap.rearrange("(c p) -> c p", p=128)
        kb.dma(kb.sp, sh16[:], v16(self.modd[l, off * D:(off + 1) * D]), writes=[r])
        kb.dma(kb.sp, sc16[:], v16(self.modd[l, (off + 1) * D:(off + 2) * D]), writes=[r])
        kb.dma(kb.sp, gm16[:], v16(self.norm_g[l, gi, :]), writes=[r])
        kb.op(kb.dve, lambda: nc.vector.scalar_tensor_tensor(out=sc16[:], in0=sc16[:], scalar=1.0, in1=gm16[:], op0=ALU.add, op1=ALU.mult), reads=[r], writes=[r])
        kb.op(kb.pe, lambda: nc.tensor.transpose(self.ps[0][:, 0:16], sc16[:], self.ident_f(16)), reads=[r, self.r_cst], writes=[self.pr[0]])
        kb.op(kb.pe, lambda: nc.tensor.transpose(self.ps[1][:, 0:16], sh16[:], self.ident_f(16)), reads=[r, self.r_cst], writes=[self.pr[1]])
        kb.op(kb.dve, lambda: nc.vector.tensor_copy(self.acol[:], self.ps[0][:, 0:16]), reads=[self.pr[0]], writes=[self.r_cols])
        kb.op(kb.dve, lambda: nc.vector.tensor_copy(self.bcol[:], self.ps[1][:, 0:16]), reads=[self.pr[1]], writes=[self.r_cols])

    def prep_G(self, ctx, l, which):
        kb, nc = self.kb, self.nc
        off = 2 if which == "a" else 5
        gi = 1 if which == "a" else 3
        T = lambda name, shape, dty: ctx.enter_context(_sbt(nc, name, shape, dty))
        grow, gmrow = T("grow", [1, D], F32), T("gmrow", [1, D], F32)
        r = Res()
        kb.dma(kb.sp, grow[:], self.modd[l:l + 1, off * D:(off + 1) * D], writes=[r])
        kb.dma(kb.sp, gmrow[:], self.norm_g[l, gi:gi + 1, :], writes=[r])
        kb.op(kb.dve, lambda: nc.vector.tensor_mul(grow[:], grow[:], gmrow[:]), reads=[r], writes=[r])
        for n in range(4):
            kb.op(kb.pe, lambda n=n: nc.tensor.matmul(self.ps[n][:, :], lhsT=self.cst_sb[0:1, C_ONE:C_ONE + 128], rhs=grow[0:1, n * 512:(n + 1) * 512], start=True, stop=True),
                  reads=[r, self.r_cst], writes=[self.pr[n]])
            kb.op(kb.dve, lambda n=n: nc.vector.tensor_copy(self.G[:, n * 512:(n + 1) * 512], self.ps[n][:, :]), reads=[self.pr[n]], writes=[self.r_G])

    def stage_normT(self, l, which, xsrc):
        kb, nc = self.kb, self.nc
        with contextlib.ExitStack() as ctx:
            T = lambda name, shape, dty: ctx.enter_context(_sbt(nc, name, shape, dty))
            with contextlib.ExitStack() as c2:
                self.prep_cols(c2, l, which)
            kb.barrier()
            rx = Ring(ctx, nc, "nx", 3, [128, D], F32)
            rxn = Ring(ctx, nc, "nxn", 2, [128, D], BF16)
            rst = Ring(ctx, nc, "nst", 4, [128, 4], F32)
            rh = Ring(ctx, nc, "nh", 2, [128, KC, 512], BF16)
            junk = T("njunk", [128, D], BF16)
            rj = Res()
            pbank = 0
            for t4 in range(NT // 4):
                hts, hres = rh.next()
                for tt in range(4):
                    t = t4 * 4 + tt
                    xt, xr = rx.next()
                    xn, xnr = rxn.next()
                    st, sr = rst.next()
                    kb.dma(kb.sp, xt[:], xsrc[t * 128:(t + 1) * 128, :], writes=[xr])
                    kb.op(kb.act, lambda: nc.scalar.activation(out=junk[:], in_=xt[:], func=ACT.Square, accum_out=st[:, 0:1]), reads=[xr], writes=[rj, sr])
                    kb.op(kb.act, lambda: nc.scalar.activation(out=st[:, 1:2], in_=st[:, 0:1], func=ACT.Sqrt, bias=self.eps_t[:], scale=1.0 / D), reads=[sr], writes=[sr])
                    kb.op(kb.dve, lambda: nc.vector.reciprocal(st[:, 2:3], st[:, 1:2]), reads=[sr], writes=[sr])
                    kb.op(kb.dve, lambda: nc.vector.tensor_scalar(out=xn[:], in0=xt[:], scalar1=st[:, 2:3], scalar2=None, op0=ALU.mult), reads=[xr, sr], writes=[xnr])
                    for half in range(2):
                        b = pbank % 4
                        pbank += 1
                        pv = self.ps[b][:].bitcast(BF16)
                        for k8 in range(8):
                            kc = half * 8 + k8
                            kb.op(kb.pe, lambda kc=kc, k8=k8, pv=pv: nc.tensor.transpose(pv[:, k8 * 128:(k8 + 1) * 128], xn[:, kc * 128:(kc + 1) * 128], self.ident_b()),
                                  reads=[xnr, self.r_cst], writes=[self.pr[b]])
                        for k8 in range(8):
                            kc = half * 8 + k8
                            dst = hts[:, kc, tt * 128:(tt + 1) * 128]
                            src = pv[:, k8 * 128:(k8 + 1) * 128]
                            if k8 % 2 == 0:
                                kb.op(kb.act, lambda dst=dst, src=src, kc=kc: nc.scalar.activation(out=dst, in_=src, func=ACT.Identity, bias=self.bcol[:, kc:kc + 1], scale=self.acol[:, kc:kc + 1]),
                                      reads=[self.pr[b], self.r_cols], writes=[hres])
                            else:
                                kb.op(kb.dve, lambda dst=dst, src=src, kc=kc: nc.vector.tensor_scalar(out=dst, in0=src, scalar1=self.acol[:, kc:kc + 1], scalar2=self.bcol[:, kc:kc + 1], op0=ALU.mult, op1=ALU.add),
                                      reads=[self.pr[b], self.r_cols], writes=[hres])
                kb.dma(kb.pool, self.hT.rearrange("k p t -> p k t")[:, :, t4 * 512:(t4 + 1) * 512], hts[:], reads=[hres])

    def rstd_from_ss(self, st, sr, ncols):
        kb, nc = self.kb, self.nc
        kb.op(kb.dve, lambda: nc.vector.tensor_reduce(out=st[:, 4:5], in_=st[:, 0:ncols], axis=AX.X, op=ALU.add), reads=[sr], writes=[sr])
        kb.op(kb.act, lambda: nc.scalar.activation(out=st[:, 5:6], in_=st[:, 4:5], func=ACT.Sqrt, bias=self.eps_t[:], scale=1.0 / D), reads=[sr], writes=[sr])
        kb.op(kb.dve, lambda: nc.vector.reciprocal(st[:, 6:7], st[:, 5:6]), reads=[sr], writes=[sr])

    def stage_outproj(self, l, xsrc):
        kb, nc = self.kb, self.nc
        with contextlib.ExitStack() as ctx:
            T = lambda name, shape, dty: ctx.enter_context(_sbt(nc, name, shape, dty))
            with contextlib.ExitStack() as c2:
                self.prep_G(c2, l, "a")
            kb.barrier()
            wo = T("wo", [128, KC, D], BF16)
            r_wo = Res()
            wv = self.wb_out[l].rearrange("(k p) c -> p k c", p=128)
            for n in range(4):
                kb.dma(kb.sp, wo[:, :, n * 512:(n + 1) * 512], wv[:, :, n * 512:(n + 1) * 512], writes=[r_wo])
            ro = Ring(ctx, nc, "oo", 2, [128, KC, 512], BF16)
            rx = Ring(ctx, nc, "ox", 2, [128, D], F32)
            rt1 = Ring(ctx, nc, "ot1", 2, [128, D], F32)
            rst = Ring(ctx, nc, "ost", 4, [128, 8], F32)
            junk = T("ojunk", [128, 512], BF16)
            rj = Res()
            for t4 in range(NT // 4):
                ot, ores = ro.next()
                kb.dma(kb.sp, ot[:], self.oT.rearrange("k p t -> p k t")[:, :, t4 * 512:(t4 + 1) * 512], writes=[ores])
                for tt in range(4):
                    t = t4 * 4 + tt
                    xt, xr = rx.next()
                    t1, t1r = rt1.next()
                    st, sr = rst.next()
                    kb.dma(kb.sp, xt[:], xsrc[t * 128:(t + 1) * 128, :], writes=[xr])
                    base = (t % 2) * 4
                    for n in range(4):
                        b = base + n
                        for kc in range(KC):
                            kb.op(kb.pe, lambda kc=kc, b=b, n=n: nc.tensor.matmul(self.ps[b][:, :], lhsT=ot[:, kc, tt * 128:(tt + 1) * 128], rhs=wo[:, kc, n * 512:(n + 1) * 512],
                                                                             start=(kc == 0), stop=(kc == KC - 1)),
                                  reads=[ores, r_wo], writes=[self.pr[b]])
                        kb.op(kb.act, lambda b=b, n=n: nc.scalar.activation(out=junk[:], in_=self.ps[b][:, :], func=ACT.Square, accum_out=st[:, n:n + 1]), reads=[self.pr[b]], writes=[rj, sr])
                        kb.op(kb.dve, lambda b=b, n=n: nc.vector.tensor_tensor(out=t1[:, n * 512:(n + 1) * 512], in0=self.ps[b][:, :], in1=self.G[:, n * 512:(n + 1) * 512], op=ALU.mult),
                              reads=[self.pr[b], self.r_G], writes=[t1r])
                    self.rstd_from_ss(st, sr, 4)
                    kb.op(kb.dve, lambda: nc.vector.scalar_tensor_tensor(out=t1[:], in0=t1[:], scalar=st[:, 6:7], in1=xt[:], op0=ALU.mult, op1=ALU.add), reads=[t1r, sr, xr], writes=[t1r])
                    kb.dma(kb.pool, self.out[t * 128:(t + 1) * 128, :], t1[:], reads=[t1r])

    def stage_ffn(self, l, xsrc):
        kb, nc = self.kb, self.nc
        with contextlib.ExitStack() as ctx:
            T = lambda name, shape, dty: ctx.enter_context(_sbt(nc, name, shape, dty))
            with contextlib.ExitStack() as c2:
                self.prep_G(c2, l, "m")
            kb.barrier()
            rh = Ring(ctx, nc, "fh", 1, [128, KC, 512], BF16)
            uT = T("uT", [128, 64, 512], BF16)
            r_u = [Res() for _ in range(64)]
            rw1 = Ring(ctx, nc, "fw1", 3, [128, KC, 256], BF16)
            rw2 = Ring(ctx, nc, "fw2", 3, [128, 8, 512], BF16)
            rtmp = Ring(ctx, nc, "ftmp", 3, [128, 512], F32)
            ysb = T("ysb", [128, 4, D], F32)
            r_y = [Res() for _ in range(4)]
            rx = Ring(ctx, nc, "fx", 2, [128, D], F32)
            rst = Ring(ctx, nc, "fst", 8, [128, 8], F32)
            junk = T("fjunk", [128, 512], BF16)
            rj = Res()
            w1v = self.wb_f1[l].rearrange("(k p) c -> p k c", p=128)
            w2v = self.wb_f2[l].rearrange("(f p) c -> p f c", p=128)
            pb1 = 0
            import os
            for t4 in range(int(os.environ.get("FFN_BLOCKS", NT // 4))):
                ht, hres = rh.next()
                kb.dma(kb.sp, ht[:], self.hT.rearrange("k p t -> p k t")[:, :, t4 * 512:(t4 + 1) * 512], writes=[hres])
                for fb in range(32):
                    w1, w1r = rw1.next()
                    kb.dma(kb.sp, w1[:], w1v[:, :, fb * 256:(fb + 1) * 256], writes=[w1r])
                    for fi in range(2):
                        f = fb * 2 + fi
                        b = pb1 % 4
                        pb1 += 1
                        for kc in range(KC):
                            kb.op(kb.pe, lambda kc=kc, b=b, fi=fi: nc.tensor.matmul(self.ps[b][:, :], lhsT=w1[:, kc, fi * 128:(fi + 1) * 128], rhs=ht[:, kc, :],
                                                                               start=(kc == 0), stop=(kc == KC - 1)),
                                  reads=[w1r, hres], writes=[self.pr[b]])
                        tmp, tr = rtmp.next()
                        kb.op(kb.act, lambda b=b: nc.scalar.activation(out=tmp[:], in_=self.ps[b][:, :], func=ACT.Relu), reads=[self.pr[b]], writes=[tr])
                        kb.op(kb.dve, lambda f=f: nc.vector.tensor_tensor(out=uT[:, f, :], in0=tmp[:], in1=tmp[:], op=ALU.mult), reads=[tr], writes=[r_u[f]])
                if os.environ.get("FFN_PHASE") == "1":
                    continue
                sts = [rst.next() for _ in range(4)]
                for n in range(4):
                    for fg in range(8):
                        w2, w2r = rw2.next()
                        kb.dma(kb.sp, w2[:], w2v[:, fg * 8:(fg + 1) * 8, n * 512:(n + 1) * 512], writes=[w2r])
                        for j in range(8):
                            f = fg * 8 + j
                            for tt in range(4):
                                b = 4 + tt
                                kb.op(kb.pe, lambda f=f, j=j, tt=tt, b=b: nc.tensor.matmul(self.ps[b][:, :], lhsT=uT[:, f, tt * 128:(tt + 1) * 128], rhs=w2[:, j, :],
                                                                                       start=(f == 0), stop=(f == 63)),
                                      reads=[r_u[f], w2r], writes=[self.pr[b]])
                    for tt in range(4):
                        b = 4 + tt
                        st, sr = sts[tt]
                        if os.environ.get("FFN_NOEV") == "1":
                            continue
                        if os.environ.get("FFN_NOEV") != "2":
                            kb.op(kb.act, lambda b=b, st=st: nc.scalar.activation(out=junk[:], in_=self.ps[b][:, :], func=ACT.Square, accum_out=st[:, n:n + 1]), reads=[self.pr[b]], writes=[rj, sr])
                        kb.op(kb.dve, lambda b=b, tt=tt: nc.vector.tensor_tensor(out=ysb[:, tt, n * 512:(n + 1) * 512], in0=self.ps[b][:, :], in1=self.G[:, n * 512:(n + 1) * 512], op=ALU.mult),
                              reads=[self.pr[b], self.r_G], writes=[r_y[tt]])
                if os.environ.get("FFN_PHASE") == "2":
                    continue
                for tt in range(4):
                    t = t4 * 4 + tt
                    st, sr = sts[tt]
                    xt, xr = rx.next()
                    kb.dma(kb.sp, xt[:], xsrc[t * 128:(t + 1) * 128, :], writes=[xr])
                    self.rstd_from_ss(st, sr, 4)
                    kb.op(kb.dve, lambda tt=tt, st=st: nc.vector.scalar_tensor_tensor(out=xt[:], in0=ysb[:, tt, :], scalar=st[:, 6:7], in1=xt[:], op0=ALU.mult, op1=ALU.add),
                          reads=[r_y[tt], sr, xr], writes=[xr])
                    kb.dma(kb.pool, self.out[t * 128:(t + 1) * 128, :], xt[:], reads=[xr])

    def stage_inproj0(self):
        kb, nc = self.kb, self.nc
        hTv = self.hT.rearrange("k p t -> p k t")
        wv = self.wb_in0.rearrange("(k p) c -> p k c", p=128)
        qk_blocks = [(0, 0, 0), (512, 4, 0), (1024, 0, 1), (1536, 4, 1), (3080, 8, 0), (3592, 12, 0), (4104, 8, 1), (4616, 12, 1)]
        v_blocks = [(2048, 0), (2560, 4), (5128, 8), (5640, 12)]
        with contextlib.ExitStack() as ctx:
            T = lambda name, shape, dty: ctx.enter_context(_sbt(nc, name, shape, dty))
            rh = Ring(ctx, nc, "ih", 2, [128, KC, 512], BF16)
            rw = Ring(ctx, nc, "iw", 3, [128, KC, 512], BF16)
            rs = Ring(ctx, nc, "is", 4, [128, 512], BF16)
            wg = T("iwg", [128, KC, 128], BF16)
            nlf = T("nlf", [8, S], F32)
            brow, nb = T("ibrow", [1, 8], F32), T("inb", [8, 1], F32)
            etmp = T("ietmp", [8, 512], F32)
            r_wg, r_nlf, r_b, r_e = Res(), Res(), Res(), Res()
            kb.dma(kb.sp, wg[:], wv[:, :, 3008:3136], writes=[r_wg])
            kb.dma(kb.sp, brow[:], self.even_b[:, :], writes=[r_b])
            kb.op(kb.pe, lambda: nc.tensor.matmul(self.ps[7][0:8, 0:1], lhsT=brow[0:1, 0:8], rhs=self.cst_sb[0:1, C_ONE:C_ONE + 1], start=True, stop=True),
                  reads=[r_b, self.r_cst], writes=[self.pr[7]])
            kb.op(kb.dve, lambda: nc.vector.tensor_scalar(out=nb[:], in0=self.ps[7][0:8, 0:1], scalar1=-1.0, scalar2=None, op0=ALU.mult), reads=[self.pr[7]], writes=[r_b])
            pb = 0
            ev = 0
            for t4 in range(NT // 4):
                ht, hres = rh.next()
                kb.dma(kb.sp, ht[:], hTv[:, :, t4 * 512:(t4 + 1) * 512], writes=[hres])
                b = pb % 7
                pb += 1
                for kc in range(KC):
                    kb.op(kb.pe, lambda: nc.tensor.matmul(self.ps[b][0:8, :], lhsT=wg[:, kc, 64:72], rhs=ht[:, kc, :], start=(kc == 0), stop=(kc == KC - 1)),
                          reads=[r_wg, hres], writes=[self.pr[b]])
                kb.op(kb.act, lambda: nc.scalar.activation(out=etmp[:], in_=self.ps[b][0:8, :], func=ACT.Exp, bias=nb[:], scale=-1.0), reads=[self.pr[b], r_b], writes=[r_e])
                kb.op(kb.act, lambda: nc.scalar.activation(out=nlf[:, t4 * 512:(t4 + 1) * 512], in_=etmp[:], func=ACT.Ln, bias=self.one_t[0:8, :], scale=1.0), reads=[r_e], writes=[r_nlf])
                for (c0, h0, isk) in qk_blocks:
                    w, wr = rw.next()
                    kb.dma(kb.sp, w[:], wv[:, :, c0:c0 + 512], writes=[wr])
                    for hi in range(4):
                        b = pb % 7
                        pb += 1
                        for kc in range(KC):
                            kb.op(kb.pe, lambda: nc.tensor.matmul(self.ps[b][:, :], lhsT=w[:, kc, hi * 128:(hi + 1) * 128], rhs=ht[:, kc, :], start=(kc == 0), stop=(kc == KC - 1)),
                                  reads=[wr, hres], writes=[self.pr[b]])
                        stg, sr = rs.next()
                        if ev % 2 == 0:
                            kb.op(kb.act, lambda: nc.scalar.copy(stg[:], self.ps[b][:, :]), reads=[self.pr[b]], writes=[sr])
                        else:
                            kb.op(kb.dve, lambda: nc.vector.tensor_copy(stg[:], self.ps[b][:, :]), reads=[self.pr[b]], writes=[sr])
                        ev += 1
                        kb.dma(kb.pool, self.qkT[2 * (h0 + hi) + isk, :, t4 * 512:(t4 + 1) * 512], stg[:], reads=[sr])
                for (c0, h0) in v_blocks:
                    w, wr = rw.next()
                    kb.dma(kb.sp, w[:], wv[:, :, c0:c0 + 512], writes=[wr])
                    for tt in range(4):
                        j = t4 * 4 + tt
                        b = pb % 7
                        pb += 1
                        for kc in range(KC):
                            kb.op(kb.pe, lambda: nc.tensor.matmul(self.ps[b][:, :], lhsT=ht[:, kc, tt * 128:(tt + 1) * 128], rhs=w[:, kc, :], start=(kc == 0), stop=(kc == KC - 1)),
                                  reads=[wr, hres], writes=[self.pr[b]])
                        stg, sr = rs.next()
                        if ev % 2 == 0:
                            kb.op(kb.act, lambda: nc.scalar.copy(stg[:], self.ps[b][:, :]), reads=[self.pr[b]], writes=[sr])
                        else:
                            kb.op(kb.dve, lambda: nc.vector.tensor_copy(stg[:], self.ps[b][:, :]), reads=[self.pr[b]], writes=[sr])
                        ev += 1
                        kb.dma(kb.pool, self.vt[h0:h0 + 4, :, j, :].rearrange("h p d -> p h d"), stg[:].rearrange("p (h d) -> p h d", h=4), reads=[sr])
            Pt = T("iP", [8, S], F32)
            R = T("iR", [8, 8, 32], F32)
            r_P, r_R = Res(), Res()
            kb.op(kb.dve, lambda: nc.vector.tensor_tensor_scan(out=Pt[:], data0=self.one_t[0:8, 0:1].to_broadcast([8, S]), data1=nlf[:], initial=0.0, op0=ALU.mult, op1=ALU.add),
                  reads=[r_nlf], writes=[r_P])
            for j in range(NT):
                kb.op(kb.pe, lambda: nc.tensor.transpose(self.ps[0][:, j * 8:(j + 1) * 8], Pt[0:8, j * 128:(j + 1) * 128], self.ident_f(8)), reads=[r_P, self.r_cst], writes=[self.pr[0]])
            kb.op(kb.dve, lambda: nc.vector.tensor_copy(self.Pcol[:], self.ps[0][:, 0:256]), reads=[self.pr[0]], writes=[self.r_P])
            for h in range(8):
                kb.op(kb.dve, lambda: nc.vector.tensor_scalar(out=R[:, h, :], in0=Pt[0:8, 0:S:128], scalar1=self.cst_sb[0:8, C_ID + h:C_ID + h + 1], scalar2=None, op0=ALU.mult),
                      reads=[r_P, self.r_cst], writes=[r_R])
            kb.op(kb.pe, lambda: nc.tensor.matmul(self.ps[1][:, 0:256], lhsT=self.cst_sb[0:8, C_ONE:C_ONE + 128], rhs=R[:].rearrange("k h i -> k (h i)"), start=True, stop=True),
                  reads=[r_R, self.r_cst], writes=[self.pr[1]])
            kb.op(kb.dve, lambda: nc.vector.tensor_copy(self.Pb[:], self.ps[1][:, 0:256]), reads=[self.pr[1]], writes=[self.r_P])
            kb.dma(kb.pool, self.dbg[:, 0:256], self.Pcol[:], reads=[self.r_P])
            kb.dma(kb.pool, self.dbg[:, 256:512], self.Pb[:], reads=[self.r_P])

    def stage_attn0(self):
        kb, nc = self.kb, self.nc
        scale = HD ** -0.5
        import os
        heads = [int(v) for v in os.environ["ATTN_HEADS"].split(",")] if "ATTN_HEADS" in os.environ else range(16)
        with contextlib.ExitStack() as ctx:
            T = lambda name, shape, dty: ctx.enter_context(_sbt(nc, name, shape, dty))
            rq = Ring(ctx, nc, "aq", 2, [128, S], BF16)
            rk = Ring(ctx, nc, "ak", 2, [128, S], BF16)
            rnk = Ring(ctx, nc, "ank", 1, [128, S], BF16)
            rv = Ring(ctx, nc, "av", 2, [128, NT, 129], BF16)
            roT = Ring(ctx, nc, "aoT", 2, [128, S], BF16)
            rp = Ring(ctx, nc, "ap", 5, [128, 128], BF16)
            rsp = Ring(ctx, nc, "asp", 4, [128, 128], BF16)
            re_ = Ring(ctx, nc, "ae", 3, [128, 128], F32)
            rT = Ring(ctx, nc, "aT", 2, [128, 128], F32)
            rR = Ring(ctx, nc, "aR", 4, [128, 128], F32)
            rbias = Ring(ctx, nc, "ab", 5, [128, 32], F32)
            rosb = Ring(ctx, nc, "aosb", 3, [128, 128], BF16)
            rrd = Ring(ctx, nc, "ard", 3, [128, 1], F32)
            for vt_, vr_ in zip(rv.t, rv.r):
                kb.op(kb.pool, lambda: nc.gpsimd.memset(vt_[:, :, 128:129], 1.0), writes=[vr_])
            cg = self.cast_gen(ctx, self.late_cast_jobs()) if (CAST_OVERLAP and self.on("cast")) else iter(())
            tri, trs = self.cb[:, C_TRI:C_TRI + 128], self.cb[:, C_TRS:C_TRS + 128]
            low, ones_b = self.cb[:, C_LOW:C_LOW + 128], self.cb[:, C_ONE:C_ONE + 128]
            Pcol = self.Pcol[:].rearrange("p (j h) -> p j h", h=8)
            Pb = self.Pb[:].rearrange("p (h i) -> p h i", i=32)
            sb_ = [0]
            xb_ = [0]
            for h in heads:
                fox = h < 8
                qT, qr = rq.next()
                kT, kr = rk.next()
                V, vr = rv.next()
                oTs, oTr = roT.next()
                kb.dma(kb.sp, qT[:], self.qkT[2 * h, :, :], writes=[qr])
                kb.dma(kb.sp, kT[:], self.qkT[2 * h + 1, :, :], writes=[kr])
                kb.dma(kb.sp, V[:, :, 0:128], self.vt[h, :, :, :], writes=[vr])
                if not fox:
                    nkT, nkr = rnk.next()
                    kb.op(kb.dve, lambda: nc.vector.tensor_scalar(out=nkT[:], in0=kT[:], scalar1=-scale, scalar2=None, op0=ALU.mult), reads=[kr], writes=[nkr])
                def finalize(i, normalize):
                    bO = 3 + (i % 2)
                    osb, osr = rosb.next()
                    if normalize:
                        rd, rdr = rrd.next()
                        kb.op(kb.dve, lambda: nc.vector.reciprocal(rd[:], self.ps[bO][:, 128:129]), reads=[self.pr[bO]], writes=[rdr])
                        kb.op(kb.act, lambda: nc.scalar.activation(out=osb[:], in_=self.ps[bO][:, 0:128], func=ACT.Copy, scale=rd[:, 0:1]), reads=[self.pr[bO], rdr], writes=[osr])
                    else:
                        kb.op(kb.act, lambda: nc.scalar.copy(osb[:], self.ps[bO][:, 0:128]), reads=[self.pr[bO]], writes=[osr])
                    pv = self.ps[5][:].bitcast(BF16)
                    kb.op(kb.pe, lambda: nc.tensor.transpose(pv[:, 0:128], osb[:], self.ident_b()), reads=[osr, self.r_cst], writes=[self.pr[5]])
                    kb.op(kb.dve, lambda: nc.vector.tensor_copy(oTs[:, i * 128:(i + 1) * 128], pv[:, 0:128]), reads=[self.pr[5]], writes=[oTr])
                    next(cg, None)

                if fox:
                    biases = {}

                    def f1(i, j):
                        if j == 0:
                            bias, br = rbias.next()
                            kb.op(kb.dve, lambda: nc.vector.tensor_scalar(out=bias[:], in0=Pcol[:, :, h], scalar1=Pb[:, h, i:i + 1], scalar2=None, op0=ALU.subtract),
                                  reads=[self.r_P], writes=[br])
                            biases[i] = (bias, br)
                        bS = sb_[0] % 3
                        sb_[0] += 1
                        kb.op(kb.pe, lambda: nc.tensor.matmul(self.ps[bS][:, 0:128], lhsT=kT[:, j * 128:(j + 1) * 128], rhs=qT[:, i * 128:(i + 1) * 128], start=True, stop=True),
                              reads=[kr, qr], writes=[self.pr[bS]])
                        return bS

                    def f2(i, j, bS):
                        bO = 3 + (i % 2)
                        bias, br = biases[i]
                        PT, pr_ = rp.next()
                        kb.op(kb.act, lambda: nc.scalar.activation(out=PT[:], in_=self.ps[bS][:, 0:128], func=ACT.Exp, bias=bias[:, j:j + 1], scale=scale),
                              reads=[self.pr[bS], br], writes=[pr_])
                        if j == i:
                            kb.op(kb.pool, lambda: nc.gpsimd.tensor_tensor(out=PT[:], in0=PT[:], in1=tri, op=ALU.mult), reads=[pr_, self.r_cst], writes=[pr_])
                        kb.op(kb.pe, lambda: nc.tensor.matmul(self.ps[bO][:, 0:129], lhsT=PT[:], rhs=V[:, j, :], start=(j == 0), stop=(j == i)),
                              reads=[pr_, vr], writes=[self.pr[bO]])
                        if j == i:
                            finalize(i, True)

                    pend = []
                    for i in range(NT):
                        for j in range(i + 1):
                            pend.append((i, j, f1(i, j)))
                            if len(pend) > 2:
                                f2(*pend.pop(0))
                    while pend:
                        f2(*pend.pop(0))
                else:
                    raccs = {}
                    sps = {}

                    def g1(i, idx, j, last):
                        bS = sb_[0] % 2
                        sb_[0] += 1
                        kb.op(kb.pe, lambda: nc.tensor.matmul(self.ps[bS][:, 0:128], lhsT=kT[:, j * 128:(j + 1) * 128], rhs=qT[:, i * 128:(i + 1) * 128], start=True, stop=True),
                              reads=[kr, qr], writes=[self.pr[bS]])
                        e, er = re_.next()
                        SP, spr = rsp.next()
                        kb.op(kb.act, lambda: nc.scalar.activation(out=e[:], in_=self.ps[bS][:, 0:128], func=ACT.Exp, scale=scale), reads=[self.pr[bS]], writes=[er])
                        kb.op(kb.act, lambda: nc.scalar.activation(out=SP[:], in_=e[:], func=ACT.Ln, bias=self.one_t[:], scale=1.0), reads=[er], writes=[spr])
                        if j == i:
                            kb.op(kb.pool, lambda: nc.gpsimd.tensor_tensor(out=SP[:], in0=SP[:], in1=trs, op=ALU.mult), reads=[spr, self.r_cst], writes=[spr])
                        sps[(i, idx)] = (SP, spr)

                    def g2(i, idx, j, last):
                        SP, spr = sps.pop((i, idx))
                        bX = (2, 6)[xb_[0] % 2]
                        xb_[0] += 1
                        kb.op(kb.pe, lambda: nc.tensor.matmul(self.ps[bX][:, 0:128], lhsT=low, rhs=SP[:], start=True, stop=False), reads=[spr, self.r_cst], writes=[self.pr[bX]])
                        kb.op(kb.pe, lambda: nc.tensor.matmul(self.ps[bX][:, 0:128], lhsT=nkT[:, j * 128:(j + 1) * 128], rhs=qT[:, i * 128:(i + 1) * 128], start=False, stop=True),
                              reads=[nkr, qr], writes=[self.pr[bX]])
                        A, ar = rp.next()
                        if idx == 0:
                            raccs[i] = rR.next()
                            kb.op(kb.act, lambda: nc.scalar.activation(out=A[:], in_=self.ps[bX][:, 0:128], func=ACT.Exp, scale=-1.0), reads=[self.pr[bX]], writes=[ar])
                        else:
                            Racc, rr = raccs[i]
                            Tt, tr_ = rT.next()
                            kb.op(kb.dve, lambda: nc.vector.tensor_tensor(out=Tt[:], in0=self.ps[bX][:, 0:128], in1=Racc[:], op=ALU.add), reads=[self.pr[bX], rr], writes=[tr_])
                            kb.op(kb.act, lambda: nc.scalar.activation(out=A[:], in_=Tt[:], func=ACT.Exp, scale=-1.0), reads=[tr_], writes=[ar])
                        if j == i:
                            kb.op(kb.pool, lambda: nc.gpsimd.tensor_tensor(out=A[:], in0=A[:], in1=trs, op=ALU.mult), reads=[ar, self.r_cst], writes=[ar])
                        if not last:
                            Racc, rr = raccs[i]
                            kb.op(kb.pe, lambda: nc.tensor.matmul(self.ps[7][:, 0:128], lhsT=ones_b, rhs=SP[:], start=True, stop=True), reads=[spr, self.r_cst], writes=[self.pr[7]])
                            if idx == 0:
                                kb.op(kb.dve, lambda: nc.vector.tensor_copy(Racc[:], self.ps[7][:, 0:128]), reads=[self.pr[7]], writes=[rr])
                            else:
                                kb.op(kb.dve, lambda: nc.vector.tensor_tensor(out=Racc[:], in0=self.ps[7][:, 0:128], in1=Racc[:], op=ALU.add), reads=[self.pr[7], rr], writes=[rr])
                        sps[("A", i, idx)] = (A, ar)

                    def g3(i, idx, j, last):
                        A, ar = sps.pop(("A", i, idx))
                        bO = 3 + (i % 2)
                        kb.op(kb.pe, lambda: nc.tensor.matmul(self.ps[bO][:, 0:128], lhsT=A[:], rhs=V[:, j, 0:128], start=(idx == 0), stop=last),
                              reads=[ar, vr], writes=[self.pr[bO]])
                        if last:
                            finalize(i, False)

                    stream = []
                    for i in range(NT):
                        js = [j for j in range(i, i - SB_WIN, -1) if j >= 0]
                        for idx, j in enumerate(js):
                            stream.append((i, idx, j, idx == len(js) - 1))
                    n = len(stream)
                    for k in range(n + 2):
                        if k < n:
                            g1(*stream[k])
                        if 0 <= k - 1 < n:
                            g2(*stream[k - 1])
                        if 0 <= k - 2 < n:
                            g3(*stream[k - 2])
                kb.dma(kb.pool, self.oT[h, :, :], oTs[:], reads=[oTr])
            for _ in cg:
                pass

    def stage_inproj1(self):
        kb, nc = self.kb, self.nc
        hTv = self.hT.rearrange("k p t -> p k t")
        wv = self.wb_in1.rearrange("(k p) c -> p k c", p=128)
        with contextlib.ExitStack() as ctx:
            T = lambda name, shape, dty: ctx.enter_context(_sbt(nc, name, shape, dty))
            sinq, cosq = T("sinq", [128, NT, 64], F32), T("cosq", [128, NT, 64], F32)
            r_tab = Res()
            with contextlib.ExitStack() as c2:
                T2 = lambda name, shape, dty: c2.enter_context(_sbt(nc, name, shape, dty))
                pi32, pf32, post = T2("pi32", [32, 128], I32), T2("pf32", [32, 128], F32), T2("post", [128, 32], F32)
                ang, u, ki, kf, m = (T2("ang", [128, NT * 64], F32), T2("ru", [128, NT * 64], F32), T2("rki", [128, NT * 64], I32),
                                     T2("rkf", [128, NT * 64], F32), T2("rm", [128, NT * 64], F32))
                npi = T2("npi", [128, 1], F32)
                r = Res()
                kb.dma(kb.sp, pi32[:], self.pos[:, :], writes=[r])
                kb.op(kb.dve, lambda: nc.vector.memset(npi[:], -3.14159), writes=[r])
                kb.op(kb.dve, lambda: nc.vector.tensor_copy(pf32[:], pi32[:]), reads=[r], writes=[r])
                kb.op(kb.pe, lambda: nc.tensor.transpose(self.ps[0][:, 0:32], pf32[:], self.ident_f(32)), reads=[r, self.r_cst], writes=[self.pr[0]])
                kb.op(kb.dve, lambda: nc.vector.tensor_copy(post[:], self.ps[0][:, 0:32]), reads=[self.pr[0]], writes=[r])
                for j in range(NT):
                    kb.op(kb.dve, lambda: nc.vector.tensor_scalar(out=ang[:, j * 64:(j + 1) * 64], in0=self.cst_sb[:, C_INV:C_INV + 64], scalar1=post[:, j:j + 1], scalar2=None, op0=ALU.mult),
                          reads=[r, self.r_cst], writes=[r])
                for tab, shift in ((sinq, 0.5), (cosq, 0.75)):
                    V = nc.vector
                    kb.op(kb.dve, lambda: V.tensor_scalar(out=u[:], in0=ang[:], scalar1=1.0 / (2 * np.pi), scalar2=shift, op0=ALU.mult, op1=ALU.add), reads=[r], writes=[r])
                    kb.op(kb.dve, lambda: V.tensor_copy(ki[:], u[:]), reads=[r], writes=[r])
                    kb.op(kb.dve, lambda: V.tensor_copy(kf[:], ki[:]), reads=[r], writes=[r])
                    kb.op(kb.dve, lambda: V.tensor_tensor(out=u[:], in0=u[:], in1=kf[:], op=ALU.subtract), reads=[r], writes=[r])
                    kb.op(kb.dve, lambda: V.tensor_scalar(out=m[:], in0=u[:], scalar1=0.0, scalar2=None, op0=ALU.is_lt), reads=[r], writes=[r])
                    kb.op(kb.dve, lambda: V.tensor_tensor(out=u[:], in0=u[:], in1=m[:], op=ALU.add), reads=[r], writes=[r])
                    kb.op(kb.dve, lambda: V.tensor_scalar(out=m[:], in0=u[:], scalar1=1.0, scalar2=None, op0=ALU.is_ge), reads=[r], writes=[r])
                    kb.op(kb.dve, lambda: V.tensor_tensor(out=u[:], in0=u[:], in1=m[:], op=ALU.subtract), reads=[r], writes=[r])
                    kb.op(kb.act, lambda: nc.scalar.activation(out=tab[:].rearrange("p j k -> p (j k)"), in_=u[:], func=ACT.Sin, bias=npi[:], scale=6.28318), reads=[r], writes=[r_tab])
                kb.barrier()
            rh = Ring(ctx, nc, "jh", 2, [128, KC, 512], BF16)
            rw = Ring(ctx, nc, "jw", 3, [128, KC, 512], BF16)
            qtile = [T(f"jq{tt}", [128, 16, 128], BF16) for tt in range(4)]
            qres = [Res() for _ in range(4)]
            rta = Ring(ctx, nc, "jta", 2, [128, 512], F32)
            rtb = Ring(ctx, nc, "jtb", 2, [128, 512], F32)
            rqT = Ring(ctx, nc, "jqT", 2, [128, 16, 512], BF16)
            rqiT = Ring(ctx, nc, "jqiT", 2, [128, 8, 512], BF16)
            rkT = Ring(ctx, nc, "jkT", 2, [128, 512], BF16)
            rkiT = Ring(ctx, nc, "jkiT", 2, [128, 512], BF16)
            rkr = Ring(ctx, nc, "jkr", 2, [128, 128], BF16)
            rvs = Ring(ctx, nc, "jvs", 2, [128, 128], BF16)
            rws = Ring(ctx, nc, "jws", 2, [128, 16], F32)
            pbank = [0]
            tbank = [0]
            evc = [0]

            def proj(ht, hres, w, wr, tt, ncols):
                b = pbank[0] % 6
                pbank[0] += 1
                for kc in range(KC):
                    kb.op(kb.pe, lambda: nc.tensor.matmul(self.ps[b][:, 0:ncols], lhsT=ht[:, kc, tt * 128:(tt + 1) * 128], rhs=w[:, kc, 0:ncols], start=(kc == 0), stop=(kc == KC - 1)),
                          reads=[wr, hres], writes=[self.pr[b]])
                return b

            def rope(b, c0, nh, half, j, dst, dres):
                x = self.ps[b][:, c0:c0 + nh * 2 * half].rearrange("p (h two d) -> p h two d", h=nh, two=2)
                x1, x2 = x[:, :, 0, :], x[:, :, 1, :]
                st = 64 // half
                cs = cosq[:, j, 0:64:st].unsqueeze(1).to_broadcast([128, nh, half])
                sn = sinq[:, j, 0:64:st].unsqueeze(1).to_broadcast([128, nh, half])
                ta, tar = rta.next()
                tb, tbr = rtb.next()
                av = ta[:, 0:nh * half].rearrange("p (h d) -> p h d", h=nh)
                bv = tb[:, 0:nh * half].rearrange("p (h d) -> p h d", h=nh)
                V = nc.vector
                kb.op(kb.dve, lambda: V.tensor_tensor(out=av, in0=x1, in1=cs, op=ALU.mult), reads=[self.pr[b], r_tab], writes=[tar])
                kb.op(kb.dve, lambda: V.tensor_tensor(out=bv, in0=x2, in1=sn, op=ALU.mult), reads=[self.pr[b], r_tab], writes=[tbr])
                kb.op(kb.pool, lambda: nc.gpsimd.tensor_tensor(out=dst[:, :, 0:half], in0=av, in1=bv, op=ALU.subtract), reads=[tar, tbr], writes=[dres])
                ta, tar = rta.next()
                tb, tbr = rtb.next()
                av = ta[:, 0:nh * half].rearrange("p (h d) -> p h d", h=nh)
                bv = tb[:, 0:nh * half].rearrange("p (h d) -> p h d", h=nh)
                kb.op(kb.dve, lambda: V.tensor_tensor(out=av, in0=x2, in1=cs, op=ALU.mult), reads=[self.pr[b], r_tab], writes=[tar])
                kb.op(kb.dve, lambda: V.tensor_tensor(out=bv, in0=x1, in1=sn, op=ALU.mult), reads=[self.pr[b], r_tab], writes=[tbr])
                kb.op(kb.pool, lambda: nc.gpsimd.tensor_tensor(out=dst[:, :, half:2 * half], in0=av, in1=bv, op=ALU.add), reads=[tar, tbr], writes=[dres])

            def transp(src_list, sres, dst_fn, dres):
                k = 0
                while k < len(src_list):
                    grp = src_list[k:k + 8]
                    b = 6 + tbank[0] % 2
                    tbank[0] += 1
                    pv = self.ps[b][:].bitcast(BF16)
                    for g, src in enumerate(grp):
                        kb.op(kb.pe, lambda: nc.tensor.transpose(pv[:, g * 128:(g + 1) * 128], src, self.ident_b()), reads=[sres, self.r_cst], writes=[self.pr[b]])
                    for g, src in enumerate(grp):
                        if evc[0] % 2 == 0:
                            kb.op(kb.act, lambda: nc.scalar.copy(dst_fn(k + g), pv[:, g * 128:(g + 1) * 128]), reads=[self.pr[b]], writes=[dres])
                        else:
                            kb.op(kb.dve, lambda: nc.vector.tensor_copy(dst_fn(k + g), pv[:, g * 128:(g + 1) * 128]), reads=[self.pr[b]], writes=[dres])
                        evc[0] += 1
                    k += 8

            for t4 in range(NT // 4):
                ht, hres = rh.next()
                kb.dma(kb.sp, ht[:], hTv[:, :, t4 * 512:(t4 + 1) * 512], writes=[hres])
                qTs, qTr = rqT.next()
                qiTs, qiTr = rqiT.next()
                kTs, kTr = rkT.next()
                kiTs, kiTr = rkiT.next()
                for cbk in range(4):
                    w, wr = rw.next()
                    kb.dma(kb.sp, w[:], wv[:, :, cbk * 512:(cbk + 1) * 512], writes=[wr])
                    for tt in range(4):
                        j = t4 * 4 + tt
                        b = proj(ht, hres, w, wr, tt, 512)
                        rope(b, 0, 4, 64, j, qtile[tt][:, cbk * 4:(cbk + 1) * 4, :], qres[tt])
                for tt in range(4):
                    transp([qtile[tt][:, h, :] for h in range(16)], qres[tt], lambda h: qTs[:, h, tt * 128:(tt + 1) * 128], qTr)
                w, wr = rw.next()
                kb.dma(kb.sp, w[:, :, 0:256], wv[:, :, 2048:2304], writes=[wr])
                for tt in range(4):
                    j = t4 * 4 + tt
                    b = proj(ht, hres, w, wr, tt, 256)
                    kr_, krr = rkr.next()
                    rope(b, 0, 1, 64, j, kr_[:].rearrange("p (h d) -> p h d", h=1), krr)
                    vs, vsr = rvs.next()
                    kb.op(kb.act, lambda: nc.scalar.copy(vs[:], self.ps[b][:, 128:256]), reads=[self.pr[b]], writes=[vsr])
                    kb.dma(kb.pool, self.v1d[:, j, :], vs[:], reads=[vsr])
                    transp([kr_[:]], krr, lambda h: kTs[:, tt * 128:(tt + 1) * 128], kTr)
                for cbk in range(2):
                    w, wr = rw.next()
                    kb.dma(kb.sp, w[:], wv[:, :, 2304 + cbk * 512:2304 + (cbk + 1) * 512], writes=[wr])
                    for tt in range(4):
                        j = t4 * 4 + tt
                        b = proj(ht, hres, w, wr, tt, 512)
                        qv = qtile[tt][:].rearrange("p a b -> p (a b)")[:, cbk * 512:(cbk + 1) * 512].rearrange("p (h d) -> p h d", h=8)
                        rope(b, 0, 8, 32, j, qv, qres[tt])
                for tt in range(4):
                    flat = qtile[tt][:].rearrange("p a b -> p (a b)")
                    transp([flat[:, pr * 128:(pr + 1) * 128] for pr in range(8)], qres[tt], lambda pr: qiTs[:, pr, tt * 128:(tt + 1) * 128], qiTr)
                w, wr = rw.next()
                kb.dma(kb.sp, w[:, :, 0:80], wv[:, :, 3328:3408], writes=[wr])
                for tt in range(4):
                    j = t4 * 4 + tt
                    b = proj(ht, hres, w, wr, tt, 80)
                    kr_, krr = rkr.next()
                    rope(b, 0, 1, 32, j, kr_[:, 0:64].rearrange("p (h d) -> p h d", h=1), krr)
                    kb.op(kb.pool, lambda: nc.gpsimd.tensor_copy(kr_[:, 64:128], kr_[:, 0:64]), reads=[krr], writes=[krr])
                    ws, wsr = rws.next()
                    kb.op(kb.act, lambda: nc.scalar.copy(ws[:], self.ps[b][:, 64:80]), reads=[self.pr[b]], writes=[wsr])
                    kb.dma(kb.pool, self.wd[:, j, :], ws[:], reads=[wsr])
                    transp([kr_[:]], krr, lambda h: kiTs[:, tt * 128:(tt + 1) * 128], kiTr)
                sl = slice(t4 * 512, (t4 + 1) * 512)
                kb.dma(kb.pool, self.qT1.rearrange("h p t -> p h t")[:, :, sl], qTs[:], reads=[qTr])
                kb.dma(kb.pool, self.qiT1.rearrange("h p t -> p h t")[:, :, sl], qiTs[:], reads=[qiTr])
                kb.dma(kb.pool, self.kT1d[:, sl], kTs[:], reads=[kTr])
                kb.dma(kb.pool, self.kiT2d[:, sl], kiTs[:], reads=[kiTr])

    def stage_dsa(self):
        kb, nc = self.kb, self.nc
        scale = HD ** -0.5
        import os
        ntiles = int(os.environ.get("DSA_TILES", NT))
        with contextlib.ExitStack() as ctx:
            T = lambda name, shape, dty: ctx.enter_context(_sbt(nc, name, shape, dty))
            kT1, kiT2 = T("kT1", [128, S], BF16), T("kiT2", [128, S], BF16)
            V1, wsb = T("V1", [128, NT, 129], BF16), T("wsb", [128, NT, 16], F32)
            id4 = T("id4", [128, 512], BF16)
            r_k = Res()
            kb.dma(kb.sp, kT1[:], self.kT1d[:, :], writes=[r_k])
            kb.dma(kb.sp, kiT2[:], self.kiT2d[:, :], writes=[r_k])
            kb.dma(kb.sp, V1[:, :, 0:128], self.v1d[:, :, :], writes=[r_k])
            kb.dma(kb.sp, wsb[:], self.wd[:, :, :], writes=[r_k])
            kb.op(kb.pool, lambda: nc.gpsimd.memset(V1[:, :, 128:129], 1.0), writes=[r_k])
            for g in range(4):
                kb.op(kb.pool, lambda: nc.gpsimd.tensor_copy(id4[:, g * 128:(g + 1) * 128], self.ident_b()), reads=[self.r_cst], writes=[r_k])
            rI = Ring(ctx, nc, "dI", 2, [128, S], F32)
            rWk = Ring(ctx, nc, "dWk", 2, [128, S], F32)
            rNM = Ring(ctx, nc, "dNM", 4, [128, S], BF16)
            rqi = Ring(ctx, nc, "dqi", 2, [128, 8, 128], BF16)
            rq = Ring(ctx, nc, "dq", 4, [128, 16, 128], BF16)
            rWd = Ring(ctx, nc, "dWd", 2, [128, 16, 128], BF16)
            rR = Ring(ctx, nc, "dR", 4, [128, 512], BF16)
            rPT = Ring(ctx, nc, "dPT", 4, [128, 512], BF16)
            roT = Ring(ctx, nc, "doT", 1, [128, 16, 512], BF16)
            rm8 = Ring(ctx, nc, "dm8", 4, [128, 8], F32)
            rthr = Ring(ctx, nc, "dthr", 4, [128, 1], F32)
            rrden = Ring(ctx, nc, "drden", 2, [128, 512], F32)
            ib_ = [0]
            sb_ = [0]
            tiles = {}
            oT_cur = [None, None]

            def phase_a_index(i):
                n_i = 128 * (i + 1)
                qiT, qir = rqi.next()
                qT, qr = rq.next()
                Wd, wdr = rWd.next()
                Isb, Ir = rI.next()
                kb.dma(kb.sp, qiT[:], self.qiT1.rearrange("h p t -> p h t")[:, :, i * 128:(i + 1) * 128], writes=[qir])
                kb.dma(kb.sp, qT[:], self.qT1.rearrange("h p t -> p h t")[:, :, i * 128:(i + 1) * 128], writes=[qr])
                for h in range(16):
                    kb.op(kb.pool, lambda: nc.gpsimd.tensor_scalar(out=Wd[:, h, :], in0=self.ident_b(), scalar1=wsb[:, i, h:h + 1], scalar2=0.25 * 0.125, op0=ALU.mult, op1=ALU.mult),
                          reads=[r_k, self.r_cst], writes=[wdr])

                def i1(sb, h):
                    nco = min(512, n_i - 512 * sb)
                    hp, pr = h % 2, h // 2
                    bI = ib_[0] % 3
                    ib_[0] += 1
                    kb.op(kb.pe, lambda: nc.tensor.matmul(self.ps[bI][:, 0:nco], lhsT=qiT[hp * 64:(hp + 1) * 64, pr, :], rhs=kiT2[hp * 64:(hp + 1) * 64, sb * 512:sb * 512 + nco], start=True, stop=True),
                          reads=[qir, r_k], writes=[self.pr[bI]])
                    return bI

                def i2(sb, h, bI):
                    nco = min(512, n_i - 512 * sb)
                    bA = 3 + (sb % 2)
                    R, rr = rR.next()
                    kb.op(kb.act, lambda: nc.scalar.activation(out=R[:, 0:nco], in_=self.ps[bI][:, 0:nco], func=ACT.Relu), reads=[self.pr[bI]], writes=[rr])
                    kb.op(kb.pe, lambda: nc.tensor.matmul(self.ps[bA][:, 0:nco], lhsT=Wd[:, h, :], rhs=R[:, 0:nco], start=(h == 0), stop=(h == 15)),
                          reads=[wdr, rr], writes=[self.pr[bA]])
                    if h == 15:
                        kb.op(kb.act, lambda: nc.scalar.copy(Isb[:, sb * 512:sb * 512 + nco], self.ps[bA][:, 0:nco]), reads=[self.pr[bA]], writes=[Ir])

                pend = []
                for sb in range((n_i + 511) // 512):
                    for h in range(16):
                        pend.append((sb, h, i1(sb, h)))
                        if len(pend) > 2:
                            i2(*pend.pop(0))
                while pend:
                    i2(*pend.pop(0))
                kb.op(kb.dve, lambda: nc.vector.memset(Isb[0:64, n_i - 64:n_i], NEG), writes=[Ir])
                tiles[i] = dict(qT=qT, qr=qr, Isb=Isb, Ir=Ir, n=n_i)

            def phase_a_topk(group):
                chains = []
                for i in group:
                    t = tiles[i]
                    thr, thr_r = rthr.next()
                    t["thr"], t["thr_r"] = thr, thr_r
                    if t["n"] <= 256:
                        kb.op(kb.dve, lambda: nc.vector.memset(thr[:], -1.0e29), writes=[thr_r])
                    else:
                        Wk, wkr = rWk.next()
                        chains.append(dict(t=t, cur=t["Isb"], cur_r=t["Ir"], Wk=Wk, wkr=wkr))
                for rnd in range(32):
                    if rnd > 0:
                        yield
                    for c in chains:
                        t, n_i = c["t"], c["t"]["n"]
                        m8, m8r = rm8.next()
                        c["m8"], c["m8r"] = m8, m8r
                        cur, cur_r = c["cur"], c["cur_r"]
                        kb.op(kb.dve, lambda: nc.vector.max(out=m8[:], in_=cur[:, 0:n_i]), reads=[cur_r], writes=[m8r])
                    for c in chains:
                        t, n_i = c["t"], c["t"]["n"]
                        m8, m8r, cur, cur_r, Wk, wkr = c["m8"], c["m8r"], c["cur"], c["cur_r"], c["Wk"], c["wkr"]
                        if rnd < 31:
                            kb.op(kb.dve, lambda: nc.vector.match_replace(out=Wk[:, 0:n_i], in_to_replace=m8[:], in_values=cur[:, 0:n_i], imm_value=NEG), reads=[cur_r, m8r], writes=[wkr])
                            c["cur"], c["cur_r"] = Wk, wkr
                        else:
                            kb.op(kb.dve, lambda: nc.vector.tensor_copy(t["thr"][:], m8[:, 7:8]), reads=[m8r], writes=[t["thr_r"]])
                for i in group:
                    t = tiles[i]
                    NM, nmr = rNM.next()
                    kb.op(kb.dve, lambda: nc.vector.tensor_scalar(out=NM[:, 0:t["n"]], in0=t["Isb"][:, 0:t["n"]], scalar1=t["thr"][:, 0:1], scalar2=-30000.0, op0=ALU.is_lt, op1=ALU.mult),
                          reads=[t["Ir"], t["thr_r"]], writes=[nmr])
                    t["NM"], t["nmr"] = NM, nmr

            def phase_b(i):
                t = tiles.pop(i)
                qT, qr, NM, nmr = t["qT"], t["qr"], t["NM"], t["nmr"]
                tt = i % 4
                if tt == 0:
                    oT_cur[0], oT_cur[1] = roT.next()
                oTs, oTr = oT_cur

                def b1(hg, j):
                    bS = (0, 1, 6)[sb_[0] % 3]
                    sb_[0] += 1
                    kb.op(kb.pe, lambda: nc.tensor.matmul(self.ps[bS][:, :], lhsT=kT1[:, j * 128:(j + 1) * 128], rhs=qT[:, hg * 4:(hg + 1) * 4, :].rearrange("p h t -> p (h t)"), start=True, stop=False),
                          reads=[r_k, qr], writes=[self.pr[bS]])
                    kb.op(kb.pe, lambda: nc.tensor.matmul(self.ps[bS][:, :], lhsT=NM[:, j * 128:(j + 1) * 128], rhs=id4[:], start=False, stop=True),
                          reads=[nmr, r_k], writes=[self.pr[bS]])
                    return bS

                def b2(hg, j, bS):
                    PT, ptr = rPT.next()
                    bOT, bDen = 2 + 2 * (hg % 2), 3 + 2 * (hg % 2)
                    kb.op(kb.act, lambda: nc.scalar.activation(out=PT[:], in_=self.ps[bS][:, :], func=ACT.Exp, scale=scale), reads=[self.pr[bS]], writes=[ptr])
                    kb.op(kb.pe, lambda: nc.tensor.matmul(self.ps[bOT][:, :], lhsT=V1[:, j, 0:128], rhs=PT[:], start=(j == 0), stop=(j == i)), reads=[ptr, r_k], writes=[self.pr[bOT]])
                    kb.op(kb.pe, lambda: nc.tensor.matmul(self.ps[bDen][:, :], lhsT=self.cb[:, C_ONE:C_ONE + 128], rhs=PT[:], start=(j == 0), stop=(j == i)), reads=[ptr, self.r_cst], writes=[self.pr[bDen]])

                def fin(hg):
                    bOT, bDen = 2 + 2 * (hg % 2), 3 + 2 * (hg % 2)
                    rden, rdr = rrden.next()
                    kb.op(kb.dve, lambda: nc.vector.reciprocal(rden[:], self.ps[bDen][:, :]), reads=[self.pr[bDen]], writes=[rdr])
                    kb.op(kb.dve, lambda: nc.vector.tensor_tensor(out=oTs[:, hg * 4:(hg + 1) * 4, tt * 128:(tt + 1) * 128], in0=self.ps[bOT][:, :].rearrange("p (h t) -> p h t", h=4),
                                                                  in1=rden[:].rearrange("p (h t) -> p h t", h=4), op=ALU.mult),
                          reads=[self.pr[bOT], rdr], writes=[oTr])

                pend = []
                for hg in range(4):
                    for j in range(i + 1):
                        pend.append((hg, j, b1(hg, j)))
                        if len(pend) > 2:
                            p = pend.pop(0)
                            b2(*p)
                            if p[1] == i:
                                yield
                                fin(p[0])
                while pend:
                    p = pend.pop(0)
                    b2(*p)
                    if p[1] == i:
                        yield
                        fin(p[0])
                if tt == 3 or i == ntiles - 1:
                    t4 = i // 4
                    kb.dma(kb.pool, self.oT.rearrange("k p t -> p k t")[:, :, t4 * 512:(t4 + 1) * 512], oTs[:], reads=[oTr])

            groups = [list(range(g, min(g + 2, ntiles))) for g in range(0, ntiles, 2)]
            for gi, grp in enumerate(groups):
                for i in grp:
                    phase_a_index(i)
                tg = phase_a_topk(grp)
                if gi > 0:
                    for i in groups[gi - 1]:
                        for _ in phase_b(i):
                            for _r in range(4):
                                next(tg, None)
                for _ in tg:
                    pass
            for i in groups[-1]:
                for _ in phase_b(i):
                    pass


def make_in_maps(inputs):
    f = lambda a: np.ascontiguousarray(a)
    cst = make_consts()
    maps = []
    for b in range(8):
        maps.append({
            "x": f(inputs["x"][b]), "c": f(inputs["c"][b].reshape(16, 128)),
            "pos": f(inputs["positions"][b].reshape(32, 128).astype(np.int32)),
            "ada_w": inputs["ada_w"], "ada_b": inputs["ada_b"], "norm_g": inputs["norm_g"],
            "mix_w_out": inputs["mix_w_out"], "even_w_in": f(inputs["even_w_in"][0]),
            "even_b": f(inputs["even_b_forget"].reshape(1, 8)), "odd_w_in": f(inputs["odd_w_in"][0]),
            "ff_w1": inputs["ff_w1"], "ff_w2": inputs["ff_w2"], "cst": cst,
        })
    return maps


def kernel(**inputs):
    inputs = {k: np.asarray(v) for k, v in inputs.items()}
    prog = Prog()
    res = run_bass_kernel_spmd(prog.nc, make_in_maps(inputs), core_ids=list(range(8)))
    return np.stack([r["out"] for r in res.results], axis=0).astype(np.float32)
```

```python
import contextlib
import numpy as np
import concourse.bass as bass
import concourse.mybir as mybir
from concourse.bass_utils import run_bass_kernel_spmd

ACT = mybir.ActivationFunctionType
ALU = mybir.AluOpType
AX = mybir.AxisListType
F32, BF16, I32 = mybir.dt.float32, mybir.dt.bfloat16, mybir.dt.int32

S, D, DFF, HD = 4096, 2048, 8192, 128
NT = S // 128
KC = D // 128
EVEN_W, ODD_W = 6152, 3408
EPS = 1e-6
NEG = -1.0e30
REM = -2.0e30
SB_WIN = 3
NSLOT = 8
CAST_OVERLAP = True

C_ID, C_TRI, C_TRS, C_LOW, C_ONE, C_INV = 0, 128, 256, 384, 512, 640
C_W = 704


def make_consts():
    c = np.zeros((128, C_W), np.float32)
    a = np.arange(128)
    c[:, C_ID:C_ID + 128] = np.eye(128)
    c[:, C_TRI:C_TRI + 128] = (a[:, None] <= a[None, :])
    c[:, C_TRS:C_TRS + 128] = (a[:, None] < a[None, :])
    c[:, C_LOW:C_LOW + 128] = (a[:, None] >= a[None, :])
    c[:, C_ONE:C_ONE + 128] = 1.0
    inv = (10000.0 ** (-np.arange(64, dtype=np.float32) / 64)).astype(np.float32)
    c[:, C_INV:C_INV + 64] = inv[None, :]
    return c


_uid = [0]


def _sbt(nc, name, shape, dty):
    _uid[0] += 1
    return nc.sbuf_tensor(f"{name}_u{_uid[0]}", shape, dty)


class Res:
    __slots__ = ("w", "r", "x")

    def __init__(self, x=False):
        self.w = None
        self.r = {}
        self.x = x


class Eng:
    def __init__(self, name, obj, sem, is_pe=False):
        self.name, self.obj, self.sem, self.is_pe = name, obj, sem, is_pe
        self.n = 0
        self.waited = {}
        self.dma_sems, self.dma_cnt, self.dma_i = [], [], 0


class KB:
    def __init__(self, nc):
        self.nc = nc
        self.stack = contextlib.ExitStack()
        mk = lambda nm: self.stack.enter_context(nc.semaphore(nm))
        self.pe = Eng("pe", nc.tensor, mk("s_pe"), True)
        self.act = Eng("act", nc.scalar, mk("s_act"))
        self.dve = Eng("dve", nc.vector, mk("s_dve"))
        self.pool = Eng("pool", nc.gpsimd, mk("s_pool"))
        self.sp = Eng("sp", nc.sync, mk("s_sp"))
        self.engs = [self.pe, self.act, self.dve, self.pool, self.sp]
        self.queues = [self.sp, self.pool, self.act]
        for q in self.queues:
            q.dma_sems = [mk(f"d_{q.name}{i}") for i in range(NSLOT)]
            q.dma_cnt = [0] * NSLOT
        self.n_ins = 0

    def _wait(self, eng, tk):
        sem, val, src = tk
        if src is eng and eng.is_pe:
            return
        key = sem.name
        if eng.waited.get(key, 0) >= val:
            return
        eng.obj.wait_ge(sem, val)
        eng.waited[key] = val

    def _deps(self, eng, reads, writes):
        for r in reads:
            if r.w is not None:
                self._wait(eng, r.w)
            if r.x:
                for k, tk in r.r.items():
                    if k != eng.name:
                        self._wait(eng, tk)
        for w in writes:
            if w.w is not None:
                self._wait(eng, w.w)
            for tk in w.r.values():
                self._wait(eng, tk)

    def _mark(self, key, tk, reads, writes):
        for r in reads:
            r.r[key] = tk
        for w in writes:
            w.w = tk
            w.r = {}

    def op(self, eng, fn, reads=(), writes=()):
        self._deps(eng, reads, writes)
        ins = fn()
        eng.n += 1
        ins.then_inc(eng.sem, 1)
        tk = (eng.sem, eng.n, eng)
        self._mark(eng.name, tk, reads, writes)
        self.n_ins += 1
        return tk

    def dma(self, q, out, in_, reads=(), writes=()):
        slot = q.dma_i % NSLOT
        q.dma_i += 1
        sem = q.dma_sems[slot]
        if q.dma_cnt[slot] > 0:
            self._wait(q, (sem, 16 * q.dma_cnt[slot], None))
        self._deps(q, reads, writes)
        q.obj.dma_start(out=out, in_=in_).then_inc(sem, 16)
        q.dma_cnt[slot] += 1
        tk = (sem, 16 * q.dma_cnt[slot], None)
        self._mark(sem.name, tk, reads, writes)
        self.n_ins += 1
        return tk

    def barrier(self):
        for e in self.engs:
            for f in self.engs:
                if f is not e and f.n > 0:
                    self._wait(e, (f.sem, f.n, f))
            for q in self.queues:
                for i in range(NSLOT):
                    if q.dma_cnt[i] > 0:
                        self._wait(e, (q.dma_sems[i], 16 * q.dma_cnt[i], None))


class Ring:
    def __init__(self, ctx, nc, name, n, shape, dtype):
        self.t = [ctx.enter_context(_sbt(nc, f"{name}{i}", shape, dtype)) for i in range(n)]
        self.r = [Res() for _ in range(n)]
        self.i = 0

    def next(self):
        k = self.i % len(self.t)
        self.i += 1
        return self.t[k], self.r[k]


class Prog:
    def __init__(self, stages=None, xsrc_is_out=False):
        self.stages = stages
        nc = self.nc = bass.Bass("TRN2", target_bir_lowering=False)
        kb = self.kb = KB(nc)
        import os
        dbg = set(os.environ.get("DEBUG_OUT", "").split(","))
        dt = lambda name, shape, dty, kind: nc.dram_tensor(name, shape, dty, kind=("ExternalOutput" if name in dbg else kind)).ap()
        I, O, N = "ExternalInput", "ExternalOutput", "Internal"
        self.x = dt("x", [S, D], F32, I)
        self.c = dt("c", [16, 128], F32, I)
        self.pos = dt("pos", [32, 128], I32, I)
        self.ada_w = dt("ada_w", [2, D, 6 * D], F32, I)
        self.ada_b = dt("ada_b", [2, 6 * D], F32, I)
        self.norm_g = dt("norm_g", [2, 4, D], F32, I)
        self.mix_w_out = dt("mix_w_out", [2, D, D], F32, I)
        self.even_w_in = dt("even_w_in", [D, EVEN_W], F32, I)
        self.even_b = dt("even_b", [1, 8], F32, I)
        self.odd_w_in = dt("odd_w_in", [D, ODD_W], F32, I)
        self.ff_w1 = dt("ff_w1", [2, D, DFF], F32, I)
        self.ff_w2 = dt("ff_w2", [2, DFF, D], F32, I)
        self.cst = dt("cst", [128, C_W], F32, I)
        self.out = dt("out", [S, D], F32, O)
        self.wb_in0 = dt("wb_in0", [D, EVEN_W], BF16, N)
        self.wb_in1 = dt("wb_in1", [D, ODD_W], BF16, N)
        self.wb_out = [dt(f"wb_out{l}", [D, D], BF16, N) for l in range(2)]
        self.wb_f1 = [dt(f"wb_f1{l}", [D, DFF], BF16, N) for l in range(2)]
        self.wb_f2 = [dt(f"wb_f2{l}", [DFF, D], BF16, N) for l in range(2)]
        self.modd = dt("modd", [2, 6 * D], F32, N)
        self.hT = dt("hT", [KC, 128, S], BF16, N)
        self.oT = dt("oT", [KC, 128, S], BF16, N)
        self.qkT = dt("qkT", [32, 128, S], BF16, N)
        self.vt = dt("vt", [16, 128, NT, 128], BF16, N)
        self.qT1 = dt("qT1", [16, 128, S], BF16, N)
        self.qiT1 = dt("qiT1", [8, 128, S], BF16, N)
        self.dbg = dt("dbg", [128, 512], F32, N)
        self.kT1d = dt("kT1d", [128, S], BF16, N)
        self.kiT2d = dt("kiT2d", [128, S], BF16, N)
        self.v1d = dt("v1d", [128, NT, 128], BF16, N)
        self.wd = dt("wd", [128, NT, 16], F32, N)
        self.ps = [nc.alloc_psum_tensor(f"ps{b}", [128, 512], F32) for b in range(8)]
        self.pr = [Res(True) for _ in range(8)]
        A = nc.alloc_sbuf_tensor
        self.cst_sb = A("cst_sb", [128, C_W], F32)
        self.cb = A("cb", [128, 640], BF16)
        self.eps_t = A("eps_t", [128, 1], F32)
        self.one_t = A("one_t", [128, 1], F32)
        self.zero_t = A("zero_t", [128, 1], F32)
        self.acol = A("acol", [128, 16], F32)
        self.bcol = A("bcol", [128, 16], F32)
        self.G = A("G", [128, D], F32)
        self.Pcol = A("Pcol", [128, 256], F32)
        self.Pb = A("Pb", [128, 256], F32)
        self.r_cst, self.r_cols, self.r_G, self.r_P = Res(), Res(), Res(), Res()
        self.build()

    def ident_f(self, n=128):
        return self.cst_sb[0:n, C_ID:C_ID + n]

    def ident_b(self):
        return self.cb[:, C_ID:C_ID + 128]

    def on(self, name):
        return self.stages is None or name in self.stages

    def build(self):
        kb, nc = self.kb, self.nc
        kb.dma(kb.sp, self.cst_sb[:], self.cst[:, :], writes=[self.r_cst])
        kb.op(kb.dve, lambda: nc.vector.tensor_copy(self.cb[:], self.cst_sb[:, 0:640]), reads=[self.r_cst], writes=[self.r_cst])
        kb.op(kb.dve, lambda: nc.vector.memset(self.eps_t[:], EPS), writes=[self.r_cst])
        kb.op(kb.dve, lambda: nc.vector.memset(self.one_t[:], 1.0), writes=[self.r_cst])
        kb.op(kb.dve, lambda: nc.vector.memset(self.zero_t[:], 0.0), writes=[self.r_cst])
        kb.barrier()
        if self.on("cast"):
            self.stage_cast()
            kb.barrier()
        if self.on("ada"):
            self.stage_ada()
            kb.barrier()
        xsrc = self.x
        for l in range(2):
            if self.on(f"mix{l}"):
                self.stage_normT(l, "a", xsrc)
                kb.barrier()
                if l == 0:
                    self.stage_inproj0()
                    kb.barrier()
                    self.stage_attn0()
                    kb.barrier()
                else:
                    self.stage_inproj1()
                    kb.barrier()
                    self.stage_dsa()
                    kb.barrier()
                self.stage_outproj(l, xsrc)
                kb.barrier()
                xsrc = self.out
            if self.on(f"ffn{l}"):
                self.stage_normT(l, "m", xsrc)
                kb.barrier()
                if self.stages is None or "skipffn" not in self.stages:
                    self.stage_ffn(l, xsrc)
                    kb.barrier()
                    xsrc = self.out
        kb.barrier()

    def stage_cast(self):
        kb, nc = self.kb, self.nc
        jobs = [(self.even_w_in, self.wb_in0)]
        if not (CAST_OVERLAP and self.on("mix0")):
            jobs += self.late_cast_jobs()
        CW = 2048
        with contextlib.ExitStack() as ctx:
            rf = Ring(ctx, nc, "cf", 4, [128, CW], F32)
            rb = Ring(ctx, nc, "cbf", 4, [128, CW], BF16)
            k = 0
            for src, dst in jobs:
                R, C = src.shape
                for rbk in range(R // 128):
                    for c0 in range(0, C, CW):
                        cw = min(CW, C - c0)
                        tf, rfr = rf.next()
                        tb, rbr = rb.next()
                        kb.dma(kb.sp, tf[:, 0:cw], src[rbk * 128:(rbk + 1) * 128, c0:c0 + cw], writes=[rfr])
                        if k % 2 == 0:
                            kb.op(kb.act, lambda: nc.scalar.copy(tb[:, 0:cw], tf[:, 0:cw]), reads=[rfr], writes=[rbr])
                        else:
                            kb.op(kb.dve, lambda: nc.vector.tensor_copy(tb[:, 0:cw], tf[:, 0:cw]), reads=[rfr], writes=[rbr])
                        kb.dma(kb.pool, dst[rbk * 128:(rbk + 1) * 128, c0:c0 + cw], tb[:, 0:cw], reads=[rbr])
                        k += 1

    def late_cast_jobs(self):
        return [(self.mix_w_out[0], self.wb_out[0]), (self.ff_w1[0], self.wb_f1[0]), (self.ff_w2[0], self.wb_f2[0]),
                (self.odd_w_in, self.wb_in1), (self.mix_w_out[1], self.wb_out[1]), (self.ff_w1[1], self.wb_f1[1]),
                (self.ff_w2[1], self.wb_f2[1])]

    def cast_gen(self, ctx, jobs):
        kb, nc = self.kb, self.nc
        CW = 2048
        rf = Ring(ctx, nc, "cgf", 3, [128, CW], F32)
        rb = Ring(ctx, nc, "cgb", 3, [128, CW], BF16)
        pieces = []
        for src, dst in jobs:
            R, C = src.shape
            for rbk in range(R // 128):
                for c0 in range(0, C, CW):
                    pieces.append((src, dst, rbk, c0, min(CW, C - c0)))
        loaded = []
        for k in range(len(pieces) + 2):
            if k < len(pieces):
                src, dst, rbk, c0, cw = pieces[k]
                tf, rfr = rf.next()
                kb.dma(kb.sp, tf[:, 0:cw], src[rbk * 128:(rbk + 1) * 128, c0:c0 + cw], writes=[rfr])
                loaded.append((pieces[k], tf, rfr))
            if k >= 2:
                (src, dst, rbk, c0, cw), tf, rfr = loaded.pop(0)
                tb, rbr = rb.next()
                kb.op(kb.dve, lambda: nc.vector.tensor_copy(tb[:, 0:cw], tf[:, 0:cw]), reads=[rfr], writes=[rbr])
                kb.dma(kb.sp, dst[rbk * 128:(rbk + 1) * 128, c0:c0 + cw], tb[:, 0:cw], reads=[rbr])
            yield

    def stage_ada(self):
        kb, nc = self.kb, self.nc
        with contextlib.ExitStack() as ctx:
            T = lambda name, shape, dty: ctx.enter_context(_sbt(nc, name, shape, dty))
            c16, sg16, csT = T("c16", [16, 128], F32), T("sg16", [16, 128], F32), T("csT", [128, 16], F32)
            brow, mrow = T("brow", [1, 6 * D], F32), T("mrow", [1, 6 * D], F32)
            r_c, r_b, r_m = Res(), Res(), Res()
            wr = Ring(ctx, nc, "adaw", 3, [128, 3072], F32)
            kb.dma(kb.sp, c16[:], self.c[:, :], writes=[r_c])
            kb.op(kb.act, lambda: nc.scalar.activation(out=sg16[:], in_=c16[:], func=ACT.Sigmoid), reads=[r_c], writes=[r_c])
            kb.op(kb.dve, lambda: nc.vector.tensor_mul(c16[:], c16[:], sg16[:]), reads=[r_c], writes=[r_c])
            kb.op(kb.pe, lambda: nc.tensor.transpose(self.ps[0][:, 0:16], c16[:], self.ident_f(16)), reads=[r_c, self.r_cst], writes=[self.pr[0]])
            kb.op(kb.dve, lambda: nc.vector.tensor_copy(csT[:], self.ps[0][:, 0:16]), reads=[self.pr[0]], writes=[r_c])
            for l in range(2):
                kb.dma(kb.sp, brow[:], self.ada_b[l:l + 1, :], writes=[r_b])
                for g in range(4):
                    for kc in range(KC):
                        wt, wres = wr.next()
                        kb.dma(kb.sp, wt[:], self.ada_w[l, kc * 128:(kc + 1) * 128, g * 3072:(g + 1) * 3072], writes=[wres])
                        for b in range(6):
                            kb.op(kb.pe, lambda b=b: nc.tensor.matmul(self.ps[b][0:1, :], lhsT=csT[:, kc:kc + 1], rhs=wt[:, b * 512:(b + 1) * 512],
                                                                   start=(kc == 0), stop=(kc == KC - 1)),
                                  reads=[r_c, wres], writes=[self.pr[b]])
                    for b in range(6):
                        c0 = g * 3072 + b * 512
                        kb.op(kb.dve, lambda b=b, c0=c0: nc.vector.tensor_tensor(out=mrow[0:1, c0:c0 + 512], in0=self.ps[b][0:1, :], in1=brow[0:1, c0:c0 + 512], op=ALU.add),
                              reads=[self.pr[b], r_b], writes=[r_m])
                kb.dma(kb.pool, self.modd[l:l + 1, :], mrow[:], reads=[r_m])

    def prep_cols(self, ctx, l, which):
        kb, nc = self.kb, self.nc
        off = 0 if which == "a" else 3
        gi = 0 if which == "a" else 2
        T = lambda name, shape, dty: ctx.enter_context(_sbt(nc, name, shape, dty))
        sc16, sh16, gm16 = T("sc16", [16, 128], F32), T("sh16", [16, 128], F32), T("gm16", [16, 128], F32)
        r = Res()
        v16 = lambda ap: ap.rearrange("(c p) -> c p", p=128)
        kb.dma(kb.sp, sh16[:], v16(self.modd[l, off * D:(off + 1) * D]), writes=[r])
        kb.dma(kb.sp, sc16[:], v16(self.modd[l, (off + 1) * D:(off + 2) * D]), writes=[r])
        kb.dma(kb.sp, gm16[:], v16(self.norm_g[l, gi, :]), writes=[r])
        kb.op(kb.dve, lambda: nc.vector.scalar_tensor_tensor(out=sc16[:], in0=sc16[:], scalar=1.0, in1=gm16[:], op0=ALU.add, op1=ALU.mult), reads=[r], writes=[r])
        kb.op(kb.pe, lambda: nc.tensor.transpose(self.ps[0][:, 0:16], sc16[:], self.ident_f(16)), reads=[r, self.r_cst], writes=[self.pr[0]])
        kb.op(kb.pe, lambda: nc.tensor.transpose(self.ps[1][:, 0:16], sh16[:], self.ident_f(16)), reads=[r, self.r_cst], writes=[self.pr[1]])
        kb.op(kb.dve, lambda: nc.vector.tensor_copy(self.acol[:], self.ps[0][:, 0:16]), reads=[self.pr[0]], writes=[self.r_cols])
        kb.op(kb.dve, lambda: nc.vector.tensor_copy(self.bcol[:], self.ps[1][:, 0:16]), reads=[self.pr[1]], writes=[self.r_cols])

    def prep_G(self, ctx, l, which):
        kb, nc = self.kb, self.nc
        off = 2 if which == "a" else 5
        gi = 1 if which == "a" else 3
        T = lambda name, shape, dty: ctx.enter_context(_sbt(nc, name, shape, dty))
        grow, gmrow = T("grow", [1, D], F32), T("gmrow", [1, D], F32)
        r = Res()
        kb.dma(kb.sp, grow[:], self.modd[l:l + 1, off * D:(off + 1) * D], writes=[r])
        kb.dma(kb.sp, gmrow[:], self.norm_g[l, gi:gi + 1, :], writes=[r])
        kb.op(kb.dve, lambda: nc.vector.tensor_mul(grow[:], grow[:], gmrow[:]), reads=[r], writes=[r])
        for n in range(4):
            kb.op(kb.pe, lambda n=n: nc.tensor.matmul(self.ps[n][:, :], lhsT=self.cst_sb[0:1, C_ONE:C_ONE + 128], rhs=grow[0:1, n * 512:(n + 1) * 512], start=True, stop=True),
                  reads=[r, self.r_cst], writes=[self.pr[n]])
            kb.op(kb.dve, lambda n=n: nc.vector.tensor_copy(self.G[:, n * 512:(n + 1) * 512], self.ps[n][:, :]), reads=[self.pr[n]], writes=[self.r_G])

    def stage_normT(self, l, which, xsrc):
        kb, nc = self.kb, self.nc
        with contextlib.ExitStack() as ctx:
            T = lambda name, shape, dty: ctx.enter_context(_sbt(nc, name, shape, dty))
            with contextlib.ExitStack() as c2:
                self.prep_cols(c2, l, which)
            kb.barrier()
            rx = Ring(ctx, nc, "nx", 3, [128, D], F32)
            rxn = Ring(ctx, nc, "nxn", 2, [128, D], BF16)
            rst = Ring(ctx, nc, "nst", 4, [128, 4], F32)
            rh = Ring(ctx, nc, "nh", 2, [128, KC, 512], BF16)
            junk = T("njunk", [128, D], BF16)
            rj = Res()
            pbank = 0
            for t4 in range(NT // 4):
                hts, hres = rh.next()
                for tt in range(4):
                    t = t4 * 4 + tt
                    xt, xr = rx.next()
                    xn, xnr = rxn.next()
                    st, sr = rst.next()
                    kb.dma(kb.sp, xt[:], xsrc[t * 128:(t + 1) * 128, :], writes=[xr])
                    kb.op(kb.act, lambda: nc.scalar.activation(out=junk[:], in_=xt[:], func=ACT.Square, accum_out=st[:, 0:1]), reads=[xr], writes=[rj, sr])
                    kb.op(kb.act, lambda: nc.scalar.activation(out=st[:, 1:2], in_=st[:, 0:1], func=ACT.Sqrt, bias=self.eps_t[:], scale=1.0 / D), reads=[sr], writes=[sr])
                    kb.op(kb.dve, lambda: nc.vector.reciprocal(st[:, 2:3], st[:, 1:2]), reads=[sr], writes=[sr])
                    kb.op(kb.dve, lambda: nc.vector.tensor_scalar(out=xn[:], in0=xt[:], scalar1=st[:, 2:3], scalar2=None, op0=ALU.mult), reads=[xr, sr], writes=[xnr])
                    for half in range(2):
                        b = pbank % 4
                        pbank += 1
                        pv = self.ps[b][:].bitcast(BF16)
                        for k8 in range(8):
                            kc = half * 8 + k8
                            kb.op(kb.pe, lambda kc=kc, k8=k8, pv=pv: nc.tensor.transpose(pv[:, k8 * 128:(k8 + 1) * 128], xn[:, kc * 128:(kc + 1) * 128], self.ident_b()),
                                  reads=[xnr, self.r_cst], writes=[self.pr[b]])
                        for k8 in range(8):
                            kc = half * 8 + k8
                            dst = hts[:, kc, tt * 128:(tt + 1) * 128]
                            src = pv[:, k8 * 128:(k8 + 1) * 128]
                            if k8 % 2 == 0:
                                kb.op(kb.act, lambda dst=dst, src=src, kc=kc: nc.scalar.activation(out=dst, in_=src, func=ACT.Identity, bias=self.bcol[:, kc:kc + 1], scale=self.acol[:, kc:kc + 1]),
                                      reads=[self.pr[b], self.r_cols], writes=[hres])
                            else:
                                kb.op(kb.dve, lambda dst=dst, src=src, kc=kc: nc.vector.tensor_scalar(out=dst, in0=src, scalar1=self.acol[:, kc:kc + 1], scalar2=self.bcol[:, kc:kc + 1], op0=ALU.mult, op1=ALU.add),
                                      reads=[self.pr[b], self.r_cols], writes=[hres])
                kb.dma(kb.pool, self.hT.rearrange("k p t -> p k t")[:, :, t4 * 512:(t4 + 1) * 512], hts[:], reads=[hres])

    def rstd_from_ss(self, st, sr, ncols):
        kb, nc = self.kb, self.nc
        kb.op(kb.dve, lambda: nc.vector.tensor_reduce(out=st[:, 4:5], in_=st[:, 0:ncols], axis=AX.X, op=ALU.add), reads=[sr], writes=[sr])
        kb.op(kb.act, lambda: nc.scalar.activation(out=st[:, 5:6], in_=st[:, 4:5], func=ACT.Sqrt, bias=self.eps_t[:], scale=1.0 / D), reads=[sr], writes=[sr])
        kb.op(kb.dve, lambda: nc.vector.reciprocal(st[:, 6:7], st[:, 5:6]), reads=[sr], writes=[sr])

    def stage_outproj(self, l, xsrc):
        kb, nc = self.kb, self.nc
        with contextlib.ExitStack() as ctx:
            T = lambda name, shape, dty: ctx.enter_context(_sbt(nc, name, shape, dty))
            with contextlib.ExitStack() as c2:
                self.prep_G(c2, l, "a")
            kb.barrier()
            wo = T("wo", [128, KC, D], BF16)
            r_wo = Res()
            wv = self.wb_out[l].rearrange("(k p) c -> p k c", p=128)
            for n in range(4):
                kb.dma(kb.sp, wo[:, :, n * 512:(n + 1) * 512], wv[:, :, n * 512:(n + 1) * 512], writes=[r_wo])
            ro = Ring(ctx, nc, "oo", 2, [128, KC, 512], BF16)
            rx = Ring(ctx, nc, "ox", 2, [128, D], F32)
            rt1 = Ring(ctx, nc, "ot1", 2, [128, D], F32)
            rst = Ring(ctx, nc, "ost", 4, [128, 8], F32)
            junk = T("ojunk", [128, 512], BF16)
            rj = Res()
            for t4 in range(NT // 4):
                ot, ores = ro.next()
                kb.dma(kb.sp, ot[:], self.oT.rearrange("k p t -> p k t")[:, :, t4 * 512:(t4 + 1) * 512], writes=[ores])
                for tt in range(4):
                    t = t4 * 4 + tt
                    xt, xr = rx.next()
                    t1, t1r = rt1.next()
                    st, sr = rst.next()
                    kb.dma(kb.sp, xt[:], xsrc[t * 128:(t + 1) * 128, :], writes=[xr])
                    base = (t % 2) * 4
                    for n in range(4):
                        b = base + n
                        for kc in range(KC):
                            kb.op(kb.pe, lambda kc=kc, b=b, n=n: nc.tensor.matmul(self.ps[b][:, :], lhsT=ot[:, kc, tt * 128:(tt + 1) * 128], rhs=wo[:, kc, n * 512:(n + 1) * 512],
                                                                             start=(kc == 0), stop=(kc == KC - 1)),
                                  reads=[ores, r_wo], writes=[self.pr[b]])
                        kb.op(kb.act, lambda b=b, n=n: nc.scalar.activation(out=junk[:], in_=self.ps[b][:, :], func=ACT.Square, accum_out=st[:, n:n + 1]), reads=[self.pr[b]], writes=[rj, sr])
                        kb.op(kb.dve, lambda b=b, n=n: nc.vector.tensor_tensor(out=t1[:, n * 512:(n + 1) * 512], in0=self.ps[b][:, :], in1=self.G[:, n * 512:(n + 1) * 512], op=ALU.mult),
                              reads=[self.pr[b], self.r_G], writes=[t1r])
                    self.rstd_from_ss(st, sr, 4)
                    kb.op(kb.dve, lambda: nc.vector.scalar_tensor_tensor(out=t1[:], in0=t1[:], scalar=st[:, 6:7], in1=xt[:], op0=ALU.mult, op1=ALU.add), reads=[t1r, sr, xr], writes=[t1r])
                    kb.dma(kb.pool, self.out[t * 128:(t + 1) * 128, :], t1[:], reads=[t1r])

    def stage_ffn(self, l, xsrc):
        kb, nc = self.kb, self.nc
        with contextlib.ExitStack() as ctx:
            T = lambda name, shape, dty: ctx.enter_context(_sbt(nc, name, shape, dty))
            with contextlib.ExitStack() as c2:
                self.prep_G(c2, l, "m")
            kb.barrier()
            rh = Ring(ctx, nc, "fh", 1, [128, KC, 512], BF16)
            uT = T("uT", [128, 64, 512], BF16)
            r_u = [Res() for _ in range(64)]
            rw1 = Ring(ctx, nc, "fw1", 3, [128, KC, 256], BF16)
            rw2 = Ring(ctx, nc, "fw2", 3, [128, 8, 512], BF16)
            rtmp = Ring(ctx, nc, "ftmp", 3, [128, 512], F32)
            ysb = T("ysb", [128, 4, D], F32)
            r_y = [Res() for _ in range(4)]
            rx = Ring(ctx, nc, "fx", 2, [128, D], F32)
            rst = Ring(ctx, nc, "fst", 8, [128, 8], F32)
            junk = T("fjunk", [128, 512], BF16)
            rj = Res()
            w1v = self.wb_f1[l].rearrange("(k p) c -> p k c", p=128)
            w2v = self.wb_f2[l].rearrange("(f p) c -> p f c", p=128)
            pb1 = 0
            import os
            for t4 in range(int(os.environ.get("FFN_BLOCKS", NT // 4))):
                ht, hres = rh.next()
                kb.dma(kb.sp, ht[:], self.hT.rearrange("k p t -> p k t")[:, :, t4 * 512:(t4 + 1) * 512], writes=[hres])
                for fb in range(32):
                    w1, w1r = rw1.next()
                    kb.dma(kb.sp, w1[:], w1v[:, :, fb * 256:(fb + 1) * 256], writes=[w1r])
                    for fi in range(2):
                        f = fb * 2 + fi
                        b = pb1 % 4
                        pb1 += 1
                        for kc in range(KC):
                            kb.op(kb.pe, lambda kc=kc, b=b, fi=fi: nc.tensor.matmul(self.ps[b][:, :], lhsT=w1[:, kc, fi * 128:(fi + 1) * 128], rhs=ht[:, kc, :],
                                                                               start=(kc == 0), stop=(kc == KC - 1)),
                                  reads=[w1r, hres], writes=[self.pr[b]])
                        tmp, tr = rtmp.next()
                        kb.op(kb.act, lambda b=b: nc.scalar.activation(out=tmp[:], in_=self.ps[b][:, :], func=ACT.Relu), reads=[self.pr[b]], writes=[tr])
                        kb.op(kb.dve, lambda f=f: nc.vector.tensor_tensor(out=uT[:, f, :], in0=tmp[:], in1=tmp[:], op=ALU.mult), reads=[tr], writes=[r_u[f]])
                if os.environ.get("FFN_PHASE") == "1":
                    continue
                sts = [rst.next() for _ in range(4)]
                for n in range(4):
                    for fg in range(8):
                        w2, w2r = rw2.next()
                        kb.dma(kb.sp, w2[:], w2v[:, fg * 8:(fg + 1) * 8, n * 512:(n + 1) * 512], writes=[w2r])
                        for j in range(8):
                            f = fg * 8 + j
                            for tt in range(4):
                                b = 4 + tt
                                kb.op(kb.pe, lambda f=f, j=j, tt=tt, b=b: nc.tensor.matmul(self.ps[b][:, :], lhsT=uT[:, f, tt * 128:(tt + 1) * 128], rhs=w2[:, j, :],
                                                                                       start=(f == 0), stop=(f == 63)),
                                      reads=[r_u[f], w2r], writes=[self.pr[b]])
                    for tt in range(4):
                        b = 4 + tt
                        st, sr = sts[tt]
                        if os.environ.get("FFN_NOEV") == "1":
                            continue
                        if os.environ.get("FFN_NOEV") != "2":
                            kb.op(kb.act, lambda b=b, st=st: nc.scalar.activation(out=junk[:], in_=self.ps[b][:, :], func=ACT.Square, accum_out=st[:, n:n + 1]), reads=[self.pr[b]], writes=[rj, sr])
                        kb.op(kb.dve, lambda b=b, tt=tt: nc.vector.tensor_tensor(out=ysb[:, tt, n * 512:(n + 1) * 512], in0=self.ps[b][:, :], in1=self.G[:, n * 512:(n + 1) * 512], op=ALU.mult),
                              reads=[self.pr[b], self.r_G], writes=[r_y[tt]])
                if os.environ.get("FFN_PHASE") == "2":
                    continue
                for tt in range(4):
                    t = t4 * 4 + tt
                    st, sr = sts[tt]
                    xt, xr = rx.next()
                    kb.dma(kb.sp, xt[:], xsrc[t * 128:(t + 1) * 128, :], writes=[xr])
                    self.rstd_from_ss(st, sr, 4)
                    kb.op(kb.dve, lambda tt=tt, st=st: nc.vector.scalar_tensor_tensor(out=xt[:], in0=ysb[:, tt, :], scalar=st[:, 6:7], in1=xt[:], op0=ALU.mult, op1=ALU.add),
                          reads=[r_y[tt], sr, xr], writes=[xr])
                    kb.dma(kb.pool, self.out[t * 128:(t + 1) * 128, :], xt[:], reads=[xr])

    def stage_inproj0(self):
        kb, nc = self.kb, self.nc
        hTv = self.hT.rearrange("k p t -> p k t")
        wv = self.wb_in0.rearrange("(k p) c -> p k c", p=128)
        qk_blocks = [(0, 0, 0), (512, 4, 0), (1024, 0, 1), (1536, 4, 1), (3080, 8, 0), (3592, 12, 0), (4104, 8, 1), (4616, 12, 1)]
        v_blocks = [(2048, 0), (2560, 4), (5128, 8), (5640, 12)]
        with contextlib.ExitStack() as ctx:
            T = lambda name, shape, dty: ctx.enter_context(_sbt(nc, name, shape, dty))
            rh = Ring(ctx, nc, "ih", 2, [128, KC, 512], BF16)
            rw = Ring(ctx, nc, "iw", 3, [128, KC, 512], BF16)
            rs = Ring(ctx, nc, "is", 4, [128, 512], BF16)
            wg = T("iwg", [128, KC, 128], BF16)
            nlf = T("nlf", [8, S], F32)
            brow, nb = T("ibrow", [1, 8], F32), T("inb", [8, 1], F32)
            etmp = T("ietmp", [8, 512], F32)
            r_wg, r_nlf, r_b, r_e = Res(), Res(), Res(), Res()
            kb.dma(kb.sp, wg[:], wv[:, :, 3008:3136], writes=[r_wg])
            kb.dma(kb.sp, brow[:], self.even_b[:, :], writes=[r_b])
            kb.op(kb.pe, lambda: nc.tensor.matmul(self.ps[7][0:8, 0:1], lhsT=brow[0:1, 0:8], rhs=self.cst_sb[0:1, C_ONE:C_ONE + 1], start=True, stop=True),
                  reads=[r_b, self.r_cst], writes=[self.pr[7]])
            kb.op(kb.dve, lambda: nc.vector.tensor_scalar(out=nb[:], in0=self.ps[7][0:8, 0:1], scalar1=-1.0, scalar2=None, op0=ALU.mult), reads=[self.pr[7]], writes=[r_b])
            pb = 0
            ev = 0
            for t4 in range(NT // 4):
                ht, hres = rh.next()
                kb.dma(kb.sp, ht[:], hTv[:, :, t4 * 512:(t4 + 1) * 512], writes=[hres])
                b = pb % 7
                pb += 1
                for kc in range(KC):
                    kb.op(kb.pe, lambda: nc.tensor.matmul(self.ps[b][0:8, :], lhsT=wg[:, kc, 64:72], rhs=ht[:, kc, :], start=(kc == 0), stop=(kc == KC - 1)),
                          reads=[r_wg, hres], writes=[self.pr[b]])
                kb.op(kb.act, lambda: nc.scalar.activation(out=etmp[:], in_=self.ps[b][0:8, :], func=ACT.Exp, bias=nb[:], scale=-1.0), reads=[self.pr[b], r_b], writes=[r_e])
                kb.op(kb.act, lambda: nc.scalar.activation(out=nlf[:, t4 * 512:(t4 + 1) * 512], in_=etmp[:], func=ACT.Ln, bias=self.one_t[0:8, :], scale=1.0), reads=[r_e], writes=[r_nlf])
                for (c0, h0, isk) in qk_blocks:
                    w, wr = rw.next()
                    kb.dma(kb.sp, w[:], wv[:, :, c0:c0 + 512], writes=[wr])
                    for hi in range(4):
                        b = pb % 7
                        pb += 1
                        for kc in range(KC):
                            kb.op(kb.pe, lambda: nc.tensor.matmul(self.ps[b][:, :], lhsT=w[:, kc, hi * 128:(hi + 1) * 128], rhs=ht[:, kc, :], start=(kc == 0), stop=(kc == KC - 1)),
                                  reads=[wr, hres], writes=[self.pr[b]])
                        stg, sr = rs.next()
                        if ev % 2 == 0:
                            kb.op(kb.act, lambda: nc.scalar.copy(stg[:], self.ps[b][:, :]), reads=[self.pr[b]], writes=[sr])
                        else:
                            kb.op(kb.dve, lambda: nc.vector.tensor_copy(stg[:], self.ps[b][:, :]), reads=[self.pr[b]], writes=[sr])
                        ev += 1
                        kb.dma(kb.pool, self.qkT[2 * (h0 + hi) + isk, :, t4 * 512:(t4 + 1) * 512], stg[:], reads=[sr])
                for (c0, h0) in v_blocks:
                    w, wr = rw.next()
                    kb.dma(kb.sp, w[:], wv[:, :, c0:c0 + 512], writes=[wr])
                    for tt in range(4):
                        j = t4 * 4 + tt
                        b = pb % 7
                        pb += 1
                        for kc in range(KC):
                            kb.op(kb.pe, lambda: nc.tensor.matmul(self.ps[b][:, :], lhsT=ht[:, kc, tt * 128:(tt + 1) * 128], rhs=w[:, kc, :], start=(kc == 0), stop=(kc == KC - 1)),
                                  reads=[wr, hres], writes=[self.pr[b]])
                        stg, sr = rs.next()
                        if ev % 2 == 0:
                            kb.op(kb.act, lambda: nc.scalar.copy(stg[:], self.ps[b][:, :]), reads=[self.pr[b]], writes=[sr])
                        else:
                            kb.op(kb.dve, lambda: nc.vector.tensor_copy(stg[:], self.ps[b][:, :]), reads=[self.pr[b]], writes=[sr])
                        ev += 1
                        kb.dma(kb.pool, self.vt[h0:h0 + 4, :, j, :].rearrange("h p d -> p h d"), stg[:].rearrange("p (h d) -> p h d", h=4), reads=[sr])
            Pt = T("iP", [8, S], F32)
            R = T("iR", [8, 8, 32], F32)
            r_P, r_R = Res(), Res()
            kb.op(kb.dve, lambda: nc.vector.tensor_tensor_scan(out=Pt[:], data0=self.one_t[0:8, 0:1].to_broadcast([8, S]), data1=nlf[:], initial=0.0, op0=ALU.mult, op1=ALU.add),
                  reads=[r_nlf], writes=[r_P])
            for j in range(NT):
                kb.op(kb.pe, lambda: nc.tensor.transpose(self.ps[0][:, j * 8:(j + 1) * 8], Pt[0:8, j * 128:(j + 1) * 128], self.ident_f(8)), reads=[r_P, self.r_cst], writes=[self.pr[0]])
            kb.op(kb.dve, lambda: nc.vector.tensor_copy(self.Pcol[:], self.ps[0][:, 0:256]), reads=[self.pr[0]], writes=[self.r_P])
            for h in range(8):
                kb.op(kb.dve, lambda: nc.vector.tensor_scalar(out=R[:, h, :], in0=Pt[0:8, 0:S:128], scalar1=self.cst_sb[0:8, C_ID + h:C_ID + h + 1], scalar2=None, op0=ALU.mult),
                      reads=[r_P, self.r_cst], writes=[r_R])
            kb.op(kb.pe, lambda: nc.tensor.matmul(self.ps[1][:, 0:256], lhsT=self.cst_sb[0:8, C_ONE:C_ONE + 128], rhs=R[:].rearrange("k h i -> k (h i)"), start=True, stop=True),
                  reads=[r_R, self.r_cst], writes=[self.pr[1]])
            kb.op(kb.dve, lambda: nc.vector.tensor_copy(self.Pb[:], self.ps[1][:, 0:256]), reads=[self.pr[1]], writes=[self.r_P])
            kb.dma(kb.pool, self.dbg[:, 0:256], self.Pcol[:], reads=[self.r_P])
            kb.dma(kb.pool, self.dbg[:, 256:512], self.Pb[:], reads=[self.r_P])

    def stage_attn0(self):
        kb, nc = self.kb, self.nc
        scale = HD ** -0.5
        import os
        heads = [int(v) for v in os.environ["ATTN_HEADS"].split(",")] if "ATTN_HEADS" in os.environ else range(16)
        with contextlib.ExitStack() as ctx:
            T = lambda name, shape, dty: ctx.enter_context(_sbt(nc, name, shape, dty))
            rq = Ring(ctx, nc, "aq", 2, [128, S], BF16)
            rk = Ring(ctx, nc, "ak", 2, [128, S], BF16)
            rnk = Ring(ctx, nc, "ank", 1, [128, S], BF16)
            rv = Ring(ctx, nc, "av", 2, [128, NT, 129], BF16)
            roT = Ring(ctx, nc, "aoT", 2, [128, S], BF16)
            rp = Ring(ctx, nc, "ap", 5, [128, 128], BF16)
            rsp = Ring(ctx, nc, "asp", 4, [128, 128], BF16)
            re_ = Ring(ctx, nc, "ae", 3, [128, 128], F32)
            rT = Ring(ctx, nc, "aT", 2, [128, 128], F32)
            rR = Ring(ctx, nc, "aR", 4, [128, 128], F32)
            rbias = Ring(ctx, nc, "ab", 5, [128, 32], F32)
            rosb = Ring(ctx, nc, "aosb", 3, [128, 128], BF16)
            rrd = Ring(ctx, nc, "ard", 3, [128, 1], F32)
            for vt_, vr_ in zip(rv.t, rv.r):
                kb.op(kb.pool, lambda: nc.gpsimd.memset(vt_[:, :, 128:129], 1.0), writes=[vr_])
            cg = self.cast_gen(ctx, self.late_cast_jobs()) if (CAST_OVERLAP and self.on("cast")) else iter(())
            tri, trs = self.cb[:, C_TRI:C_TRI + 128], self.cb[:, C_TRS:C_TRS + 128]
            low, ones_b = self.cb[:, C_LOW:C_LOW + 128], self.cb[:, C_ONE:C_ONE + 128]
            Pcol = self.Pcol[:].rearrange("p (j h) -> p j h", h=8)
            Pb = self.Pb[:].rearrange("p (h i) -> p h i", i=32)
            sb_ = [0]
            xb_ = [0]
            for h in heads:
                fox = h < 8
                qT, qr = rq.next()
                kT, kr = rk.next()
                V, vr = rv.next()
                oTs, oTr = roT.next()
                kb.dma(kb.sp, qT[:], self.qkT[2 * h, :, :], writes=[qr])
                kb.dma(kb.sp, kT[:], self.qkT[2 * h + 1, :, :], writes=[kr])
                kb.dma(kb.sp, V[:, :, 0:128], self.vt[h, :, :, :], writes=[vr])
                if not fox:
                    nkT, nkr = rnk.next()
                    kb.op(kb.dve, lambda: nc.vector.tensor_scalar(out=nkT[:], in0=kT[:], scalar1=-scale, scalar2=None, op0=ALU.mult), reads=[kr], writes=[nkr])
                def finalize(i, normalize):
                    bO = 3 + (i % 2)
                    osb, osr = rosb.next()
                    if normalize:
                        rd, rdr = rrd.next()
                        kb.op(kb.dve, lambda: nc.vector.reciprocal(rd[:], self.ps[bO][:, 128:129]), reads=[self.pr[bO]], writes=[rdr])
                        kb.op(kb.act, lambda: nc.scalar.activation(out=osb[:], in_=self.ps[bO][:, 0:128], func=ACT.Copy, scale=rd[:, 0:1]), reads=[self.pr[bO], rdr], writes=[osr])
                    else:
                        kb.op(kb.act, lambda: nc.scalar.copy(osb[:], self.ps[bO][:, 0:128]), reads=[self.pr[bO]], writes=[osr])
                    pv = self.ps[5][:].bitcast(BF16)
                    kb.op(kb.pe, lambda: nc.tensor.transpose(pv[:, 0:128], osb[:], self.ident_b()), reads=[osr, self.r_cst], writes=[self.pr[5]])
                    kb.op(kb.dve, lambda: nc.vector.tensor_copy(oTs[:, i * 128:(i + 1) * 128], pv[:, 0:128]), reads=[self.pr[5]], writes=[oTr])
                    next(cg, None)

                if fox:
                    biases = {}

                    def f1(i, j):
                        if j == 0:
                            bias, br = rbias.next()
                            kb.op(kb.dve, lambda: nc.vector.tensor_scalar(out=bias[:], in0=Pcol[:, :, h], scalar1=Pb[:, h, i:i + 1], scalar2=None, op0=ALU.subtract),
                                  reads=[self.r_P], writes=[br])
                            biases[i] = (bias, br)
                        bS = sb_[0] % 3
                        sb_[0] += 1
                        kb.op(kb.pe, lambda: nc.tensor.matmul(self.ps[bS][:, 0:128], lhsT=kT[:, j * 128:(j + 1) * 128], rhs=qT[:, i * 128:(i + 1) * 128], start=True, stop=True),
                              reads=[kr, qr], writes=[self.pr[bS]])
                        return bS

                    def f2(i, j, bS):
                        bO = 3 + (i % 2)
                        bias, br = biases[i]
                        PT, pr_ = rp.next()
                        kb.op(kb.act, lambda: nc.scalar.activation(out=PT[:], in_=self.ps[bS][:, 0:128], func=ACT.Exp, bias=bias[:, j:j + 1], scale=scale),
                              reads=[self.pr[bS], br], writes=[pr_])
                        if j == i:
                            kb.op(kb.pool, lambda: nc.gpsimd.tensor_tensor(out=PT[:], in0=PT[:], in1=tri, op=ALU.mult), reads=[pr_, self.r_cst], writes=[pr_])
                        kb.op(kb.pe, lambda: nc.tensor.matmul(self.ps[bO][:, 0:129], lhsT=PT[:], rhs=V[:, j, :], start=(j == 0), stop=(j == i)),
                              reads=[pr_, vr], writes=[self.pr[bO]])
                        if j == i:
                            finalize(i, True)

                    pend = []
                    for i in range(NT):
                        for j in range(i + 1):
                            pend.append((i, j, f1(i, j)))
                            if len(pend) > 2:
                                f2(*pend.pop(0))
                    while pend:
                        f2(*pend.pop(0))
                else:
                    raccs = {}
                    sps = {}

                    def g1(i, idx, j, last):
                        bS = sb_[0] % 2
                        sb_[0] += 1
                        kb.op(kb.pe, lambda: nc.tensor.matmul(self.ps[bS][:, 0:128], lhsT=kT[:, j * 128:(j + 1) * 128], rhs=qT[:, i * 128:(i + 1) * 128], start=True, stop=True),
                              reads=[kr, qr], writes=[self.pr[bS]])
                        e, er = re_.next()
                        SP, spr = rsp.next()
                        kb.op(kb.act, lambda: nc.scalar.activation(out=e[:], in_=self.ps[bS][:, 0:128], func=ACT.Exp, scale=scale), reads=[self.pr[bS]], writes=[er])
                        kb.op(kb.act, lambda: nc.scalar.activation(out=SP[:], in_=e[:], func=ACT.Ln, bias=self.one_t[:], scale=1.0), reads=[er], writes=[spr])
                        if j == i:
                            kb.op(kb.pool, lambda: nc.gpsimd.tensor_tensor(out=SP[:], in0=SP[:], in1=trs, op=ALU.mult), reads=[spr, self.r_cst], writes=[spr])
                        sps[(i, idx)] = (SP, spr)

                    def g2(i, idx, j, last):
                        SP, spr = sps.pop((i, idx))
                        bX = (2, 6)[xb_[0] % 2]
                        xb_[0] += 1
                        kb.op(kb.pe, lambda: nc.tensor.matmul(self.ps[bX][:, 0:128], lhsT=low, rhs=SP[:], start=True, stop=False), reads=[spr, self.r_cst], writes=[self.pr[bX]])
                        kb.op(kb.pe, lambda: nc.tensor.matmul(self.ps[bX][:, 0:128], lhsT=nkT[:, j * 128:(j + 1) * 128], rhs=qT[:, i * 128:(i + 1) * 128], start=False, stop=True),
                              reads=[nkr, qr], writes=[self.pr[bX]])
                        A, ar = rp.next()
                        if idx == 0:
                            raccs[i] = rR.next()
                            kb.op(kb.act, lambda: nc.scalar.activation(out=A[:], in_=self.ps[bX][:, 0:128], func=ACT.Exp, scale=-1.0), reads=[self.pr[bX]], writes=[ar])
                        else:
                            Racc, rr = raccs[i]
                            Tt, tr_ = rT.next()
                            kb.op(kb.dve, lambda: nc.vector.tensor_tensor(out=Tt[:], in0=self.ps[bX][:, 0:128], in1=Racc[:], op=ALU.add), reads=[self.pr[bX], rr], writes=[tr_])
                            kb.op(kb.act, lambda: nc.scalar.activation(out=A[:], in_=Tt[:], func=ACT.Exp, scale=-1.0), reads=[tr_], writes=[ar])
                        if j == i:
                            kb.op(kb.pool, lambda: nc.gpsimd.tensor_tensor(out=A[:], in0=A[:], in1=trs, op=ALU.mult), reads=[ar, self.r_cst], writes=[ar])
                        if not last:
                            Racc, rr = raccs[i]
                            kb.op(kb.pe, lambda: nc.tensor.matmul(self.ps[7][:, 0:128], lhsT=ones_b, rhs=SP[:], start=True, stop=True), reads=[spr, self.r_cst], writes=[self.pr[7]])
                            if idx == 0:
                                kb.op(kb.dve, lambda: nc.vector.tensor_copy(Racc[:], self.ps[7][:, 0:128]), reads=[self.pr[7]], writes=[rr])
                            else:
                                kb.op(kb.dve, lambda: nc.vector.tensor_tensor(out=Racc[:], in0=self.ps[7][:, 0:128], in1=Racc[:], op=ALU.add), reads=[self.pr[7], rr], writes=[rr])
                        sps[("A", i, idx)] = (A, ar)

                    def g3(i, idx, j, last):
                        A, ar = sps.pop(("A", i, idx))
                        bO = 3 + (i % 2)
                        kb.op(kb.pe, lambda: nc.tensor.matmul(self.ps[bO][:, 0:128], lhsT=A[:], rhs=V[:, j, 0:128], start=(idx == 0), stop=last),
                              reads=[ar, vr], writes=[self.pr[bO]])
                        if last:
                            finalize(i, False)

                    stream = []
                    for i in range(NT):
                        js = [j for j in range(i, i - SB_WIN, -1) if j >= 0]
                        for idx, j in enumerate(js):
                            stream.append((i, idx, j, idx == len(js) - 1))
                    n = len(stream)
                    for k in range(n + 2):
                        if k < n:
                            g1(*stream[k])
                        if 0 <= k - 1 < n:
                            g2(*stream[k - 1])
                        if 0 <= k - 2 < n:
                            g3(*stream[k - 2])
                kb.dma(kb.pool, self.oT[h, :, :], oTs[:], reads=[oTr])
            for _ in cg:
                pass

    def stage_inproj1(self):
        kb, nc = self.kb, self.nc
        hTv = self.hT.rearrange("k p t -> p k t")
        wv = self.wb_in1.rearrange("(k p) c -> p k c", p=128)
        with contextlib.ExitStack() as ctx:
            T = lambda name, shape, dty: ctx.enter_context(_sbt(nc, name, shape, dty))
            sinq, cosq = T("sinq", [128, NT, 64], F32), T("cosq", [128, NT, 64], F32)
            r_tab = Res()
            with contextlib.ExitStack() as c2:
                T2 = lambda name, shape, dty: c2.enter_context(_sbt(nc, name, shape, dty))
                pi32, pf32, post = T2("pi32", [32, 128], I32), T2("pf32", [32, 128], F32), T2("post", [128, 32], F32)
                ang, u, ki, kf, m = (T2("ang", [128, NT * 64], F32), T2("ru", [128, NT * 64], F32), T2("rki", [128, NT * 64], I32),
                                     T2("rkf", [128, NT * 64], F32), T2("rm", [128, NT * 64], F32))
                npi = T2("npi", [128, 1], F32)
                r = Res()
                kb.dma(kb.sp, pi32[:], self.pos[:, :], writes=[r])
                kb.op(kb.dve, lambda: nc.vector.memset(npi[:], -3.14159), writes=[r])
                kb.op(kb.dve, lambda: nc.vector.tensor_copy(pf32[:], pi32[:]), reads=[r], writes=[r])
                kb.op(kb.pe, lambda: nc.tensor.transpose(self.ps[0][:, 0:32], pf32[:], self.ident_f(32)), reads=[r, self.r_cst], writes=[self.pr[0]])
                kb.op(kb.dve, lambda: nc.vector.tensor_copy(post[:], self.ps[0][:, 0:32]), reads=[self.pr[0]], writes=[r])
                for j in range(NT):
                    kb.op(kb.dve, lambda: nc.vector.tensor_scalar(out=ang[:, j * 64:(j + 1) * 64], in0=self.cst_sb[:, C_INV:C_INV + 64], scalar1=post[:, j:j + 1], scalar2=None, op0=ALU.mult),
                          reads=[r, self.r_cst], writes=[r])
                for tab, shift in ((sinq, 0.5), (cosq, 0.75)):
                    V = nc.vector
                    kb.op(kb.dve, lambda: V.tensor_scalar(out=u[:], in0=ang[:], scalar1=1.0 / (2 * np.pi), scalar2=shift, op0=ALU.mult, op1=ALU.add), reads=[r], writes=[r])
                    kb.op(kb.dve, lambda: V.tensor_copy(ki[:], u[:]), reads=[r], writes=[r])
                    kb.op(kb.dve, lambda: V.tensor_copy(kf[:], ki[:]), reads=[r], writes=[r])
                    kb.op(kb.dve, lambda: V.tensor_tensor(out=u[:], in0=u[:], in1=kf[:], op=ALU.subtract), reads=[r], writes=[r])
                    kb.op(kb.dve, lambda: V.tensor_scalar(out=m[:], in0=u[:], scalar1=0.0, scalar2=None, op0=ALU.is_lt), reads=[r], writes=[r])
                    kb.op(kb.dve, lambda: V.tensor_tensor(out=u[:], in0=u[:], in1=m[:], op=ALU.add), reads=[r], writes=[r])
                    kb.op(kb.dve, lambda: V.tensor_scalar(out=m[:], in0=u[:], scalar1=1.0, scalar2=None, op0=ALU.is_ge), reads=[r], writes=[r])
                    kb.op(kb.dve, lambda: V.tensor_tensor(out=u[:], in0=u[:], in1=m[:], op=ALU.subtract), reads=[r], writes=[r])
                    kb.op(kb.act, lambda: nc.scalar.activation(out=tab[:].rearrange("p j k -> p (j k)"), in_=u[:], func=ACT.Sin, bias=npi[:], scale=6.28318), reads=[r], writes=[r_tab])
                kb.barrier()
            rh = Ring(ctx, nc, "jh", 2, [128, KC, 512], BF16)
            rw = Ring(ctx, nc, "jw", 3, [128, KC, 512], BF16)
            qtile = [T(f"jq{tt}", [128, 16, 128], BF16) for tt in range(4)]
            qres = [Res() for _ in range(4)]
            rta = Ring(ctx, nc, "jta", 2, [128, 512], F32)
            rtb = Ring(ctx, nc, "jtb", 2, [128, 512], F32)
            rqT = Ring(ctx, nc, "jqT", 2, [128, 16, 512], BF16)
            rqiT = Ring(ctx, nc, "jqiT", 2, [128, 8, 512], BF16)
            rkT = Ring(ctx, nc, "jkT", 2, [128, 512], BF16)
            rkiT = Ring(ctx, nc, "jkiT", 2, [128, 512], BF16)
            rkr = Ring(ctx, nc, "jkr", 2, [128, 128], BF16)
            rvs = Ring(ctx, nc, "jvs", 2, [128, 128], BF16)
            rws = Ring(ctx, nc, "jws", 2, [128, 16], F32)
            pbank = [0]
            tbank = [0]
            evc = [0]

            def proj(ht, hres, w, wr, tt, ncols):
                b = pbank[0] % 6
                pbank[0] += 1
                for kc in range(KC):
                    kb.op(kb.pe, lambda: nc.tensor.matmul(self.ps[b][:, 0:ncols], lhsT=ht[:, kc, tt * 128:(tt + 1) * 128], rhs=w[:, kc, 0:ncols], start=(kc == 0), stop=(kc == KC - 1)),
                          reads=[wr, hres], writes=[self.pr[b]])
                return b

            def rope(b, c0, nh, half, j, dst, dres):
                x = self.ps[b][:, c0:c0 + nh * 2 * half].rearrange("p (h two d) -> p h two d", h=nh, two=2)
                x1, x2 = x[:, :, 0, :], x[:, :, 1, :]
                st = 64 // half
                cs = cosq[:, j, 0:64:st].unsqueeze(1).to_broadcast([128, nh, half])
                sn = sinq[:, j, 0:64:st].unsqueeze(1).to_broadcast([128, nh, half])
                ta, tar = rta.next()
                tb, tbr = rtb.next()
                av = ta[:, 0:nh * half].rearrange("p (h d) -> p h d", h=nh)
                bv = tb[:, 0:nh * half].rearrange("p (h d) -> p h d", h=nh)
                V = nc.vector
                kb.op(kb.dve, lambda: V.tensor_tensor(out=av, in0=x1, in1=cs, op=ALU.mult), reads=[self.pr[b], r_tab], writes=[tar])
                kb.op(kb.dve, lambda: V.tensor_tensor(out=bv, in0=x2, in1=sn, op=ALU.mult), reads=[self.pr[b], r_tab], writes=[tbr])
                kb.op(kb.pool, lambda: nc.gpsimd.tensor_tensor(out=dst[:, :, 0:half], in0=av, in1=bv, op=ALU.subtract), reads=[tar, tbr], writes=[dres])
                ta, tar = rta.next()
                tb, tbr = rtb.next()
                av = ta[:, 0:nh * half].rearrange("p (h d) -> p h d", h=nh)
                bv = tb[:, 0:nh * half].rearrange("p (h d) -> p h d", h=nh)
                kb.op(kb.dve, lambda: V.tensor_tensor(out=av, in0=x2, in1=cs, op=ALU.mult), reads=[self.pr[b], r_tab], writes=[tar])
                kb.op(kb.dve, lambda: V.tensor_tensor(out=bv, in0=x1, in1=sn, op=ALU.mult), reads=[self.pr[b], r_tab], writes=[tbr])
                kb.op(kb.pool, lambda: nc.gpsimd.tensor_tensor(out=dst[:, :, half:2 * half], in0=av, in1=bv, op=ALU.add), reads=[tar, tbr], writes=[dres])

            def transp(src_list, sres, dst_fn, dres):
                k = 0
                while k < len(src_list):
                    grp = src_list[k:k + 8]
                    b = 6 + tbank[0] % 2
                    tbank[0] += 1
                    pv = self.ps[b][:].bitcast(BF16)
                    for g, src in enumerate(grp):
                        kb.op(kb.pe, lambda: nc.tensor.transpose(pv[:, g * 128:(g + 1) * 128], src, self.ident_b()), reads=[sres, self.r_cst], writes=[self.pr[b]])
                    for g, src in enumerate(grp):
                        if evc[0] % 2 == 0:
                            kb.op(kb.act, lambda: nc.scalar.copy(dst_fn(k + g), pv[:, g * 128:(g + 1) * 128]), reads=[self.pr[b]], writes=[dres])
                        else:
                            kb.op(kb.dve, lambda: nc.vector.tensor_copy(dst_fn(k + g), pv[:, g * 128:(g + 1) * 128]), reads=[self.pr[b]], writes=[dres])
                        evc[0] += 1
                    k += 8

            for t4 in range(NT // 4):
                ht, hres = rh.next()
                kb.dma(kb.sp, ht[:], hTv[:, :, t4 * 512:(t4 + 1) * 512], writes=[hres])
                qTs, qTr = rqT.next()
                qiTs, qiTr = rqiT.next()
                kTs, kTr = rkT.next()
                kiTs, kiTr = rkiT.next()
                for cbk in range(4):
                    w, wr = rw.next()
                    kb.dma(kb.sp, w[:], wv[:, :, cbk * 512:(cbk + 1) * 512], writes=[wr])
                    for tt in range(4):
                        j = t4 * 4 + tt
                        b = proj(ht, hres, w, wr, tt, 512)
                        rope(b, 0, 4, 64, j, qtile[tt][:, cbk * 4:(cbk + 1) * 4, :], qres[tt])
                for tt in range(4):
                    transp([qtile[tt][:, h, :] for h in range(16)], qres[tt], lambda h: qTs[:, h, tt * 128:(tt + 1) * 128], qTr)
                w, wr = rw.next()
                kb.dma(kb.sp, w[:, :, 0:256], wv[:, :, 2048:2304], writes=[wr])
                for tt in range(4):
                    j = t4 * 4 + tt
                    b = proj(ht, hres, w, wr, tt, 256)
                    kr_, krr = rkr.next()
                    rope(b, 0, 1, 64, j, kr_[:].rearrange("p (h d) -> p h d", h=1), krr)
                    vs, vsr = rvs.next()
                    kb.op(kb.act, lambda: nc.scalar.copy(vs[:], self.ps[b][:, 128:256]), reads=[self.pr[b]], writes=[vsr])
                    kb.dma(kb.pool, self.v1d[:, j, :], vs[:], reads=[vsr])
                    transp([kr_[:]], krr, lambda h: kTs[:, tt * 128:(tt + 1) * 128], kTr)
                for cbk in range(2):
                    w, wr = rw.next()
                    kb.dma(kb.sp, w[:], wv[:, :, 2304 + cbk * 512:2304 + (cbk + 1) * 512], writes=[wr])
                    for tt in range(4):
                        j = t4 * 4 + tt
                        b = proj(ht, hres, w, wr, tt, 512)
                        qv = qtile[tt][:].rearrange("p a b -> p (a b)")[:, cbk * 512:(cbk + 1) * 512].rearrange("p (h d) -> p h d", h=8)
                        rope(b, 0, 8, 32, j, qv, qres[tt])
                for tt in range(4):
                    flat = qtile[tt][:].rearrange("p a b -> p (a b)")
                    transp([flat[:, pr * 128:(pr + 1) * 128] for pr in range(8)], qres[tt], lambda pr: qiTs[:, pr, tt * 128:(tt + 1) * 128], qiTr)
                w, wr = rw.next()
                kb.dma(kb.sp, w[:, :, 0:80], wv[:, :, 3328:3408], writes=[wr])
                for tt in range(4):
                    j = t4 * 4 + tt
                    b = proj(ht, hres, w, wr, tt, 80)
                    kr_, krr = rkr.next()
                    rope(b, 0, 1, 32, j, kr_[:, 0:64].rearrange("p (h d) -> p h d", h=1), krr)
                    kb.op(kb.pool, lambda: nc.gpsimd.tensor_copy(kr_[:, 64:128], kr_[:, 0:64]), reads=[krr], writes=[krr])
                    ws, wsr = rws.next()
                    kb.op(kb.act, lambda: nc.scalar.copy(ws[:], self.ps[b][:, 64:80]), reads=[self.pr[b]], writes=[wsr])
                    kb.dma(kb.pool, self.wd[:, j, :], ws[:], reads=[wsr])
                    transp([kr_[:]], krr, lambda h: kiTs[:, tt * 128:(tt + 1) * 128], kiTr)
                sl = slice(t4 * 512, (t4 + 1) * 512)
                kb.dma(kb.pool, self.qT1.rearrange("h p t -> p h t")[:, :, sl], qTs[:], reads=[qTr])
                kb.dma(kb.pool, self.qiT1.rearrange("h p t -> p h t")[:, :, sl], qiTs[:], reads=[qiTr])
                kb.dma(kb.pool, self.kT1d[:, sl], kTs[:], reads=[kTr])
                kb.dma(kb.pool, self.kiT2d[:, sl], kiTs[:], reads=[kiTr])

    def stage_dsa(self):
        kb, nc = self.kb, self.nc
        scale = HD ** -0.5
        import os
        ntiles = int(os.environ.get("DSA_TILES", NT))
        with contextlib.ExitStack() as ctx:
            T = lambda name, shape, dty: ctx.enter_context(_sbt(nc, name, shape, dty))
            kT1, kiT2 = T("kT1", [128, S], BF16), T("kiT2", [128, S], BF16)
            V1, wsb = T("V1", [128, NT, 129], BF16), T("wsb", [128, NT, 16], F32)
            id4 = T("id4", [128, 512], BF16)
            r_k = Res()
            kb.dma(kb.sp, kT1[:], self.kT1d[:, :], writes=[r_k])
            kb.dma(kb.sp, kiT2[:], self.kiT2d[:, :], writes=[r_k])
            kb.dma(kb.sp, V1[:, :, 0:128], self.v1d[:, :, :], writes=[r_k])
            kb.dma(kb.sp, wsb[:], self.wd[:, :, :], writes=[r_k])
            kb.op(kb.pool, lambda: nc.gpsimd.memset(V1[:, :, 128:129], 1.0), writes=[r_k])
            for g in range(4):
                kb.op(kb.pool, lambda: nc.gpsimd.tensor_copy(id4[:, g * 128:(g + 1) * 128], self.ident_b()), reads=[self.r_cst], writes=[r_k])
            rI = Ring(ctx, nc, "dI", 4, [128, S], F32)
            rNM = Ring(ctx, nc, "dNM", 4, [128, S], BF16)
            rqi = Ring(ctx, nc, "dqi", 2, [128, 8, 128], BF16)
            rq = Ring(ctx, nc, "dq", 3, [128, 16, 128], BF16)
            rWd = Ring(ctx, nc, "dWd", 2, [128, 16, 128], BF16)
            rR = Ring(ctx, nc, "dR", 4, [128, 512], BF16)
            rPT = Ring(ctx, nc, "dPT", 4, [128, 512], BF16)
            roT = Ring(ctx, nc, "doT", 1, [128, 16, 512], BF16)
            rm8 = Ring(ctx, nc, "dm8", 4, [128, 8], F32)
            rthr = Ring(ctx, nc, "dthr", 4, [128, 1], F32)
            rrden = Ring(ctx, nc, "drden", 2, [128, 512], F32)
            ib_ = [0]
            sb_ = [0]
            tiles = {}
            oT_cur = [None, None]

            qts = {}

            def load_q(i):
                qT, qr = rq.next()
                kb.dma(kb.sp, qT[:], self.qT1.rearrange("h p t -> p h t")[:, :, i * 128:(i + 1) * 128], writes=[qr])
                qts[i] = (qT, qr)

            def phase_a_index(i):
                n_i = 128 * (i + 1)
                qiT, qir = rqi.next()
                Wd, wdr = rWd.next()
                Isb, Ir = rI.next()
                kb.dma(kb.sp, qiT[:], self.qiT1.rearrange("h p t -> p h t")[:, :, i * 128:(i + 1) * 128], writes=[qir])
                for h in range(16):
                    kb.op(kb.pool, lambda: nc.gpsimd.tensor_scalar(out=Wd[:, h, :], in0=self.ident_b(), scalar1=wsb[:, i, h:h + 1], scalar2=0.25 * 0.125, op0=ALU.mult, op1=ALU.mult),
                          reads=[r_k, self.r_cst], writes=[wdr])

                def i1(sb, h):
                    nco = min(512, n_i - 512 * sb)
                    hp, pr = h % 2, h // 2
                    bI = ib_[0] % 3
                    ib_[0] += 1
                    kb.op(kb.pe, lambda: nc.tensor.matmul(self.ps[bI][:, 0:nco], lhsT=qiT[hp * 64:(hp + 1) * 64, pr, :], rhs=kiT2[hp * 64:(hp + 1) * 64, sb * 512:sb * 512 + nco], start=True, stop=True),
                          reads=[qir, r_k], writes=[self.pr[bI]])
                    return bI

                def i2(sb, h, bI):
                    nco = min(512, n_i - 512 * sb)
                    bA = 3 + (sb % 2)
                    R, rr = rR.next()
                    kb.op(kb.act, lambda: nc.scalar.activation(out=R[:, 0:nco], in_=self.ps[bI][:, 0:nco], func=ACT.Relu), reads=[self.pr[bI]], writes=[rr])
                    kb.op(kb.pe, lambda: nc.tensor.matmul(self.ps[bA][:, 0:nco], lhsT=Wd[:, h, :], rhs=R[:, 0:nco], start=(h == 0), stop=(h == 15)),
                          reads=[wdr, rr], writes=[self.pr[bA]])
                    if h == 15:
                        kb.op(kb.act, lambda: nc.scalar.copy(Isb[:, sb * 512:sb * 512 + nco], self.ps[bA][:, 0:nco]), reads=[self.pr[bA]], writes=[Ir])

                pend = []
                for sb in range((n_i + 511) // 512):
                    for h in range(16):
                        pend.append((sb, h, i1(sb, h)))
                        if len(pend) > 2:
                            i2(*pend.pop(0))
                    yield
                while pend:
                    i2(*pend.pop(0))
                kb.op(kb.dve, lambda: nc.vector.memset(Isb[0:64, n_i - 64:n_i], NEG), writes=[Ir])
                tiles[i] = dict(Isb=Isb, Ir=Ir, n=n_i)

            def phase_a_topk(group):
                chains = []
                for i in group:
                    t = tiles[i]
                    thr, thr_r = rthr.next()
                    t["thr"], t["thr_r"] = thr, thr_r
                    if t["n"] <= 256:
                        kb.op(kb.dve, lambda: nc.vector.memset(thr[:], -1.0e29), writes=[thr_r])
                    else:
                        chains.append(dict(t=t))
                for rnd in range(32):
                    if rnd > 0:
                        yield
                    for c in chains:
                        t, n_i = c["t"], c["t"]["n"]
                        m8, m8r = rm8.next()
                        c["m8"], c["m8r"] = m8, m8r
                        kb.op(kb.dve, lambda: nc.vector.max(out=m8[:], in_=t["Isb"][:, 0:n_i]), reads=[t["Ir"]], writes=[m8r])
                    for c in chains:
                        t, n_i = c["t"], c["t"]["n"]
                        m8, m8r = c["m8"], c["m8r"]
                        if rnd < 31:
                            kb.op(kb.dve, lambda: nc.vector.match_replace(out=t["Isb"][:, 0:n_i], in_to_replace=m8[:], in_values=t["Isb"][:, 0:n_i], imm_value=REM), reads=[m8r], writes=[t["Ir"]])
                        else:
                            kb.op(kb.dve, lambda: nc.vector.tensor_copy(t["thr"][:], m8[:, 7:8]), reads=[m8r], writes=[t["thr_r"]])
                for i in group:
                    t = tiles[i]
                    NM, nmr = rNM.next()
                    kb.op(kb.dve, lambda: nc.vector.tensor_scalar(out=NM[:, 0:t["n"]], in0=t["Isb"][:, 0:t["n"]], scalar1=t["thr"][:, 0:1], scalar2=-30000.0, op0=ALU.is_lt, op1=ALU.mult),
                          reads=[t["Ir"], t["thr_r"]], writes=[nmr])
                    kb.op(kb.dve, lambda: nc.vector.scalar_tensor_tensor(out=NM[:, 0:t["n"]], in0=t["Isb"][:, 0:t["n"]], scalar=-1.5e30, in1=NM[:, 0:t["n"]], op0=ALU.is_gt, op1=ALU.mult),
                          reads=[t["Ir"]], writes=[nmr])
                    t["NM"], t["nmr"] = NM, nmr

            def phase_b(i):
                t = tiles.pop(i)
                NM, nmr = t["NM"], t["nmr"]
                qT, qr = qts.pop(i)
                if i + 1 < ntiles:
                    load_q(i + 1)
                tt = i % 4
                if tt == 0:
                    oT_cur[0], oT_cur[1] = roT.next()
                oTs, oTr = oT_cur

                def b1(hg, j):
                    bS = (0, 1, 6)[sb_[0] % 3]
                    sb_[0] += 1
                    kb.op(kb.pe, lambda: nc.tensor.matmul(self.ps[bS][:, :], lhsT=kT1[:, j * 128:(j + 1) * 128], rhs=qT[:, hg * 4:(hg + 1) * 4, :].rearrange("p h t -> p (h t)"), start=True, stop=False),
                          reads=[r_k, qr], writes=[self.pr[bS]])
                    kb.op(kb.pe, lambda: nc.tensor.matmul(self.ps[bS][:, :], lhsT=NM[:, j * 128:(j + 1) * 128], rhs=id4[:], start=False, stop=True),
                          reads=[nmr, r_k], writes=[self.pr[bS]])
                    return bS

                def b2(hg, j, bS):
                    PT, ptr = rPT.next()
                    bOT, bDen = 2 + 2 * (hg % 2), 3 + 2 * (hg % 2)
                    kb.op(kb.act, lambda: nc.scalar.activation(out=PT[:], in_=self.ps[bS][:, :], func=ACT.Exp, scale=scale), reads=[self.pr[bS]], writes=[ptr])
                    kb.op(kb.pe, lambda: nc.tensor.matmul(self.ps[bOT][:, :], lhsT=V1[:, j, 0:128], rhs=PT[:], start=(j == 0), stop=(j == i)), reads=[ptr, r_k], writes=[self.pr[bOT]])
                    kb.op(kb.pe, lambda: nc.tensor.matmul(self.ps[bDen][:, :], lhsT=self.cb[:, C_ONE:C_ONE + 128], rhs=PT[:], start=(j == 0), stop=(j == i)), reads=[ptr, self.r_cst], writes=[self.pr[bDen]])

                def fin(hg):
                    bOT, bDen = 2 + 2 * (hg % 2), 3 + 2 * (hg % 2)
                    rden, rdr = rrden.next()
                    kb.op(kb.dve, lambda: nc.vector.reciprocal(rden[:], self.ps[bDen][:, :]), reads=[self.pr[bDen]], writes=[rdr])
                    kb.op(kb.dve, lambda: nc.vector.tensor_tensor(out=oTs[:, hg * 4:(hg + 1) * 4, tt * 128:(tt + 1) * 128], in0=self.ps[bOT][:, :].rearrange("p (h t) -> p h t", h=4),
                                                                  in1=rden[:].rearrange("p (h t) -> p h t", h=4), op=ALU.mult),
                          reads=[self.pr[bOT], rdr], writes=[oTr])

                pend = []
                for hg in range(4):
                    for j in range(i + 1):
                        pend.append((hg, j, b1(hg, j)))
                        if len(pend) > 2:
                            p = pend.pop(0)
                            b2(*p)
                            if p[1] == i:
                                yield
                                fin(p[0])
                while pend:
                    p = pend.pop(0)
                    b2(*p)
                    if p[1] == i:
                        yield
                        fin(p[0])
                if tt == 3 or i == ntiles - 1:
                    t4 = i // 4
                    kb.dma(kb.pool, self.oT.rearrange("k p t -> p k t")[:, :, t4 * 512:(t4 + 1) * 512], oTs[:], reads=[oTr])

            groups = [list(range(g, min(g + 2, ntiles))) for g in range(0, ntiles, 2)]
            load_q(0)
            for i in groups[0]:
                for _ in phase_a_index(i):
                    pass
            for gi, grp in enumerate(groups):
                tg = phase_a_topk(grp)
                if gi > 0:
                    for i in groups[gi - 1]:
                        for _ in phase_b(i):
                            for _r in range(3):
                                next(tg, None)
                if gi + 1 < len(groups):
                    for i in groups[gi + 1]:
                        for _ in phase_a_index(i):
                            next(tg, None)
                for _ in tg:
                    pass
            for i in groups[-1]:
                for _ in phase_b(i):
                    pass


def make_in_maps(inputs):
    f = lambda a: np.ascontiguousarray(a)
    cst = make_consts()
    maps = []
    for b in range(8):
        maps.append({
            "x": f(inputs["x"][b]), "c": f(inputs["c"][b].reshape(16, 128)),
            "pos": f(inputs["positions"][b].reshape(32, 128).astype(np.int32)),
            "ada_w": inputs["ada_w"], "ada_b": inputs["ada_b"], "norm_g": inputs["norm_g"],
            "mix_w_out": inputs["mix_w_out"], "even_w_in": f(inputs["even_w_in"][0]),
            "even_b": f(inputs["even_b_forget"].reshape(1, 8)), "odd_w_in": f(inputs["odd_w_in"][0]),
            "ff_w1": inputs["ff_w1"], "ff_w2": inputs["ff_w2"], "cst": cst,
        })
    return maps


def kernel(**inputs):
    inputs = {k: np.asarray(v) for k, v in inputs.items()}
    prog = Prog()
    res = run_bass_kernel_spmd(prog.nc, make_in_maps(inputs), core_ids=list(range(8)))
    return np.stack([r["out"] for r in res.results], axis=0).astype(np.float32)
```
